# Optimizing a Trainium2 kernel written in Bass

```python
import math
import jax, jax.numpy as jnp
from jax import lax
import numpy as np

D_MODEL = 2048
BATCH = 4
SEQ = 4096
DEPTH = 1

CONV_WIDTH = D_MODEL // 2
CONV_KERNEL = 31
SSM_WIDTH = D_MODEL // 2
SSM_GROUP = 16
SSM_GROUPS = SSM_WIDTH // SSM_GROUP
SSM_STATE = 64
N_BRANCHES = 2
IN_WIDTH = 2 * CONV_WIDTH + SSM_WIDTH + N_BRANCHES * D_MODEL
PEER_HEADS = 8
PEER_KEYS = 128
PEER_EXPERTS = PEER_KEYS * PEER_KEYS
PEER_QDIM = 256
PEER_HALF = PEER_QDIM // 2
PEER_TOPK = 16
PEER_TOKEN_BLOCK = 128
RMS_EPS = 1e-6
LN_EPS = 1e-5
DT_MIN = 1e-3
DT_MAX = 1e-1

kernel_name = "hybrid_conv_s5_peer_block"


def rmsnorm(x, g):
    xf = x.astype(jnp.float32)
    y = xf * lax.rsqrt(jnp.mean(xf * xf, axis=-1, keepdims=True) + RMS_EPS)
    return (y * g.astype(jnp.float32)).astype(x.dtype)


def layernorm(x, g, b):
    xf = x.astype(jnp.float32)
    mu = jnp.mean(xf, axis=-1, keepdims=True)
    var = jnp.mean(jnp.square(xf - mu), axis=-1, keepdims=True)
    y = (xf - mu) * lax.rsqrt(var + LN_EPS)
    return (y * g.astype(jnp.float32) + b.astype(jnp.float32)).astype(x.dtype)


def conformer_conv(a, w_dw, b_dw, ln_g, ln_b, w_pw):
    glu = a[..., :CONV_WIDTH] * jax.nn.sigmoid(a[..., CONV_WIDTH:])
    padded = jnp.pad(glu, ((0, 0), (CONV_KERNEL - 1, 0), (0, 0)))
    y = lax.conv_general_dilated(
        padded, w_dw.astype(glu.dtype)[:, None, :], window_strides=(1,), padding='VALID',
        dimension_numbers=('NWC', 'WIO', 'NWC'), feature_group_count=CONV_WIDTH)
    y = y + b_dw.astype(glu.dtype)
    y = jax.nn.silu(layernorm(y, ln_g, ln_b))
    return y @ w_pw


def s5_ssm(u, a_re, a_im, log_dt, b_re, b_im, c_re, c_im, d_skip, w_val, w_gate):
    bsz, seq, _ = u.shape
    uf = u.astype(jnp.float32)
    ug = uf.reshape(bsz, seq, SSM_GROUPS, SSM_GROUP)
    lam = lax.complex(a_re.astype(jnp.float32), a_im.astype(jnp.float32))
    dt = jnp.exp(log_dt.astype(jnp.float32))[:, None]
    lam_bar = jnp.exp(lam * dt)
    b_mat = lax.complex(b_re.astype(jnp.float32), b_im.astype(jnp.float32))
    b_bar = ((lam_bar - 1.0) / lam)[..., None] * b_mat
    bu = jnp.einsum('gph,bsgh->bsgp', b_bar, ug.astype(jnp.complex64))
    a_seq = jnp.broadcast_to(lam_bar[None, None], (1, seq, SSM_GROUPS, SSM_STATE))

    def combine(left, right):
        a_l, b_l = left
        a_r, b_r = right
        return a_r * a_l, a_r * b_l + b_r

    _, states = lax.associative_scan(combine, (a_seq, bu), axis=1)
    y = (jnp.einsum('ghp,bsgp->bsgh', c_re.astype(jnp.float32), jnp.real(states))
         - jnp.einsum('ghp,bsgp->bsgh', c_im.astype(jnp.float32), jnp.imag(states)))
    y = y.reshape(bsz, seq, SSM_WIDTH) + d_skip.astype(jnp.float32) * uf
    z = jax.nn.gelu(y).astype(u.dtype)
    return (z @ w_val) * jax.nn.sigmoid(z @ w_gate)


def peer(h, w_q, sub_keys, u_tab, v_tab):
    bsz, seq, d = h.shape
    tokens = h.reshape(bsz * seq // PEER_TOKEN_BLOCK, PEER_TOKEN_BLOCK, d)

    def block(xb):
        q = (xb @ w_q).reshape(PEER_TOKEN_BLOCK, PEER_HEADS, 2, PEER_HALF)
        s = jnp.einsum('thcd,hcnd->thcn', q, sub_keys).astype(jnp.float32)
        s_top, i_top = lax.top_k(s, PEER_TOPK)
        cand = s_top[:, :, 0, :, None] + s_top[:, :, 1, None, :]
        cand_idx = i_top[:, :, 0, :, None] * PEER_KEYS + i_top[:, :, 1, None, :]
        cand = cand.reshape(PEER_TOKEN_BLOCK, PEER_HEADS, PEER_TOPK * PEER_TOPK)
        cand_idx = cand_idx.reshape(PEER_TOKEN_BLOCK, PEER_HEADS, PEER_TOPK * PEER_TOPK)
        best, pos = lax.top_k(cand, PEER_TOPK)
        expert = jnp.take_along_axis(cand_idx, pos, axis=-1)
        gates = jax.nn.softmax(best, axis=-1).astype(xb.dtype)
        u_sel = u_tab[expert]
        v_sel = v_tab[expert]
        act = jax.nn.gelu(jnp.einsum('td,thkd->thk', xb, u_sel))
        return jnp.einsum('thk,thkd->td', gates * act, v_sel)

    out = lax.map(block, tokens)
    return out.reshape(bsz, seq, d)


def setup_inputs(seed: int = 0) -> dict:
    key = jax.random.key(seed)
    ks = jax.random.split(key, 32)
    f32 = jnp.float32
    L, D = DEPTH, D_MODEL

    def nrm(k, shape, scale):
        return jax.random.normal(k, shape, f32) * scale

    n_idx = jnp.arange(SSM_STATE, dtype=f32)
    return {
        "x": nrm(ks[0], (BATCH, SEQ, D), 1.0),
        "norm_mix": 1.0 + nrm(ks[1], (L, D), 0.02),
        "w_in": nrm(ks[2], (L, D, IN_WIDTH), D ** -0.5),
        "b_gate": nrm(ks[3], (L, N_BRANCHES * D), 0.01),
        "conv_w_dw": nrm(ks[4], (L, CONV_KERNEL, CONV_WIDTH), CONV_KERNEL ** -0.5),
        "conv_b_dw": nrm(ks[5], (L, CONV_WIDTH), 0.01),
        "conv_ln_g": 1.0 + nrm(ks[6], (L, CONV_WIDTH), 0.02),
        "conv_ln_b": nrm(ks[7], (L, CONV_WIDTH), 0.01),
        "conv_w_out": nrm(ks[8], (L, CONV_WIDTH, D), CONV_WIDTH ** -0.5),
        "ssm_a_re": -0.5 + nrm(ks[9], (L, SSM_GROUPS, SSM_STATE), 0.01),
        "ssm_a_im": math.pi * n_idx + nrm(ks[10], (L, SSM_GROUPS, SSM_STATE), 0.01),
        "ssm_log_dt": jax.random.uniform(ks[11], (L, SSM_GROUPS), f32, math.log(DT_MIN), math.log(DT_MAX)),
        "ssm_b_re": nrm(ks[12], (L, SSM_GROUPS, SSM_STATE, SSM_GROUP), (2.0 * SSM_GROUP) ** -0.5),
        "ssm_b_im": nrm(ks[13], (L, SSM_GROUPS, SSM_STATE, SSM_GROUP), (2.0 * SSM_GROUP) ** -0.5),
        "ssm_c_re": nrm(ks[14], (L, SSM_GROUPS, SSM_GROUP, SSM_STATE), (2.0 * SSM_STATE) ** -0.5),
        "ssm_c_im": nrm(ks[15], (L, SSM_GROUPS, SSM_GROUP, SSM_STATE), (2.0 * SSM_STATE) ** -0.5),
        "ssm_d": nrm(ks[16], (L, SSM_WIDTH), 1.0),
        "ssm_w_val": nrm(ks[17], (L, SSM_WIDTH, D), SSM_WIDTH ** -0.5),
        "ssm_w_gate": nrm(ks[18], (L, SSM_WIDTH, D), SSM_WIDTH ** -0.5),
        "w_out": nrm(ks[19], (L, D, D), D ** -0.5),
        "norm_ffn": 1.0 + nrm(ks[20], (L, D), 0.02),
        "peer_w_q": nrm(ks[21], (L, D, PEER_HEADS * PEER_QDIM), D ** -0.5),
        "peer_sub_keys": nrm(ks[22], (L, PEER_HEADS, 2, PEER_KEYS, PEER_HALF), PEER_HALF ** -0.5),
        "peer_u": nrm(ks[23], (L, PEER_EXPERTS, D), D ** -0.5),
        "peer_v": nrm(ks[24], (L, PEER_EXPERTS, D), PEER_HEADS ** -0.5),
        "norm_final": 1.0 + nrm(ks[25], (D,), 0.02),
    }


def reference(x, norm_mix, w_in, b_gate, conv_w_dw, conv_b_dw, conv_ln_g, conv_ln_b, conv_w_out,
              ssm_a_re, ssm_a_im, ssm_log_dt, ssm_b_re, ssm_b_im, ssm_c_re, ssm_c_im, ssm_d,
              ssm_w_val, ssm_w_gate, w_out, norm_ffn, peer_w_q, peer_sub_keys, peer_u, peer_v,
              norm_final):
    bsz, seq, _ = x.shape
    for l in range(DEPTH):
        h = rmsnorm(x, norm_mix[l])
        proj = h @ w_in[l]
        conv_in = proj[..., :2 * CONV_WIDTH]
        ssm_in = proj[..., 2 * CONV_WIDTH:2 * CONV_WIDTH + SSM_WIDTH]
        gate_in = proj[..., 2 * CONV_WIDTH + SSM_WIDTH:]
        branch_conv = conformer_conv(conv_in, conv_w_dw[l], conv_b_dw[l], conv_ln_g[l], conv_ln_b[l], conv_w_out[l])
        branch_ssm = s5_ssm(ssm_in, ssm_a_re[l], ssm_a_im[l], ssm_log_dt[l], ssm_b_re[l], ssm_b_im[l],
                            ssm_c_re[l], ssm_c_im[l], ssm_d[l], ssm_w_val[l], ssm_w_gate[l])
        gates = jax.nn.sigmoid(gate_in + b_gate[l]).reshape(bsz, seq, N_BRANCHES, D_MODEL)
        merged = gates[:, :, 0, :] * branch_conv + gates[:, :, 1, :] * branch_ssm
        x = x + merged @ w_out[l]
        h = rmsnorm(x, norm_ffn[l])
        x = x + peer(h, peer_w_q[l], peer_sub_keys[l], peer_u[l], peer_v[l])
    return rmsnorm(x, norm_final)
```

```python
import math
import numpy as np
import concourse.bass as bass
import concourse.mybir as mybir
from concourse.bass_utils import run_bass_kernel_spmd

F32 = mybir.dt.float32
BF16 = mybir.dt.bfloat16
U32 = mybir.dt.uint32
AF = mybir.ActivationFunctionType
ALU = mybir.AluOpType
AX = mybir.AxisListType

D = 2048
DK = 16
CW = 1024
CT = 8
KCONV = 31
NSEQ = 4096
NCORES = 8
RMS_EPS = 1e-6
LN_EPS = 1e-5
NEG = -1.0e30


class Buf:
    __slots__ = ("name", "w", "r")

    def __init__(self, name=""):
        self.name = name
        self.w = None
        self.r = []


class Op:
    __slots__ = ("eng", "fn", "deps", "signal", "tok", "dma_key")

    def __init__(self, eng, fn, dma_key):
        self.eng = eng
        self.fn = fn
        self.deps = []
        self.signal = False
        self.tok = None
        self.dma_key = dma_key


class Ctx:
    ENGS = ("pe", "dve", "act", "pool", "sp")

    def __init__(self, nc):
        self.nc = nc
        self.e = {"pe": nc.tensor, "dve": nc.vector, "act": nc.scalar, "pool": nc.gpsimd, "sp": nc.sync}
        self.ops = []
        self.gen = 0
        self.sem = {k: nc.semaphore("sem_" + k).__enter__() for k in self.ENGS if k != "sp"}
        self.cnt = {k: 0 for k in self.ENGS}
        self.seen = {k: {} for k in self.ENGS}
        self.dsem = {}
        self.dcnt = {}
        self.bufs = []
        self.nops = 0

    def buf(self, name=""):
        b = Buf(name)
        self.bufs.append(b)
        return b

    def add(self, eng, fn, reads=(), writes=(), dma_key=None, chain=False):
        op = Op(eng, fn, dma_key)
        deps = set()
        for b in reads:
            if b.w is not None:
                deps.add(b.w)
        for b in writes:
            if b.w is not None:
                if not (chain and b.w.eng == eng and b.w.dma_key is None and dma_key is None):
                    deps.add(b.w)
            for o in b.r:
                deps.add(o)
        deps.discard(op)
        op.deps = list(deps)
        for d in op.deps:
            d.signal = True
        for b in writes:
            b.w = op
            b.r = []
        for b in reads:
            b.r.append(op)
        self.ops.append(op)
        return op

    def dma(self, out, in_, reads=(), writes=(), key="d", q="sp"):
        return self.add(q, lambda e: e.dma_start(out=out, in_=in_), reads, writes, dma_key=key)

    def dma_k(self, sb_t, dram2d, nk, reads, writes, key, store=False, q="sp"):
        for k in range(nk):
            if store:
                self.dma(dram2d[k * 128:(k + 1) * 128, :], sb_t[:, k, :], reads, writes, key=key, q=q)
            else:
                self.dma(sb_t[:, k, :], dram2d[k * 128:(k + 1) * 128, :], reads, writes, key=key)

    def _wait(self, eng, semkey, sem, val):
        s = self.seen[eng]
        if s.get(semkey, 0) >= val:
            return
        s[semkey] = val
        self.e[eng].wait_ge(sem, val)

    def flush(self):
        last = {}
        for op in self.ops:
            if op.dma_key is None:
                last[op.eng] = op
        for op in last.values():
            op.signal = True
        for op in self.ops:
            eng = op.eng
            for d in op.deps:
                if d.dma_key is not None:
                    k = d.dma_key
                    self._wait(eng, "D" + k, self.dsem[k], self.dcnt[k])
                else:
                    if d.eng == "pe" and eng == "pe" and op.dma_key is None:
                        continue
                    self._wait(eng, d.eng, self.sem[d.eng], d.tok)
            ins = op.fn(self.e[eng])
            self.nops += 1
            if op.dma_key is not None:
                k = op.dma_key
                if k not in self.dsem:
                    self.dsem[k] = self.nc.semaphore("dsem_" + k).__enter__()
                    self.dcnt[k] = 0
                self.dcnt[k] += 16
                ins.then_inc(self.dsem[k], 16)
                op.tok = self.dcnt[k]
            elif op.signal:
                self.cnt[eng] += 1
                ins.then_inc(self.sem[eng], 1)
                op.tok = self.cnt[eng]
        for eng in self.ENGS:
            for k in self.dsem:
                if self.dcnt[k] > 0:
                    self._wait(eng, "D" + k, self.dsem[k], self.dcnt[k])
            for o in self.ENGS:
                if o != eng and self.cnt[o] > 0:
                    self._wait(eng, o, self.sem[o], self.cnt[o])
        self.ops = []
        for b in self.bufs:
            b.w = None
            b.r = []
        self.gen += 1
        self.sem = {k: self.nc.semaphore(f"sem_{k}_{self.gen}").__enter__() for k in self.ENGS if k != "sp"}
        self.cnt = {k: 0 for k in self.ENGS}
        for e in self.ENGS:
            for k in self.ENGS:
                self.seen[e].pop(k, None)


class SB:
    def __init__(self, nc):
        self.nc = nc
        self.guards = []

    def sb(self, name, shape, dt):
        g = self.nc.sbuf_tensor(name, list(shape), dt)
        t = g.__enter__()
        self.guards.append(g)
        return t

    def ps(self, name, shape, dt):
        g = self.nc.psum_tensor(name, list(shape), dt)
        t = g.__enter__()
        self.guards.append(g)
        return t

    def release(self):
        for g in reversed(self.guards):
            g.__exit__(None, None, None)
        self.guards = []


def build(NT, debug=False, iso=False, big=True):
    NALL = 2 * NT
    NG = NALL // 512
    NGO = NT // 512
    nc = bass.Bass("TRN2", target_bir_lowering=False)
    cx = Ctx(nc)

    def din(name, shape, dt=F32):
        return nc.dram_tensor(name, list(shape), dt, kind="ExternalInput").ap()

    def dscr(name, shape, dt=F32):
        if iso:
            w = nc.dram_tensor(name, list(shape), dt, kind="ExternalOutput").ap()
            r = nc.dram_tensor(name + "_in", list(shape), dt, kind="ExternalInput").ap()
            return r, w
        kind = "ExternalOutput" if debug else "Internal"
        a = nc.dram_tensor(name, list(shape), dt, kind=kind).ap()
        return a, a

    xin = din("xin", [NALL, D])
    g1b = din("g1b", [128, D])
    g2b = din("g2b", [128, D])
    gfb = din("gfb", [128, D])
    w_in_r = din("w_in_r", [56, 128, DK * 128])
    bgate = din("bgate", [128, 32])
    cw = din("cw", [128, CT, KCONV])
    cb = din("cb", [128, CT])
    lng = din("lng", [128, CT])
    lnb = din("lnb", [128, CT])
    cwo = din("cwo", [128, CT, D])
    wval = din("wval", [128, CT, D])
    wgate = din("wgate", [128, CT, D])
    wout = din("wout", [128, DK, D])
    wq = din("wq", [128, DK, D])
    kT = din("kT", [128, 16, 128])
    UTr = din("UTr", [128, 128, DK * 128] if big else [1, 1, 1])
    Vr = din("Vr", [128, 128, D] if big else [1, 1, 1])
    sA = din("sA", [128, 3, 32])
    sB = din("sB", [128, 3, CT * 64])
    bT = din("bT", [128, 2, CT * 64])
    cTp = din("cTp", [128, 2, 32 * 128])
    dsk = din("dsk", [128, CT])
    maskB = din("maskB", [128, 128])
    rowm = din("rowm", [128, 1])
    ident = din("ident", [128, 128])
    onesc = din("onesc", [128, 128])
    iota = din("iota", [128, 128])

    out = nc.dram_tensor("out", [NT, D], F32, kind="ExternalOutput").ap()

    gluT_r, gluT_w = dscr("gluT", [CW, NT + 512])
    ssmT_r, ssmT_w = dscr("ssmT", [CW, NALL])
    gateT_r, gateT_w = dscr("gateT", [2 * D, NT])
    mconvT_r, mconvT_w = dscr("mconvT", [D, NT])
    zT_r, zT_w = dscr("zT", [CW, NT], BF16)
    mergedT_r, mergedT_w = dscr("mergedT", [D, NT], BF16)
    x1s_r, x1s_w = dscr("x1s", [NT, D])
    hn2T_r, hn2T_w = dscr("hn2T", [D, NT], BF16)
    Mscr_r, Mscr_w = dscr("Mscr", [NGO, 16, 128, 512 * 8], BF16)
    qTs_r, qTs_w = dscr("qTs", [D, NT])

    PS = [nc.psum_tensor(f"ps{i}", [128, 512], F32).__enter__() for i in range(8)]
    PSB = [cx.buf(f"ps{i}") for i in range(8)]

    def psbufs():
        return [cx.buf(f"ps{i}") for i in range(8)]

    def rmsnorm_T(sb, xt_ap, xb, gb_t, gb_b, hn, hn_b, junk, junk_b, st, st_b, identb, identb_b, psb, ps_i, dst_fn, dst_b):
        ss, rs = st[:, 0:1], st[:, 1:2]
        cx.add("act", lambda e: e.activation(out=junk[:], in_=xt_ap, func=AF.Square, accum_out=ss), [xb], [junk_b, st_b])
        cx.add("act", lambda e: e.activation(out=rs, in_=ss, func=AF.Sqrt, scale=1.0 / D, bias=eps_rms[:, 0:1]), [st_b], [st_b])
        cx.add("dve", lambda e: e.reciprocal(out=rs, in_=rs), [st_b], [st_b])
        cx.add("dve", lambda e: e.scalar_tensor_tensor(out=hn[:], in0=xt_ap, scalar=rs, in1=gb_t[:], op0=ALU.mult, op1=ALU.mult),
               [xb, st_b, gb_b], [hn_b])
        for half in range(2):
            pst = PS[ps_i + half].bitcast(BF16)
            for kk in range(8):
                k = half * 8 + kk
                cx.add("pe", lambda e, k=k, kk=kk, pst=pst: e.transpose(out=pst[:, kk * 128:(kk + 1) * 128], in_=hn[:, k * 128:(k + 1) * 128], identity=identb[:]),
                       [hn_b, identb_b], [psb[ps_i + half]])
            dst = dst_fn(half * 8, half * 8 + 8)
            cx.add("act", lambda e, pst=pst, dst=dst: e.copy(out=dst, in_=pst[:].rearrange("p (k t) -> p k t", k=8)),
                   [psb[ps_i + half]], [dst_b])

    def gelu_tanh(src_ap, src_b, shape, tmp1, tmp1_b, tmp2, tmp2_b, out_ap, out_b, extra_mul=None, extra_b=None):
        cx.add("act", lambda e: e.activation(out=tmp1, in_=src_ap, func=AF.Square), [src_b], [tmp1_b])
        cx.add("dve", lambda e: e.tensor_scalar(out=tmp1, in0=tmp1, scalar1=0.044715, scalar2=1.0, op0=ALU.mult, op1=ALU.add), [tmp1_b], [tmp1_b])
        cx.add("dve", lambda e: e.tensor_tensor(out=tmp1, in0=tmp1, in1=src_ap, op=ALU.mult), [tmp1_b, src_b], [tmp1_b])
        cx.add("act", lambda e: e.activation(out=tmp2, in_=tmp1, func=AF.Sigmoid, scale=1.5957691216057308), [tmp1_b], [tmp2_b])
        if extra_mul is None:
            cx.add("dve", lambda e: e.tensor_tensor(out=out_ap, in0=tmp2, in1=src_ap, op=ALU.mult), [tmp2_b, src_b], [out_b])
        else:
            cx.add("dve", lambda e: e.tensor_tensor(out=tmp2, in0=tmp2, in1=src_ap, op=ALU.mult), [tmp2_b, src_b], [tmp2_b])
            cx.add("dve", lambda e: e.tensor_tensor(out=out_ap, in0=tmp2, in1=extra_mul, op=ALU.mult), [tmp2_b, extra_b], [out_b])

    def load_cast(sb_stage, stage_bufs, counter, dram_ap, dst_ap, dst_b, n, key, cast_eng="pool"):
        s = counter[0] % 2
        counter[0] += 1
        cx.dma(dst_ap, dram_ap, [], [dst_b], key=f"{key}{s}", q="pool")

    eps_rms = nc.sbuf_tensor("eps_rms", [128, 1], F32).__enter__()
    eps_ln = nc.sbuf_tensor("eps_ln", [128, 1], F32).__enter__()
    ident_f = nc.sbuf_tensor("ident_f", [128, 128], F32).__enter__()
    ident_b = nc.sbuf_tensor("ident_b", [128, 128], BF16).__enter__()
    cb0 = cx.buf("const")
    cx.add("dve", lambda e: e.memset(eps_rms[:], RMS_EPS), [], [cb0])
    cx.add("dve", lambda e: e.memset(eps_ln[:], LN_EPS), [], [cb0])
    cx.dma(ident_f[:], ident, [], [cb0], key="c0")
    cx.add("dve", lambda e: e.tensor_copy(out=ident_b[:], in_=ident_f[:]), [cb0], [cb0])
    cx.flush()

    def stage_A():
        sb = SB(nc)
        psb = psbufs()
        constb = cx.buf("constA")
        hnT = sb.sb("A_hnT", [128, DK, NALL], BF16)
        hnTb = [cx.buf(f"hnT{g}") for g in range(NG)]
        gb_t = sb.sb("A_gb", [128, D], F32)
        gbb = cx.buf("gb")
        cx.dma(gb_t[:], g1b, [], [gbb], key="c0")
        bg_t = sb.sb("A_bg", [128, 32], F32)
        cx.dma(bg_t[:], bgate, [], [constb], key="c0")
        xts = [sb.sb(f"A_xt{i}", [128, D], F32) for i in range(2)]
        xtb = [cx.buf(f"xt{i}") for i in range(2)]
        hn = sb.sb("A_hn", [128, D], BF16)
        hn_b = cx.buf("hn")
        junk = sb.sb("A_junk", [128, D], BF16)
        junk_b = cx.buf("junk")
        sts = sb.sb("A_st", [128, 2], F32)
        st_b = cx.buf("st")
        idb = cx.buf("identb")
        for tt in range(NALL // 128):
            s = tt % 2
            cx.dma(xts[s][:], xin[tt * 128:(tt + 1) * 128, :], [], [xtb[s]], key=f"xt{s}")
            g = tt // 4
            rmsnorm_T(sb, xts[s][:], xtb[s], gb_t, gbb, hn, hn_b, junk, junk_b, sts, st_b, ident_b, idb, psb, 0,
                      lambda k0, k1, tt=tt: hnT[:, k0:k1, tt * 128:(tt + 1) * 128], hnTb[g])
        NW = 3
        wbf = [sb.sb(f"A_wbf{i}", [128, DK * 128], BF16) for i in range(NW)]
        wbfb = [cx.buf(f"wbf{i}") for i in range(NW)]
        ngc = NGO + 1
        abuf = sb.sb("A_abuf", [128, ngc, 512], F32)
        abufb = [cx.buf(f"abuf{i}") for i in range(ngc)]
        evs = [sb.sb(f"A_ev{i}", [128, 512], F32) for i in range(3)]
        evb = [cx.buf(f"ev{i}") for i in range(3)]
        sgs = [sb.sb(f"A_sg{i}", [128, 512], F32) for i in range(2)]
        sgb = [cx.buf(f"sg{i}") for i in range(2)]
        cnt = {"w": 0, "ps": 0, "ev": 0, "sg": 0}
        conv_groups = list(range(NG // 2 - 1, NG))
        own_groups = list(range(NG // 2, NG))
        order = []
        for c in range(8):
            order.append((c, "a", conv_groups))
            order.append((c + 8, "g", conv_groups))
        for c in range(16, 24):
            order.append((c, "s", list(range(NG))))
        for c in range(24, 56):
            order.append((c, "t", own_groups))
        for (ct, kind, groups) in order:
            ws = cnt["w"] % NW
            cnt["w"] += 1
            cx.dma(wbf[ws][:], w_in_r[ct], [], [wbfb[ws]], key=f"wbf{ws}", q="pool")
            for gi, g in enumerate(groups):
                pi = 2 + cnt["ps"] % 4
                cnt["ps"] += 1
                for k in range(DK):
                    cx.add("pe", lambda e, ws=ws, k=k, g=g, pi=pi: e.matmul(PS[pi][:], lhsT=wbf[ws][:, k * 128:(k + 1) * 128], rhs=hnT[:, k, g * 512:(g + 1) * 512],
                                                                            start=(k == 0), stop=(k == DK - 1)),
                           [wbfb[ws], hnTb[g]], [psb[pi]])
                if kind == "a":
                    cx.add("act", lambda e, gi=gi, pi=pi: e.copy(out=abuf[:, gi, :], in_=PS[pi][:]), [psb[pi]], [abufb[gi]])
                elif kind == "g":
                    c = ct - 8
                    si = cnt["sg"] % 2
                    cnt["sg"] += 1
                    ei = cnt["ev"] % 3
                    cnt["ev"] += 1
                    cx.add("act", lambda e, si=si, pi=pi: e.activation(out=sgs[si][:], in_=PS[pi][:], func=AF.Sigmoid), [psb[pi]], [sgb[si]])
                    cx.add("dve", lambda e, si=si, ei=ei, gi=gi: e.tensor_tensor(out=evs[ei][:], in0=abuf[:, gi, :], in1=sgs[si][:], op=ALU.mult),
                           [abufb[gi], sgb[si]], [evb[ei]])
                    cx.dma(gluT_w[c * 128:(c + 1) * 128, gi * 512:(gi + 1) * 512], evs[ei][:], [evb[ei]], [], key=f"ev{ei}", q="pool")
                elif kind == "s":
                    c = ct - 16
                    ei = cnt["ev"] % 3
                    cnt["ev"] += 1
                    cx.add("act", lambda e, ei=ei, pi=pi: e.copy(out=evs[ei][:], in_=PS[pi][:]), [psb[pi]], [evb[ei]])
                    cx.dma(ssmT_w[c * 128:(c + 1) * 128, g * 512:(g + 1) * 512], evs[ei][:], [evb[ei]], [], key=f"ev{ei}", q="pool")
                else:
                    c = ct - 24
                    ei = cnt["ev"] % 3
                    cnt["ev"] += 1
                    cx.add("act", lambda e, ei=ei, pi=pi, c=c: e.activation(out=evs[ei][:], in_=PS[pi][:], func=AF.Sigmoid, bias=bg_t[:, c:c + 1]),
                           [psb[pi], constb], [evb[ei]])
                    go = g - NG // 2
                    cx.dma(gateT_w[c * 128:(c + 1) * 128, go * 512:(go + 1) * 512], evs[ei][:], [evb[ei]], [], key=f"ev{ei}", q="pool")
        cx.flush()
        sb.release()

    def stage_C():
        sb = SB(nc)
        psb = psbufs()
        cb_ = cx.buf("constC")
        cw_t = sb.sb("C_cw", [128, CT, KCONV], F32)
        cb_t = sb.sb("C_cb", [128, CT], F32)
        lng_t = sb.sb("C_lng", [128, CT], F32)
        lnb_t = sb.sb("C_lnb", [128, CT], F32)
        ones_t = sb.sb("C_ones", [128, 128], F32)
        for t, d in ((cw_t, cw), (cb_t, cb), (lng_t, lng), (lnb_t, lnb), (ones_t, onesc)):
            cx.dma(t[:], d, [], [cb_], key="c0")
        cwo_bf = sb.sb("C_cwo", [128, CT, D], BF16)
        cwob = cx.buf("cwo")
        stg = [sb.sb(f"C_stg{i}", [128, D], F32) for i in range(2)]
        stgb = [cx.buf(f"stg{i}") for i in range(2)]
        ctr = [0]
        for c in range(CT):
            load_cast(stg, stgb, ctr, cwo[:, c, :], cwo_bf[:, c, :], cwob, D, "stg")
        gts = [sb.sb(f"C_gt{i}", [128, 512 + 32], F32) for i in range(2)]
        gtb = [cx.buf(f"gt{i}") for i in range(2)]
        gth = [sb.sb(f"C_gth{i}", [128, 512 + 32], BF16) for i in range(2)]
        gthb = [cx.buf(f"gth{i}") for i in range(2)]
        dg = sb.sb("C_dg", [128, CT, KCONV, 128], BF16)
        dgb = cx.buf("dg")
        idf = ident_f[:].unsqueeze(1).to_broadcast([128, KCONV, 128])
        for c in range(CT):
            cx.add("dve", lambda e, c=c: e.tensor_tensor(out=dg[:, c, :, :], in0=idf, in1=cw_t[:, c, :].unsqueeze(2).to_broadcast([128, KCONV, 128]), op=ALU.mult),
                   [cb_], [dgb], chain=True)
        y = sb.sb("C_y", [128, CT, 512], F32)
        yb = [cx.buf(f"y{c}") for c in range(CT)]
        ysq = sb.sb("C_ysq", [128, CT, 512], F32)
        ysqb = [cx.buf(f"ysq{c}") for c in range(CT)]
        mean_t = sb.sb("C_mean", [128, 512], F32)
        rstd_t = sb.sb("C_rstd", [128, 512], F32)
        stb = cx.buf("stats")
        zt = [sb.sb(f"C_z{i}", [128, 512], F32) for i in range(2)]
        ztb = [cx.buf(f"z{i}") for i in range(2)]
        actT = sb.sb("C_act", [128, CT, 512], BF16)
        actb = [cx.buf(f"act{c}") for c in range(CT)]
        g0t = [sb.sb(f"C_g0{i}", [128, 512], F32) for i in range(2)]
        g0b = [cx.buf(f"g0{i}") for i in range(2)]
        mct = [sb.sb(f"C_mc{i}", [128, 512], F32) for i in range(2)]
        mcb = [cx.buf(f"mc{i}") for i in range(2)]
        n = 0
        for gi in range(NGO):
            for c in range(CT):
                s = n % 2
                n += 1
                base = 512 + gi * 512 - 32
                cx.dma(gts[s][:], gluT_r[c * 128:(c + 1) * 128, base:base + 544], [], [gtb[s]], key=f"gt{s}")
                cx.add("act", lambda e, s=s: e.copy(out=gth[s][:], in_=gts[s][:]), [gtb[s]], [gthb[s]])
                pc = 4 + (n % 4)
                for k in range(KCONV):
                    cx.add("pe", lambda e, s=s, c=c, k=k, pc=pc: e.matmul(PS[pc][:], lhsT=dg[:, c, k, :], rhs=gth[s][:, 2 + k:514 + k], start=(k == 0), stop=(k == KCONV - 1)),
                           [dgb, gthb[s]], [psb[pc]])
                cx.add("act", lambda e, c=c, pc=pc: e.activation(out=y[:, c, :], in_=PS[pc][:], func=AF.Identity, bias=cb_t[:, c:c + 1]), [psb[pc], cb_], [yb[c]])
                cx.add("act", lambda e, c=c: e.activation(out=ysq[:, c, :], in_=y[:, c, :], func=AF.Square), [yb[c]], [ysqb[c]])
            for c in range(CT):
                cx.add("pe", lambda e, c=c: e.matmul(PS[0][:], lhsT=ones_t[:], rhs=y[:, c, :], start=(c == 0), stop=(c == CT - 1)), [cb_, yb[c]], [psb[0]])
            for c in range(CT):
                cx.add("pe", lambda e, c=c: e.matmul(PS[1][:], lhsT=ones_t[:], rhs=ysq[:, c, :], start=(c == 0), stop=(c == CT - 1)), [cb_, ysqb[c]], [psb[1]])
            cx.add("act", lambda e: e.copy(out=mean_t[:], in_=PS[0][:]), [psb[0]], [stb])
            cx.add("dve", lambda e: e.tensor_tensor(out=rstd_t[:], in0=mean_t[:], in1=mean_t[:], op=ALU.mult), [stb], [stb])
            cx.add("dve", lambda e: e.tensor_tensor(out=rstd_t[:], in0=PS[1][:], in1=rstd_t[:], op=ALU.subtract), [stb, psb[1]], [stb])
            cx.add("act", lambda e: e.activation(out=rstd_t[:], in_=rstd_t[:], func=AF.Sqrt, bias=eps_ln[:, 0:1]), [stb], [stb])
            cx.add("dve", lambda e: e.reciprocal(out=rstd_t[:], in_=rstd_t[:]), [stb], [stb])
            for c in range(CT):
                s = c % 2
                cx.add("dve", lambda e, s=s, c=c: e.tensor_tensor(out=zt[s][:], in0=y[:, c, :], in1=mean_t[:], op=ALU.subtract), [yb[c], stb], [ztb[s]])
                cx.add("dve", lambda e, s=s: e.tensor_tensor(out=zt[s][:], in0=zt[s][:], in1=rstd_t[:], op=ALU.mult), [ztb[s], stb], [ztb[s]])
                cx.add("act", lambda e, s=s, c=c: e.activation(out=actT[:, c, :], in_=zt[s][:], func=AF.Silu, scale=lng_t[:, c:c + 1], bias=lnb_t[:, c:c + 1]),
                       [ztb[s], cb_], [actb[c]])
            for dt in range(DK):
                pi = 2 + dt % 2
                s = dt % 2
                for c in range(CT):
                    cx.add("pe", lambda e, c=c, dt=dt, pi=pi: e.matmul(PS[pi][:], lhsT=cwo_bf[:, c, dt * 128:(dt + 1) * 128], rhs=actT[:, c, :],
                                                                        start=(c == 0), stop=(c == CT - 1)), [cwob, actb[c]], [psb[pi]])
                cx.dma(g0t[s][:], gateT_r[dt * 128:(dt + 1) * 128, gi * 512:(gi + 1) * 512], [], [g0b[s]], key=f"g0{s}")
                cx.add("dve", lambda e, s=s, pi=pi: e.tensor_tensor(out=mct[s][:], in0=PS[pi][:], in1=g0t[s][:], op=ALU.mult), [psb[pi], g0b[s]], [mcb[s]])
                cx.dma(mconvT_w[dt * 128:(dt + 1) * 128, gi * 512:(gi + 1) * 512], mct[s][:], [mcb[s]], [], key=f"mc{s}", q="pool")
        cx.flush()
        sb.release()

    TS = 256
    NCH = NALL // TS

    def stage_S():
        sb = SB(nc)
        sbt = SB(nc)
        psb = psbufs()
        PI = math.pi
        cosT = sb.sb("S_cosT", [128, 32, TS], F32)
        sinT = sb.sb("S_sinT", [128, 32, TS], F32)
        ur = sb.sb("S_ur", [128, 32], F32)
        ui = sb.sb("S_ui", [128, 32], F32)
        rA = sb.sb("S_rA", [128, 32], F32)
        BTr = sb.sb("S_BTr", [128, CT, 128], BF16)
        BTi = sb.sb("S_BTi", [128, CT, 128], BF16)
        BTr3 = sb.sb("S_BTr3", [128, CT, 128], BF16)
        BTi3 = sb.sb("S_BTi3", [128, CT, 128], BF16)
        CTr = sb.sb("S_CTr", [128, 32 * 128], BF16)
        CTi = sb.sb("S_CTi", [128, 32 * 128], BF16)
        dsk_t = sb.sb("S_dsk", [128, CT], F32)

        def lam_bar(pref, src, n):
            t = {k: sbt.sb(f"S_{pref}_{k}", [128, n], F32) for k in ("dt", "ar", "th", "r", "sn", "cs", "lr", "li", "tmp")}
            b = cx.buf(pref)
            raw = sbt.sb(f"S_{pref}_raw", [128, 3, n], F32)
            cx.dma(raw[:], src, [], [b], key="c0")
            cx.add("act", lambda e: e.activation(out=t["dt"][:], in_=raw[:, 2, :], func=AF.Exp), [b], [b])
            cx.add("dve", lambda e: e.tensor_tensor(out=t["ar"][:], in0=raw[:, 0, :], in1=t["dt"][:], op=ALU.mult), [b], [b])
            cx.add("dve", lambda e: e.tensor_tensor(out=t["th"][:], in0=raw[:, 1, :], in1=t["dt"][:], op=ALU.mult), [b], [b])
            cx.add("act", lambda e: e.activation(out=t["r"][:], in_=t["ar"][:], func=AF.Exp), [b], [b])
            for _ in range(5):
                cx.add("dve", lambda e: e.tensor_scalar(out=t["tmp"][:], in0=t["th"][:], scalar1=PI, scalar2=2 * PI, op0=ALU.is_gt, op1=ALU.mult), [b], [b])
                cx.add("dve", lambda e: e.tensor_tensor(out=t["th"][:], in0=t["th"][:], in1=t["tmp"][:], op=ALU.subtract), [b], [b])
            cx.add("act", lambda e: e.activation(out=t["sn"][:], in_=t["th"][:], func=AF.Sin), [b], [b])
            cx.add("dve", lambda e: e.tensor_scalar(out=t["cs"][:], in0=t["th"][:], scalar1=PI / 2, scalar2=None, op0=ALU.add), [b], [b])
            cx.add("dve", lambda e: e.tensor_scalar(out=t["tmp"][:], in0=t["cs"][:], scalar1=PI, scalar2=2 * PI, op0=ALU.is_gt, op1=ALU.mult), [b], [b])
            cx.add("dve", lambda e: e.tensor_tensor(out=t["tmp"][:], in0=t["cs"][:], in1=t["tmp"][:], op=ALU.subtract), [b], [b])
            cx.add("act", lambda e: e.activation(out=t["cs"][:], in_=t["tmp"][:], func=AF.Sin), [b], [b])
            cx.add("dve", lambda e: e.tensor_tensor(out=t["lr"][:], in0=t["r"][:], in1=t["cs"][:], op=ALU.mult), [b], [b])
            cx.add("dve", lambda e: e.tensor_tensor(out=t["li"][:], in0=t["r"][:], in1=t["sn"][:], op=ALU.mult), [b], [b])
            t["raw"] = raw
            return t, b

        def cmul(o_r, o_i, a_r, a_i, b_r, b_i, t1, t2, bufs_r, bufs_w, eng="dve"):
            cx.add(eng, lambda e: e.tensor_tensor(out=t1, in0=a_r, in1=b_r, op=ALU.mult), bufs_r, bufs_w)
            cx.add(eng, lambda e: e.tensor_tensor(out=t2, in0=a_i, in1=b_i, op=ALU.mult), bufs_r, bufs_w)
            cx.add(eng, lambda e: e.tensor_tensor(out=o_r, in0=t1, in1=t2, op=ALU.subtract), bufs_r, bufs_w)
            cx.add(eng, lambda e: e.tensor_tensor(out=t1, in0=a_r, in1=b_i, op=ALU.mult), bufs_r, bufs_w)
            cx.add(eng, lambda e: e.tensor_tensor(out=t2, in0=a_i, in1=b_r, op=ALU.mult), bufs_r, bufs_w)
            cx.add(eng, lambda e: e.tensor_tensor(out=o_i, in0=t1, in1=t2, op=ALU.add), bufs_r, bufs_w)

        A, Ab = lam_bar("A", sA, 32)
        tb = cx.buf("tables")
        ur2 = sbt.sb("S_ur2", [128, 32], F32)
        ui2 = sbt.sb("S_ui2", [128, 32], F32)
        tt1 = sbt.sb("S_tt1", [128, 32, TS // 2], F32)
        tt2 = sbt.sb("S_tt2", [128, 32, TS // 2], F32)
        cx.add("dve", lambda e: e.tensor_copy(out=rA[:], in_=A["r"][:]), [Ab], [tb])
        cx.add("dve", lambda e: e.memset(cosT[:, :, 0:1], 1.0), [], [tb])
        cx.add("dve", lambda e: e.memset(sinT[:, :, 0:1], 0.0), [], [tb])
        cx.add("dve", lambda e: e.tensor_copy(out=ur[:], in_=A["cs"][:]), [Ab], [tb])
        cx.add("dve", lambda e: e.tensor_copy(out=ui[:], in_=A["sn"][:]), [Ab], [tb])
        m = 1
        while m < TS:
            urb = ur[:].unsqueeze(2).to_broadcast([128, 32, m])
            uib = ui[:].unsqueeze(2).to_broadcast([128, 32, m])
            cmul(cosT[:, :, m:2 * m], sinT[:, :, m:2 * m], cosT[:, :, 0:m], sinT[:, :, 0:m], urb, uib, tt1[:, :, 0:m], tt2[:, :, 0:m], [tb], [tb])
            cmul(ur2[:], ui2[:], ur[:], ui[:], ur[:], ui[:], tt1[:, :, 0], tt2[:, :, 0], [tb], [tb])
            cx.add("dve", lambda e: e.tensor_copy(out=ur[:], in_=ur2[:]), [tb], [tb])
            cx.add("dve", lambda e: e.tensor_copy(out=ui[:], in_=ui2[:]), [tb], [tb])
            m *= 2
        Bp, Bb = lam_bar("B", sB, CT * 64)
        nB = CT * 64
        braw = sbt.sb("S_braw", [128, 2, nB], F32)
        cx.dma(braw[:], bT, [], [Bb], key="c0")
        tB = {k: sbt.sb(f"S_tB_{k}", [128, nB], F32) for k in ("nr", "den", "cr", "ci", "t1", "t2", "br", "bi")}
        cx.add("dve", lambda e: e.tensor_scalar(out=tB["nr"][:], in0=Bp["lr"][:], scalar1=-1.0, scalar2=None, op0=ALU.add), [Bb], [Bb])
        are, aim = Bp["raw"][:, 0, :], Bp["raw"][:, 1, :]
        cx.add("dve", lambda e: e.tensor_tensor(out=tB["den"][:], in0=are, in1=are, op=ALU.mult), [Bb], [Bb])
        cx.add("dve", lambda e: e.tensor_tensor(out=tB["t1"][:], in0=aim, in1=aim, op=ALU.mult), [Bb], [Bb])
        cx.add("dve", lambda e: e.tensor_tensor(out=tB["den"][:], in0=tB["den"][:], in1=tB["t1"][:], op=ALU.add), [Bb], [Bb])
        cx.add("dve", lambda e: e.reciprocal(out=tB["den"][:], in_=tB["den"][:]), [Bb], [Bb])
        cx.add("dve", lambda e: e.tensor_tensor(out=tB["t1"][:], in0=tB["nr"][:], in1=are, op=ALU.mult), [Bb], [Bb])
        cx.add("dve", lambda e: e.tensor_tensor(out=tB["t2"][:], in0=Bp["li"][:], in1=aim, op=ALU.mult), [Bb], [Bb])
        cx.add("dve", lambda e: e.tensor_tensor(out=tB["cr"][:], in0=tB["t1"][:], in1=tB["t2"][:], op=ALU.add), [Bb], [Bb])
        cx.add("dve", lambda e: e.tensor_tensor(out=tB["t1"][:], in0=Bp["li"][:], in1=are, op=ALU.mult), [Bb], [Bb])
        cx.add("dve", lambda e: e.tensor_tensor(out=tB["t2"][:], in0=tB["nr"][:], in1=aim, op=ALU.mult), [Bb], [Bb])
        cx.add("dve", lambda e: e.tensor_tensor(out=tB["ci"][:], in0=tB["t1"][:], in1=tB["t2"][:], op=ALU.subtract), [Bb], [Bb])
        cx.add("dve", lambda e: e.tensor_tensor(out=tB["cr"][:], in0=tB["cr"][:], in1=tB["den"][:], op=ALU.mult), [Bb], [Bb])
        cx.add("dve", lambda e: e.tensor_tensor(out=tB["ci"][:], in0=tB["ci"][:], in1=tB["den"][:], op=ALU.mult), [Bb], [Bb])
        cmul(tB["br"][:], tB["bi"][:], tB["cr"][:], tB["ci"][:], braw[:, 0, :], braw[:, 1, :], tB["t1"][:], tB["t2"][:], [Bb], [Bb])
        mB = sbt.sb("S_maskB", [128, 128], F32)
        cx.dma(mB[:], maskB, [], [Bb], key="c0")
        mBb = mB[:].rearrange("p (g q) -> p g q", g=2).unsqueeze(1).to_broadcast([128, CT, 2, 64])
        for src, dst in ((tB["br"], BTr), (tB["bi"], BTi)):
            sv = src[:].rearrange("p (k q) -> p k q", k=CT).unsqueeze(2).to_broadcast([128, CT, 2, 64])
            cx.add("dve", lambda e, sv=sv, dst=dst: e.tensor_tensor(out=dst[:].rearrange("p k (g q) -> p k g q", g=2), in0=sv, in1=mBb, op=ALU.mult), [Bb], [Bb])
        rm_t = sbt.sb("S_rowm", [128, 1], F32)
        cx.dma(rm_t[:], rowm, [], [Bb], key="c0")
        for src, dst in ((BTr, BTr3), (BTi, BTi3)):
            cx.add("dve", lambda e, src=src, dst=dst: e.tensor_scalar(out=dst[:], in0=src[:], scalar1=rm_t[:, 0:1], scalar2=None, op0=ALU.mult), [Bb], [Bb])
        Cb = cx.buf("C")
        cx.dma(CTr[:], cTp[:, 0, :], [], [Cb], key="cc", q="pool")
        cx.dma(CTi[:], cTp[:, 1, :], [], [Cb], key="cc", q="pool")
        cx.add("pool", lambda e: e.tensor_scalar(out=CTi[:], in0=CTi[:], scalar1=-1.0, scalar2=None, op0=ALU.mult), [Cb], [Cb])
        cx.dma(dsk_t[:], dsk, [], [Cb], key="c0")
        cx.flush()
        sbt.release()
        psb = psbufs()
        zin_r = sb.sb("S_zinr", [128, 32], F32)
        zin_i = sb.sb("S_zini", [128, 32], F32)
        zl_r = sb.sb("S_zlr", [128, 32], F32)
        zl_i = sb.sb("S_zli", [128, 32], F32)
        zc1 = sb.sb("S_zc1", [128, 32], F32)
        zc2 = sb.sb("S_zc2", [128, 32], F32)
        zb = cx.buf("zstate")
        cx.add("dve", lambda e: e.memset(zin_r[:], 0.0), [], [zb])
        cx.add("dve", lambda e: e.memset(zin_i[:], 0.0), [], [zb])
        NTMP = 4
        uts = [sb.sb(f"S_ut{i}", [128, CT, TS], F32) for i in range(2)]
        utb = [cx.buf(f"ut{i}") for i in range(2)]
        utsb = [sb.sb(f"S_utb{i}", [128, CT, TS], BF16) for i in range(2)]
        utbb = [cx.buf(f"utbb{i}") for i in range(2)]
        xrb = [sb.sb(f"S_xrb{i}", [128, TS], BF16) for i in range(NTMP)]
        xib = [sb.sb(f"S_xib{i}", [128, TS], BF16) for i in range(NTMP)]
        xrbb = [cx.buf(f"xrb{i}") for i in range(NTMP)]
        xibb = [cx.buf(f"xib{i}") for i in range(NTMP)]
        tmp = {k: [sb.sb(f"S_{k}{i}", [128, TS], F32) for i in range(NTMP)] for k in ("t1", "t2", "wr", "wi", "zr", "zi", "xr", "xi")}
        tmpb = {k: [cx.buf(f"{k}{i}") for i in range(NTMP)] for k in tmp}
        yv = [sb.sb(f"S_yv{i}", [128, TS], F32) for i in range(2)]
        yvb = [cx.buf(f"yv{i}") for i in range(2)]
        g1 = [sb.sb(f"S_g1{i}", [128, TS], F32) for i in range(2)]
        g1b_ = [cx.buf(f"g1{i}") for i in range(2)]
        g2 = [sb.sb(f"S_g2{i}", [128, TS], F32) for i in range(2)]
        g2b_ = [cx.buf(f"g2{i}") for i in range(2)]
        zo = [sb.sb(f"S_zo{i}", [128, TS], BF16) for i in range(2)]
        zob = [cx.buf(f"zo{i}") for i in range(2)]
        n = 0
        for ci in range(NCH):
            own = ci >= NCH // 2
            us = ci % 2
            cx.dma_k(uts[us], ssmT_r[:, ci * TS:(ci + 1) * TS], CT, [], [utb[us]], f"ut{us}")
            cx.add("act", lambda e, us=us: e.copy(out=utsb[us][:], in_=uts[us][:]), [utb[us]], [utbb[us]])
            for j in range(32):
                k, po = j // 4, 32 * (j % 4)
                s = n % NTMP
                n += 1
                pb = 0 + 2 * (j % 2)
                if po == 96:
                    lr_, li_, p0, p1 = BTr3, BTi3, 64, 128
                else:
                    lr_, li_, p0, p1 = BTr, BTi, po, po + 32
                cx.add("pe", lambda e, k=k, pb=pb, us=us, lr_=lr_, p0=p0, p1=p1: e.matmul(PS[pb][:, 0:TS], lhsT=lr_[p0:p1, k, :], rhs=utsb[us][p0:p1, k, :], start=True, stop=True),
                       [Bb, utbb[us]], [psb[pb]])
                cx.add("pe", lambda e, k=k, pb=pb, us=us, li_=li_, p0=p0, p1=p1: e.matmul(PS[pb + 1][:, 0:TS], lhsT=li_[p0:p1, k, :], rhs=utsb[us][p0:p1, k, :], start=True, stop=True),
                       [Bb, utbb[us]], [psb[pb + 1]])
                bre, bim = PS[pb][:, 0:TS], PS[pb + 1][:, 0:TS]
                cs, sn = cosT[:, j, :], sinT[:, j, :]
                T = {kk: tmp[kk][s][:] for kk in tmp}
                TB = {kk: tmpb[kk][s] for kk in tmp}
                cx.add("dve", lambda e, T=T, bre=bre, cs=cs: e.tensor_tensor(out=T["t1"], in0=bre, in1=cs, op=ALU.mult), [psb[pb], tb], [TB["t1"]])
                cx.add("dve", lambda e, T=T, bim=bim, sn=sn: e.tensor_tensor(out=T["t2"], in0=bim, in1=sn, op=ALU.mult), [psb[pb + 1], tb], [TB["t2"]])
                cx.add("pool", lambda e, T=T: e.tensor_tensor(out=T["wr"], in0=T["t1"], in1=T["t2"], op=ALU.add), [TB["t1"], TB["t2"]], [TB["wr"]])
                cx.add("dve", lambda e, T=T, bim=bim, cs=cs: e.tensor_tensor(out=T["xr"], in0=bim, in1=cs, op=ALU.mult), [psb[pb + 1], tb], [TB["xr"]])
                cx.add("dve", lambda e, T=T, bre=bre, sn=sn: e.tensor_tensor(out=T["xi"], in0=bre, in1=sn, op=ALU.mult), [psb[pb], tb], [TB["xi"]])
                cx.add("pool", lambda e, T=T: e.tensor_tensor(out=T["wi"], in0=T["xr"], in1=T["xi"], op=ALU.subtract), [TB["xr"], TB["xi"]], [TB["wi"]])
                rb = rA[:, j:j + 1].to_broadcast([128, TS])
                cx.add("dve", lambda e, T=T, rb=rb, j=j: e.tensor_tensor_scan(out=T["zr"], data0=rb, data1=T["wr"], initial=zin_r[:, j:j + 1], op0=ALU.mult, op1=ALU.add),
                       [TB["wr"], tb, zb], [TB["zr"]])
                cx.add("dve", lambda e, T=T, rb=rb, j=j: e.tensor_tensor_scan(out=T["zi"], data0=rb, data1=T["wi"], initial=zin_i[:, j:j + 1], op0=ALU.mult, op1=ALU.add),
                       [TB["wi"], tb, zb], [TB["zi"]])
                cx.add("pool", lambda e, T=T, j=j: e.tensor_copy(out=zl_r[:, j:j + 1], in_=T["zr"][:, TS - 1:TS]), [TB["zr"]], [zb])
                cx.add("pool", lambda e, T=T, j=j: e.tensor_copy(out=zl_i[:, j:j + 1], in_=T["zi"][:, TS - 1:TS]), [TB["zi"]], [zb])
                if own:
                    cx.add("dve", lambda e, T=T, cs=cs: e.tensor_tensor(out=T["t1"], in0=T["zr"], in1=cs, op=ALU.mult), [TB["zr"], tb], [TB["t1"]])
                    cx.add("dve", lambda e, T=T, sn=sn: e.tensor_tensor(out=T["t2"], in0=T["zi"], in1=sn, op=ALU.mult), [TB["zi"], tb], [TB["t2"]])
                    cx.add("pool", lambda e, T=T, s=s: e.tensor_tensor(out=xrb[s][:], in0=T["t1"], in1=T["t2"], op=ALU.subtract), [TB["t1"], TB["t2"]], [xrbb[s]])
                    cx.add("dve", lambda e, T=T, sn=sn: e.tensor_tensor(out=T["wr"], in0=T["zr"], in1=sn, op=ALU.mult), [TB["zr"], tb], [TB["wr"]])
                    cx.add("dve", lambda e, T=T, cs=cs: e.tensor_tensor(out=T["wi"], in0=T["zi"], in1=cs, op=ALU.mult), [TB["zi"], tb], [TB["wi"]])
                    cx.add("pool", lambda e, T=T, s=s: e.tensor_tensor(out=xib[s][:], in0=T["wr"], in1=T["wi"], op=ALU.add), [TB["wr"], TB["wi"]], [xibb[s]])
                    py = 4 + (k % 2)
                    cx.add("pe", lambda e, s=s, j=j, py=py: e.matmul(PS[py][:, 0:TS], lhsT=CTr[:, j * 128:(j + 1) * 128], rhs=xrb[s][:], start=(j % 4 == 0), stop=False),
                           [Cb, xrbb[s]], [psb[py]])
                    cx.add("pe", lambda e, s=s, j=j, py=py: e.matmul(PS[py][:, 0:TS], lhsT=CTi[:, j * 128:(j + 1) * 128], rhs=xib[s][:], start=False, stop=(j % 4 == 3)),
                           [Cb, xibb[s]], [psb[py]])
                    if j % 4 == 3:
                        ys = k % 2
                        cx.add("dve", lambda e, ys=ys, k=k, py=py, us=us: e.scalar_tensor_tensor(out=yv[ys][:], in0=uts[us][:, k, :], scalar=dsk_t[:, k:k + 1], in1=PS[py][:, 0:TS],
                                                                                               op0=ALU.mult, op1=ALU.add), [utb[us], Cb, psb[py]], [yvb[ys]])
                        gelu_tanh(yv[ys][:], yvb[ys], None, g1[ys][:], g1b_[ys], g2[ys][:], g2b_[ys], zo[ys][:], zob[ys])
                        t0 = (ci - NCH // 2) * TS
                        cx.dma(zT_w[k * 128:(k + 1) * 128, t0:t0 + TS], zo[ys][:], [zob[ys]], [], key=f"zo{ys}")
            cmul(zin_r[:], zin_i[:], zl_r[:], zl_i[:], ur[:], ui[:], zc1[:], zc2[:], [zb, tb], [zb])
        cx.flush()
        sb.release()

    def stage_M1():
        sb = SB(nc)
        psb = psbufs()
        wv = sb.sb("M_wv", [128, CT, D], BF16)
        wg = sb.sb("M_wg", [128, CT, D], BF16)
        wb = cx.buf("w")
        stg = [sb.sb(f"M_stg{i}", [128, D], F32) for i in range(2)]
        stgb = [cx.buf(f"stg{i}") for i in range(2)]
        ctr = [0]
        for c in range(CT):
            load_cast(stg, stgb, ctr, wval[:, c, :], wv[:, c, :], wb, D, "stg")
            load_cast(stg, stgb, ctr, wgate[:, c, :], wg[:, c, :], wb, D, "stg", cast_eng="act")
        zts = [sb.sb(f"M_zt{i}", [128, CT, 512], BF16) for i in range(2)]
        ztb = [cx.buf(f"zt{i}") for i in range(2)]
        sg = [sb.sb(f"M_sg{i}", [128, 512], F32) for i in range(2)]
        sgb = [cx.buf(f"sg{i}") for i in range(2)]
        g1t = [sb.sb(f"M_g1{i}", [128, 512], F32) for i in range(2)]
        g1tb = [cx.buf(f"g1{i}") for i in range(2)]
        mct = [sb.sb(f"M_mc{i}", [128, 512], F32) for i in range(2)]
        mctb = [cx.buf(f"mc{i}") for i in range(2)]
        mo = [sb.sb(f"M_mo{i}", [128, 512], BF16) for i in range(2)]
        mob = [cx.buf(f"mo{i}") for i in range(2)]
        for gi in range(NGO):
            zs = gi % 2
            cx.dma_k(zts[zs], zT_r[:, gi * 512:(gi + 1) * 512], CT, [], [ztb[zs]], f"zt{zs}")
            for dt in range(DK):
                s = dt % 2
                pv, pg = 0 + s, 2 + s
                for c in range(CT):
                    cx.add("pe", lambda e, c=c, dt=dt, pv=pv, zs=zs: e.matmul(PS[pv][:], lhsT=wv[:, c, dt * 128:(dt + 1) * 128], rhs=zts[zs][:, c, :], start=(c == 0), stop=(c == CT - 1)),
                           [wb, ztb[zs]], [psb[pv]])
                for c in range(CT):
                    cx.add("pe", lambda e, c=c, dt=dt, pg=pg, zs=zs: e.matmul(PS[pg][:], lhsT=wg[:, c, dt * 128:(dt + 1) * 128], rhs=zts[zs][:, c, :], start=(c == 0), stop=(c == CT - 1)),
                           [wb, ztb[zs]], [psb[pg]])
                cx.dma(g1t[s][:], gateT_r[D + dt * 128:D + (dt + 1) * 128, gi * 512:(gi + 1) * 512], [], [g1tb[s]], key=f"g1{s}")
                cx.dma(mct[s][:], mconvT_r[dt * 128:(dt + 1) * 128, gi * 512:(gi + 1) * 512], [], [mctb[s]], key=f"mc{s}")
                cx.add("act", lambda e, s=s, pg=pg: e.activation(out=sg[s][:], in_=PS[pg][:], func=AF.Sigmoid), [psb[pg]], [sgb[s]])
                cx.add("dve", lambda e, s=s, pv=pv: e.tensor_tensor(out=sg[s][:], in0=PS[pv][:], in1=sg[s][:], op=ALU.mult), [psb[pv], sgb[s]], [sgb[s]])
                cx.add("dve", lambda e, s=s: e.tensor_tensor(out=sg[s][:], in0=sg[s][:], in1=g1t[s][:], op=ALU.mult), [sgb[s], g1tb[s]], [sgb[s]])
                cx.add("dve", lambda e, s=s: e.tensor_tensor(out=mo[s][:], in0=sg[s][:], in1=mct[s][:], op=ALU.add), [sgb[s], mctb[s]], [mob[s]])
                cx.dma(mergedT_w[dt * 128:(dt + 1) * 128, gi * 512:(gi + 1) * 512], mo[s][:], [mob[s]], [], key=f"mo{s}", q="pool")
        cx.flush()
        sb.release()

    def stage_M2():
        sb = SB(nc)
        psb = psbufs()
        wo = sb.sb("O_wo", [128, DK, D], BF16)
        wb = cx.buf("w")
        stg = [sb.sb(f"O_stg{i}", [128, D], F32) for i in range(2)]
        stgb = [cx.buf(f"stg{i}") for i in range(2)]
        ctr = [0]
        for k in range(DK):
            load_cast(stg, stgb, ctr, wout[:, k, :], wo[:, k, :], wb, D, "stg", cast_eng=("pool" if k % 2 else "act"))
        gb_t = sb.sb("O_gb", [128, D], F32)
        gbb = cx.buf("gb")
        cx.dma(gb_t[:], g2b, [], [gbb], key="c0")
        mts = [sb.sb(f"O_mt{i}", [128, DK, 512], BF16) for i in range(2)]
        mtb = [cx.buf(f"mt{i}") for i in range(2)]
        xts = [sb.sb(f"O_xt{i}", [128, D], F32) for i in range(2)]
        xtb = [cx.buf(f"xt{i}") for i in range(2)]
        hn = sb.sb("O_hn", [128, D], BF16)
        hn_b = cx.buf("hn")
        junk = sb.sb("O_junk", [128, D], BF16)
        junk_b = cx.buf("junk")
        sts = sb.sb("O_st", [128, 2], F32)
        st_b = cx.buf("st")
        idb = cx.buf("identb")
        hts = [sb.sb(f"O_ht{i}", [128, DK, 128], BF16) for i in range(2)]
        htb = [cx.buf(f"ht{i}") for i in range(2)]
        n = 0
        for gi in range(NGO):
            ms = gi % 2
            cx.dma_k(mts[ms], mergedT_r[:, gi * 512:(gi + 1) * 512], DK, [], [mtb[ms]], f"mt{ms}")
            for ts_ in range(4):
                s = n % 2
                n += 1
                tok0 = gi * 512 + ts_ * 128
                cx.dma(xts[s][:], xin[NT + tok0:NT + tok0 + 128, :], [], [xtb[s]], key=f"xt{s}")
                for dc in range(4):
                    pi = 2 + dc
                    for k in range(DK):
                        cx.add("pe", lambda e, k=k, dc=dc, pi=pi, ms=ms, ts_=ts_: e.matmul(PS[pi][:], lhsT=mts[ms][:, k, ts_ * 128:(ts_ + 1) * 128], rhs=wo[:, k, dc * 512:(dc + 1) * 512],
                                                                                          start=(k == 0), stop=(k == DK - 1)), [mtb[ms], wb], [psb[pi]])
                    cx.add("dve", lambda e, s=s, dc=dc, pi=pi: e.tensor_tensor(out=xts[s][:, dc * 512:(dc + 1) * 512], in0=PS[pi][:], in1=xts[s][:, dc * 512:(dc + 1) * 512], op=ALU.add),
                           [psb[pi], xtb[s]], [xtb[s]])
                cx.dma(x1s_w[tok0:tok0 + 128, :], xts[s][:], [xtb[s]], [], key=f"x1{s}", q="pool")
                rmsnorm_T(sb, xts[s][:], xtb[s], gb_t, gbb, hn, hn_b, junk, junk_b, sts, st_b, ident_b, idb, psb, 0,
                          lambda k0, k1, s=s: hts[s][:, k0:k1, :], htb[s])
                cx.dma_k(hts[s], hn2T_w[:, tok0:tok0 + 128], DK, [htb[s]], [], f"ht{s}", store=True, q="pool")
        cx.flush()
        sb.release()

    def stage_Q0():
        sb = SB(nc)
        psb = psbufs()
        wq_bf = sb.sb("Q0_wq", [128, DK, D], BF16)
        wb = cx.buf("w")
        stg = [sb.sb(f"Q0_stg{i}", [128, D], F32) for i in range(2)]
        stgb = [cx.buf(f"stg{i}") for i in range(2)]
        ctr = [0]
        for k in range(DK):
            load_cast(stg, stgb, ctr, wq[:, k, :], wq_bf[:, k, :], wb, D, "stg", cast_eng=("pool" if k % 2 else "act"))
        hts = [sb.sb(f"Q0_ht{i}", [128, DK, 512], BF16) for i in range(2)]
        htb = [cx.buf(f"ht{i}") for i in range(2)]
        evs = [sb.sb(f"Q0_ev{i}", [128, 512], F32) for i in range(3)]
        evb = [cx.buf(f"ev{i}") for i in range(3)]
        n = 0
        for gi in range(NGO):
            hs = gi % 2
            cx.dma_k(hts[hs], hn2T_r[:, gi * 512:(gi + 1) * 512], DK, [], [htb[hs]], f"ht{hs}")
            for hc in range(16):
                pi = hc % 4
                ei = n % 3
                n += 1
                for k in range(DK):
                    cx.add("pe", lambda e, k=k, hc=hc, pi=pi, hs=hs: e.matmul(PS[pi][:], lhsT=wq_bf[:, k, hc * 128:(hc + 1) * 128], rhs=hts[hs][:, k, :], start=(k == 0), stop=(k == DK - 1)),
                           [wb, htb[hs]], [psb[pi]])
                cx.add("act", lambda e, ei=ei, pi=pi: e.copy(out=evs[ei][:], in_=PS[pi][:]), [psb[pi]], [evb[ei]])
                cx.dma(qTs_w[hc * 128:(hc + 1) * 128, gi * 512:(gi + 1) * 512], evs[ei][:], [evb[ei]], [], key=f"ev{ei}", q="pool")
        cx.flush()
        sb.release()

    def stage_Q():
        sb = SB(nc)
        psb = psbufs()
        kT_t = sb.sb("Q_kT", [128, 16, 128], F32)
        io_t = sb.sb("Q_iota", [128, 128], F32)
        cb_ = cx.buf("const")
        cx.dma(kT_t[:], kT, [], [cb_], key="c0")
        cx.dma(io_t[:], iota, [], [cb_], key="c0")
        qT = sb.sb("Q_qT", [128, 16, 512], F32)
        qTb1 = cx.buf("qT")
        qTb = [qTb1] * 16
        S = sb.sb("Q_S", [128, 16, 128], F32)
        Sb = cx.buf("S")
        Sw = sb.sb("Q_Sw", [128, 128], F32)
        Swb = cx.buf("Sw")
        v16 = sb.sb("Q_v16", [128, 16, 16], F32)
        i16u = sb.sb("Q_i16u", [128, 16, 16], U32)
        i16 = sb.sb("Q_i16", [128, 16, 16], F32)
        vb = cx.buf("v16")
        cand = sb.sb("Q_cand", [128, 8, 256], F32)
        candw = sb.sb("Q_candw", [128, 256], F32)
        candb = cx.buf("cand")
        candwb = cx.buf("candw")
        best = sb.sb("Q_best", [128, 8, 16], F32)
        posu = sb.sb("Q_posu", [128, 8, 16], U32)
        r1u = sb.sb("Q_r1u", [128, 8, 16], U32)
        r2u = sb.sb("Q_r2u", [128, 8, 16], U32)
        r1f = sb.sb("Q_r1f", [128, 8, 16], F32)
        r2f = sb.sb("Q_r2f", [128, 8, 16], F32)
        bb = cx.buf("best")
        eq = sb.sb("Q_eq", [128, 8, 16, 16], F32)
        eqb = cx.buf("eq")
        IJG = sb.sb("Q_IJG", [128, 3, 128], F32)
        ijgb = cx.buf("ijg")
        zs_ = sb.sb("Q_zs", [128, 8], F32)
        IJGT = sb.sb("Q_IJGT", [128, 3, 128], F32)
        ijgtb = cx.buf("ijgt")
        Aoh = [sb.sb(f"Q_A{i}", [128, 64, 128], BF16) for i in range(2)]
        Boh = [sb.sb(f"Q_B{i}", [128, 64, 128], BF16) for i in range(2)]
        Abs = [cx.buf(f"A{i}") for i in range(2)]
        Bbs = [cx.buf(f"B{i}") for i in range(2)]
        nh = 0
        nta = 0
        nJG = sb.sb("Q_nJG", [128, 2, 128], F32)
        njgb = cx.buf("njg")
        tmpA = [sb.sb(f"Q_tmpA{i}", [128, 128], F32) for i in range(4)]
        tmpAb = [cx.buf(f"tmpA{i}") for i in range(4)]
        Mst = [sb.sb(f"Q_Mst{i}", [128, 16, 128, 8], BF16) for i in range(1)]
        Mstb = [cx.buf(f"Mst{i}") for i in range(1)]
        n = 0
        for gi in range(NGO):
            cx.dma_k(qT, qTs_r[:, gi * 512:(gi + 1) * 512], 16, [], [qTb1], "qT")
            for ts_ in range(4):
                ms = 0
                n += 1
                for hc in range(16):
                    pi = hc // 4
                    cx.add("pe", lambda e, hc=hc, pi=pi, ts_=ts_: e.matmul(PS[pi][:, (hc % 4) * 128:(hc % 4 + 1) * 128], lhsT=qT[:, hc, ts_ * 128:(ts_ + 1) * 128], rhs=kT_t[:, hc, :],
                                                                            start=True, stop=True), [qTb[hc], cb_], [psb[pi]])
                for pi in range(4):
                    cx.add("act", lambda e, pi=pi: e.copy(out=S[:, pi * 4:(pi + 1) * 4, :], in_=PS[pi][:].rearrange("p (a n) -> p a n", a=4)), [psb[pi]], [Sb])
                for hc in range(16):
                    cx.add("dve", lambda e, hc=hc: e.max(out=v16[:, hc, 0:8], in_=S[:, hc, :]), [Sb], [vb])
                    cx.add("dve", lambda e, hc=hc: e.max_index(out=i16u[:, hc, 0:8], in_max=v16[:, hc, 0:8], in_values=S[:, hc, :]), [Sb, vb], [vb])
                    cx.add("dve", lambda e, hc=hc: e.match_replace(out=Sw[:], in_to_replace=v16[:, hc, 0:8], in_values=S[:, hc, :], imm_value=NEG), [Sb, vb], [Swb])
                    cx.add("dve", lambda e, hc=hc: e.max(out=v16[:, hc, 8:16], in_=Sw[:]), [Swb], [vb])
                    cx.add("dve", lambda e, hc=hc: e.max_index(out=i16u[:, hc, 8:16], in_max=v16[:, hc, 8:16], in_values=Sw[:]), [Swb, vb], [vb])
                cx.add("dve", lambda e: e.tensor_copy(out=i16[:], in_=i16u[:]), [vb], [vb])
                v4 = v16[:].rearrange("p (h c) k -> p h c k", c=2)
                i4 = i16[:].rearrange("p (h c) k -> p h c k", c=2)
                for h in range(8):
                    cx.add("dve", lambda e, h=h: e.tensor_tensor(out=cand[:, h, :].rearrange("p (a b) -> p a b", a=16),
                                                                  in0=v4[:, h, 0, :].unsqueeze(2).to_broadcast([128, 16, 16]),
                                                                  in1=v4[:, h, 1, :].unsqueeze(1).to_broadcast([128, 16, 16]), op=ALU.add), [vb], [candb])
                    cx.add("dve", lambda e, h=h: e.max(out=best[:, h, 0:8], in_=cand[:, h, :]), [candb], [bb])
                    cx.add("dve", lambda e, h=h: e.max_index(out=posu[:, h, 0:8], in_max=best[:, h, 0:8], in_values=cand[:, h, :]), [candb, bb], [bb])
                    cx.add("dve", lambda e, h=h: e.match_replace(out=candw[:], in_to_replace=best[:, h, 0:8], in_values=cand[:, h, :], imm_value=NEG), [candb, bb], [candwb])
                    cx.add("dve", lambda e, h=h: e.max(out=best[:, h, 8:16], in_=candw[:]), [candwb], [bb])
                    cx.add("dve", lambda e, h=h: e.max_index(out=posu[:, h, 8:16], in_max=best[:, h, 8:16], in_values=candw[:]), [candwb, bb], [bb])
                cx.add("dve", lambda e: e.tensor_single_scalar(out=r1u[:], in_=posu[:], scalar=4, op=ALU.logical_shift_right), [bb], [bb])
                cx.add("dve", lambda e: e.tensor_single_scalar(out=r2u[:], in_=posu[:], scalar=15, op=ALU.bitwise_and), [bb], [bb])
                cx.add("dve", lambda e: e.tensor_copy(out=r1f[:], in_=r1u[:]), [bb], [bb])
                cx.add("dve", lambda e: e.tensor_copy(out=r2f[:], in_=r2u[:]), [bb], [bb])
                io16 = io_t[:, 0:16].unsqueeze(1).unsqueeze(1).to_broadcast([128, 8, 16, 16])
                for (rf, cidx, slot) in ((r1f, 0, 0), (r2f, 1, 1)):
                    cx.add("dve", lambda e, rf=rf: e.tensor_tensor(out=eq[:], in0=rf[:].unsqueeze(3).to_broadcast([128, 8, 16, 16]), in1=io16, op=ALU.is_equal), [bb, cb_], [eqb])
                    cx.add("dve", lambda e, cidx=cidx: e.tensor_tensor(out=eq[:], in0=eq[:], in1=i4[:, :, cidx, :].unsqueeze(2).to_broadcast([128, 8, 16, 16]), op=ALU.mult), [eqb, vb], [eqb])
                    cx.add("dve", lambda e, slot=slot: e.tensor_reduce(out=IJG[:, slot, :].rearrange("p (h k) -> p h k", h=8), in_=eq[:], axis=AX.X, op=ALU.add), [eqb], [ijgb])
                cx.add("dve", lambda e: e.tensor_tensor(out=best[:], in0=best[:], in1=best[:, :, 0:1].to_broadcast([128, 8, 16]), op=ALU.subtract), [bb], [bb])
                cx.add("act", lambda e: e.activation(out=best[:], in_=best[:], func=AF.Exp), [bb], [bb])
                cx.add("dve", lambda e: e.tensor_reduce(out=zs_[:], in_=best[:], axis=AX.X, op=ALU.add), [bb], [bb])
                cx.add("dve", lambda e: e.reciprocal(out=zs_[:], in_=zs_[:]), [bb], [bb])
                cx.add("dve", lambda e: e.tensor_tensor(out=IJG[:, 2, :].rearrange("p (h k) -> p h k", h=8), in0=best[:], in1=zs_[:].unsqueeze(2).to_broadcast([128, 8, 16]), op=ALU.mult),
                       [bb], [ijgb])
                for a in range(3):
                    cx.add("pe", lambda e, a=a: e.transpose(out=PS[4][:, a * 128:(a + 1) * 128], in_=IJG[:, a, :], identity=ident_f[:]), [ijgb], [psb[4]])
                cx.add("act", lambda e: e.copy(out=IJGT[:], in_=PS[4][:, 0:384].rearrange("p (a t) -> p a t", a=3)), [psb[4]], [ijgtb])
                cx.add("dve", lambda e: e.tensor_scalar(out=nJG[:], in0=IJGT[:, 1:3, :], scalar1=-1.0, scalar2=None, op0=ALU.mult), [ijgtb], [njgb])
                for hf in range(2):
                    hb_ = nh % 2
                    nh += 1
                    A_, B_ = Aoh[hb_], Boh[hb_]
                    for tl in range(64):
                        t = hf * 64 + tl
                        cx.add("pool", lambda e, A_=A_, tl=tl, t=t: e.tensor_scalar(out=A_[:, tl, :], in0=io_t[:], scalar1=IJGT[:, 0, t:t + 1], scalar2=None, op0=ALU.is_equal),
                               [cb_, ijgtb], [Abs[hb_]], chain=True)
                    for tl in range(64):
                        t = hf * 64 + tl
                        ta = nta % 4
                        nta += 1
                        cx.add("act", lambda e, ta=ta, t=t: e.activation(out=tmpA[ta][:], in_=io_t[:], func=AF.Abs, bias=nJG[:, 0, t:t + 1]), [cb_, njgb], [tmpAb[ta]])
                        cx.add("act", lambda e, ta=ta, t=t, tl=tl, B_=B_: e.activation(out=B_[:, tl, :], in_=tmpA[ta][:], func=AF.Relu, scale=nJG[:, 1, t:t + 1], bias=IJGT[:, 2, t:t + 1]),
                               [tmpAb[ta], njgb, ijgtb], [Bbs[hb_]], chain=True)
                    for t16 in range(4):
                        pbase = 4 * (t16 % 2)
                        for tl in range(16):
                            tloc = t16 * 16 + tl
                            pi = pbase + tl // 4
                            cx.add("pe", lambda e, tloc=tloc, pi=pi, tl=tl, A_=A_, B_=B_: e.matmul(PS[pi][:, (tl % 4) * 128:(tl % 4 + 1) * 128], lhsT=A_[:, tloc, :], rhs=B_[:, tloc, :], start=True, stop=True),
                                   [Abs[hb_], Bbs[hb_]], [psb[pi]])
                        for q4 in range(4):
                            pi = pbase + q4
                            tt0 = hf * 64 + t16 * 16 + q4 * 4
                            cx.add("act", lambda e, pi=pi, tt0=tt0, ms=ms: e.copy(out=Mst[ms][:, :, tt0:tt0 + 4, :].rearrange("p c t j -> p t c j"),
                                                                                  in_=PS[pi][:].rearrange("p (t c j) -> p t c j", t=4, c=16)), [psb[pi]], [Mstb[ms]], chain=True)
                t0 = ts_ * 128
                for jc in range(16):
                    cx.dma(Mscr_w[gi, jc, :, t0 * 8:(t0 + 128) * 8], Mst[ms][:, jc, :, :].rearrange("p t j -> p (t j)"), [Mstb[ms]], [], key=f"Mst{ms}")
        cx.flush()
        sb.release()

    def stage_P():
        sb = SB(nc)
        psb = psbufs()
        gb_t = sb.sb("P_gb", [128, D], F32)
        gbb = cx.buf("gb")
        cx.dma(gb_t[:], gfb, [], [gbb], key="c0")
        hts = sb.sb("P_ht", [128, DK, 512], BF16)
        htb = cx.buf("ht")
        acc = sb.sb("P_acc", [128, 4, D], F32)
        accb = [cx.buf(f"acc{i}") for i in range(4)]
        Mc = [sb.sb(f"P_Mc{i}", [128, 512, 8], BF16) for i in range(2)]
        Mcb = [cx.buf(f"Mc{i}") for i in range(2)]
        AT = sb.sb("P_AT", [128, 8, 512], BF16)
        ATb = [cx.buf(f"AT{i}") for i in range(8)]
        vtb = sb.sb("P_vtb", [128, 8, D], BF16)
        vtbb = [cx.buf(f"vtb{i}") for i in range(8)]
        NU = 4
        utb = [sb.sb(f"P_utb{i}", [128, DK * 128], BF16) for i in range(NU)]
        utbb = [cx.buf(f"utb{i}") for i in range(NU)]
        t1 = [sb.sb(f"P_t1{i}", [128, 512], F32) for i in range(2)]
        t1b = [cx.buf(f"t1{i}") for i in range(2)]
        t2 = [sb.sb(f"P_t2{i}", [128, 512], F32) for i in range(2)]
        t2b = [cx.buf(f"t2{i}") for i in range(2)]
        x1t = sb.sb("P_x1t", [128, D], F32)
        x1b = cx.buf("x1t")
        junk = sb.sb("P_junk", [128, D], BF16)
        junk_b = cx.buf("junk")
        sts = sb.sb("P_st", [128, 2], F32)
        st_b = cx.buf("st")
        n = 0
        for tp in range(NGO):
            cx.dma_k(hts, hn2T_r[:, tp * 512:(tp + 1) * 512], DK, [], [htb], "ht")
            for jc in range(16):
                mcs = jc % 2
                cx.dma(Mc[mcs][:].rearrange("p t j -> p (t j)"), Mscr_r[tp, jc, :, :], [], [Mcb[mcs]], key=f"Mc{mcs}")
                for jj in range(8):
                    j = jc * 8 + jj
                    s = n % 2
                    su = n % NU
                    n += 1
                    cx.dma(utb[su][:], UTr[j], [], [utbb[su]], key=f"utb{su}", q="pool")
                    cx.dma(vtb[:, jj, :], Vr[j], [], [vtbb[jj]], key=f"vtb{jj}", q="pool")
                    pi = 4 + (n % 4)
                    for k in range(DK):
                        cx.add("pe", lambda e, k=k, su=su, pi=pi: e.matmul(PS[pi][:], lhsT=utb[su][:, k * 128:(k + 1) * 128], rhs=hts[:, k, :], start=(k == 0), stop=(k == DK - 1)),
                               [utbb[su], htb], [psb[pi]])
                    gelu_tanh(PS[pi][:], psb[pi], None, t1[s][:], t1b[s], t2[s][:], t2b[s], AT[:, jj, :], ATb[jj], extra_mul=Mc[mcs][:, :, jj], extra_b=Mcb[mcs])
                for ts_ in range(4):
                    for dc in range(4):
                        pi = dc
                        for jj in range(8):
                            cx.add("pe", lambda e, jj=jj, ts_=ts_, dc=dc, pi=pi: e.matmul(PS[pi][:], lhsT=AT[:, jj, ts_ * 128:(ts_ + 1) * 128], rhs=vtb[:, jj, dc * 512:(dc + 1) * 512],
                                                                                          start=(jj == 0), stop=(jj == 7)), [ATb[jj], vtbb[jj]], [psb[pi]])
                        if jc == 0:
                            cx.add("act", lambda e, ts_=ts_, dc=dc, pi=pi: e.copy(out=acc[:, ts_, dc * 512:(dc + 1) * 512], in_=PS[pi][:]), [psb[pi]], [accb[ts_]])
                        else:
                            cx.add("dve", lambda e, ts_=ts_, dc=dc, pi=pi: e.tensor_tensor(out=acc[:, ts_, dc * 512:(dc + 1) * 512], in0=PS[pi][:], in1=acc[:, ts_, dc * 512:(dc + 1) * 512], op=ALU.add),
                                   [psb[pi], accb[ts_]], [accb[ts_]])
            for ts_ in range(4):
                tok0 = tp * 512 + ts_ * 128
                cx.dma(x1t[:], x1s_r[tok0:tok0 + 128, :], [], [x1b], key="x1t")
                cx.add("dve", lambda e, ts_=ts_: e.tensor_tensor(out=x1t[:], in0=x1t[:], in1=acc[:, ts_, :], op=ALU.add), [x1b, accb[ts_]], [x1b])
                ss, rs = sts[:, 0:1], sts[:, 1:2]
                cx.add("act", lambda e: e.activation(out=junk[:], in_=x1t[:], func=AF.Square, accum_out=ss), [x1b], [junk_b, st_b])
                cx.add("act", lambda e: e.activation(out=rs, in_=ss, func=AF.Sqrt, scale=1.0 / D, bias=eps_rms[:, 0:1]), [st_b], [st_b])
                cx.add("dve", lambda e: e.reciprocal(out=rs, in_=rs), [st_b], [st_b])
                cx.add("dve", lambda e, ts_=ts_: e.scalar_tensor_tensor(out=acc[:, ts_, :], in0=x1t[:], scalar=rs, in1=gb_t[:], op0=ALU.mult, op1=ALU.mult),
                       [x1b, st_b, gbb], [accb[ts_]])
                cx.dma(out[tok0:tok0 + 128, :], acc[:, ts_, :], [accb[ts_]], [], key=f"out{ts_ % 2}")
        cx.flush()
        sb.release()

    stages = {"A": stage_A, "C": stage_C, "S": stage_S, "M1": stage_M1, "M2": stage_M2, "Q0": stage_Q0, "Q": stage_Q, "P": stage_P}
    return nc, cx, stages


def _c(a):
    return np.ascontiguousarray(a, dtype=np.float32)


def prep_shared(p):
    s = {}
    tile128 = lambda v: _c(np.broadcast_to(v[None, :], (128, v.shape[0])))
    s["g1b"] = tile128(p["norm_mix"])
    s["g2b"] = tile128(p["norm_ffn"])
    s["gfb"] = tile128(p["norm_final"])
    s["w_in_r"] = _c(p["w_in"].reshape(16, 128, 56, 128).transpose(2, 1, 0, 3).reshape(56, 128, 2048))
    s["bgate"] = _c(p["b_gate"].reshape(32, 128).T)
    s["cw"] = _c(p["conv_w_dw"].reshape(KCONV, CT, 128).transpose(2, 1, 0))
    s["cb"] = _c(p["conv_b_dw"].reshape(CT, 128).T)
    s["lng"] = _c(p["conv_ln_g"].reshape(CT, 128).T)
    s["lnb"] = _c(p["conv_ln_b"].reshape(CT, 128).T)
    s["cwo"] = _c(p["conv_w_out"].reshape(CT, 128, D).transpose(1, 0, 2))
    s["wval"] = _c(p["ssm_w_val"].reshape(CT, 128, D).transpose(1, 0, 2))
    s["wgate"] = _c(p["ssm_w_gate"].reshape(CT, 128, D).transpose(1, 0, 2))
    s["wout"] = _c(p["w_out"].reshape(DK, 128, D).transpose(1, 0, 2))
    s["wq"] = _c(p["peer_w_q"].reshape(DK, 128, D).transpose(1, 0, 2))
    s["kT"] = _c(p["peer_sub_keys"].reshape(16, 128, 128).transpose(2, 0, 1))
    s["UTr"] = _c(p["peer_u"].reshape(128, 128, 16, 128).transpose(1, 3, 2, 0).reshape(128, 128, 2048))
    s["Vr"] = _c(p["peer_v"].reshape(128, 128, D).transpose(1, 0, 2))
    def st_layout(a):
        return a.reshape(32, 2, 64).transpose(1, 2, 0).reshape(128, 32)
    ldt = np.broadcast_to(p["ssm_log_dt"][:, None], (64, 64))
    s["sA"] = _c(np.stack([st_layout(p["ssm_a_re"]), st_layout(p["ssm_a_im"]), st_layout(ldt)], axis=1))
    def b_layout_rep(a):
        x = a.reshape(8, 8, 1, 64)
        x = np.broadcast_to(x, (8, 8, 16, 64))
        return x.transpose(1, 2, 0, 3).reshape(128, 8 * 64)
    s["sB"] = _c(np.stack([b_layout_rep(p["ssm_a_re"]), b_layout_rep(p["ssm_a_im"]), b_layout_rep(ldt)], axis=1))
    def bT_layout(b):
        return b.reshape(8, 8, 64, 16).transpose(1, 3, 0, 2).reshape(128, 8 * 64)
    s["bT"] = _c(np.stack([bT_layout(p["ssm_b_re"]), bT_layout(p["ssm_b_im"])], axis=1))
    def cT_layout(c):
        o = np.zeros((2, 64, 32, 128), np.float32)
        cc = c.reshape(32, 2, 16, 64)
        for j in range(32):
            for g2 in range(2):
                col0 = 32 * (j % 4) + 16 * g2
                o[g2, :, j, col0:col0 + 16] = cc[j, g2].T
        return o.reshape(128, 32 * 128)
    s["cTp"] = _c(np.stack([cT_layout(p["ssm_c_re"]), cT_layout(p["ssm_c_im"])], axis=1))
    s["dsk"] = _c(p["ssm_d"].reshape(CT, 128).T)
    mB = np.zeros((128, 128), np.float32)
    for q in range(128):
        gl = q // 16
        mB[q, (gl % 2) * 64:(gl % 2) * 64 + 64] = 1.0
    s["maskB"] = mB
    rm = np.zeros((128, 1), np.float32)
    rm[96:] = 1.0
    s["rowm"] = rm
    s["ident"] = np.eye(128, dtype=np.float32)
    s["onesc"] = np.full((128, 128), 1.0 / CW, np.float32)
    s["iota"] = _c(np.broadcast_to(np.arange(128, dtype=np.float32)[None, :], (128, 128)))
    return s


_CACHE = {}


def kernel(**inputs):
    x = np.asarray(inputs["x"], dtype=np.float32)
    B, S, _ = x.shape
    NT = S // 2
    p = {}
    for k, v in inputs.items():
        if k == "x":
            continue
        v = np.asarray(v, dtype=np.float32)
        p[k] = v if k == "norm_final" else v[0]
    shared = prep_shared(p)
    if NT not in _CACHE:
        nc, cx, stages = build(NT)
        for name in ("A", "C", "S", "M1", "M2", "Q0", "Q", "P"):
            stages[name]()
        _CACHE[NT] = nc
    nc = _CACHE[NT]
    in_maps = []
    for c in range(NCORES):
        b, half = c // 2, c % 2
        own = x[b, half * NT:(half + 1) * NT]
        prev = x[b, 0:NT] if half == 1 else np.zeros_like(own)
        m = dict(shared)
        m["xin"] = _c(np.concatenate([prev, own], axis=0))
        in_maps.append(m)
    res = run_bass_kernel_spmd(nc, in_maps, core_ids=list(range(NCORES)))
    outp = np.empty((B, S, D), np.float32)
    for c in range(NCORES):
        b, half = c // 2, c % 2
        outp[b, half * NT:(half + 1) * NT] = res.results[c]["out"]
    return outp
```

```python
import math
import numpy as np
import concourse.bass as bass
import concourse.mybir as mybir
from concourse.bass_utils import run_bass_kernel_spmd

F32 = mybir.dt.float32
BF16 = mybir.dt.bfloat16
U32 = mybir.dt.uint32
AF = mybir.ActivationFunctionType
ALU = mybir.AluOpType
AX = mybir.AxisListType

D = 2048
DK = 16
CW = 1024
CT = 8
KCONV = 31
NSEQ = 4096
NCORES = 8
RMS_EPS = 1e-6
LN_EPS = 1e-5
NEG = -1.0e30


class Buf:
    __slots__ = ("name", "w", "r")

    def __init__(self, name=""):
        self.name = name
        self.w = None
        self.r = []


class Op:
    __slots__ = ("eng", "fn", "deps", "signal", "tok", "dma_key")

    def __init__(self, eng, fn, dma_key):
        self.eng = eng
        self.fn = fn
        self.deps = []
        self.signal = False
        self.tok = None
        self.dma_key = dma_key


class Ctx:
    ENGS = ("pe", "dve", "act", "pool", "sp")

    def __init__(self, nc):
        self.nc = nc
        self.e = {"pe": nc.tensor, "dve": nc.vector, "act": nc.scalar, "pool": nc.gpsimd, "sp": nc.sync}
        self.ops = []
        self.gen = 0
        self.sem = {k: nc.semaphore("sem_" + k).__enter__() for k in self.ENGS if k != "sp"}
        self.cnt = {k: 0 for k in self.ENGS}
        self.seen = {k: {} for k in self.ENGS}
        self.dsem = {}
        self.dcnt = {}
        self.bufs = []
        self.nops = 0

    def buf(self, name=""):
        b = Buf(name)
        self.bufs.append(b)
        return b

    def add(self, eng, fn, reads=(), writes=(), dma_key=None, chain=False):
        op = Op(eng, fn, dma_key)
        deps = set()
        for b in reads:
            if b.w is not None:
                deps.add(b.w)
        for b in writes:
            if b.w is not None:
                if not (chain and b.w.eng == eng and b.w.dma_key is None and dma_key is None):
                    deps.add(b.w)
            for o in b.r:
                deps.add(o)
        deps.discard(op)
        op.deps = list(deps)
        for d in op.deps:
            d.signal = True
        for b in writes:
            b.w = op
            b.r = []
        for b in reads:
            b.r.append(op)
        self.ops.append(op)
        return op

    def dma(self, out, in_, reads=(), writes=(), key="d", q="sp"):
        return self.add(q, lambda e: e.dma_start(out=out, in_=in_), reads, writes, dma_key=key)

    def dma_k(self, sb_t, dram2d, nk, reads, writes, key, store=False, q="sp"):
        for k in range(nk):
            if store:
                self.dma(dram2d[k * 128:(k + 1) * 128, :], sb_t[:, k, :], reads, writes, key=key, q=q)
            else:
                self.dma(sb_t[:, k, :], dram2d[k * 128:(k + 1) * 128, :], reads, writes, key=key)

    def _wait(self, eng, semkey, sem, val):
        s = self.seen[eng]
        if s.get(semkey, 0) >= val:
            return
        s[semkey] = val
        self.e[eng].wait_ge(sem, val)

    def flush(self):
        last = {}
        for op in self.ops:
            if op.dma_key is None:
                last[op.eng] = op
        for op in last.values():
            op.signal = True
        for op in self.ops:
            eng = op.eng
            for d in op.deps:
                if d.dma_key is not None:
                    k = d.dma_key
                    self._wait(eng, "D" + k, self.dsem[k], self.dcnt[k])
                else:
                    if d.eng == "pe" and eng == "pe" and op.dma_key is None:
                        continue
                    self._wait(eng, d.eng, self.sem[d.eng], d.tok)
            ins = op.fn(self.e[eng])
            self.nops += 1
            if op.dma_key is not None:
                k = op.dma_key
                if k not in self.dsem:
                    self.dsem[k] = self.nc.semaphore("dsem_" + k).__enter__()
                    self.dcnt[k] = 0
                self.dcnt[k] += 16
                ins.then_inc(self.dsem[k], 16)
                op.tok = self.dcnt[k]
            elif op.signal:
                self.cnt[eng] += 1
                ins.then_inc(self.sem[eng], 1)
                op.tok = self.cnt[eng]
        for eng in self.ENGS:
            for k in self.dsem:
                if self.dcnt[k] > 0:
                    self._wait(eng, "D" + k, self.dsem[k], self.dcnt[k])
            for o in self.ENGS:
                if o != eng and self.cnt[o] > 0:
                    self._wait(eng, o, self.sem[o], self.cnt[o])
        self.ops = []
        for b in self.bufs:
            b.w = None
            b.r = []
        self.gen += 1
        self.sem = {k: self.nc.semaphore(f"sem_{k}_{self.gen}").__enter__() for k in self.ENGS if k != "sp"}
        self.cnt = {k: 0 for k in self.ENGS}
        for e in self.ENGS:
            for k in self.ENGS:
                self.seen[e].pop(k, None)


class SB:
    def __init__(self, nc):
        self.nc = nc
        self.guards = []

    def sb(self, name, shape, dt):
        g = self.nc.sbuf_tensor(name, list(shape), dt)
        t = g.__enter__()
        self.guards.append(g)
        return t

    def ps(self, name, shape, dt):
        g = self.nc.psum_tensor(name, list(shape), dt)
        t = g.__enter__()
        self.guards.append(g)
        return t

    def release(self):
        for g in reversed(self.guards):
            g.__exit__(None, None, None)
        self.guards = []


def build(NT, debug=False, iso=False, big=True):
    NALL = 2 * NT
    NG = NALL // 512
    NGO = NT // 512
    nc = bass.Bass("TRN2", target_bir_lowering=False)
    cx = Ctx(nc)

    def din(name, shape, dt=F32):
        return nc.dram_tensor(name, list(shape), dt, kind="ExternalInput").ap()

    def dscr(name, shape, dt=F32):
        if iso:
            w = nc.dram_tensor(name, list(shape), dt, kind="ExternalOutput").ap()
            r = nc.dram_tensor(name + "_in", list(shape), dt, kind="ExternalInput").ap()
            return r, w
        kind = "ExternalOutput" if debug else "Internal"
        a = nc.dram_tensor(name, list(shape), dt, kind=kind).ap()
        return a, a

    xin = din("xin", [NALL, D])
    g1b = din("g1b", [128, D])
    g2b = din("g2b", [128, D])
    gfb = din("gfb", [128, D])
    w_in_r = din("w_in_r", [56, 128, DK * 128])
    bgate = din("bgate", [128, 32])
    cw = din("cw", [128, CT, KCONV])
    cb = din("cb", [128, CT])
    lng = din("lng", [128, CT])
    lnb = din("lnb", [128, CT])
    cwo = din("cwo", [128, CT, D])
    wval = din("wval", [128, CT, D])
    wgate = din("wgate", [128, CT, D])
    wout = din("wout", [128, DK, D])
    wq = din("wq", [128, DK, D])
    kT = din("kT", [128, 16, 128])
    UTr = din("UTr", [128, 128, DK * 128] if big else [1, 1, 1])
    Vr = din("Vr", [128, 128, D] if big else [1, 1, 1])
    sA = din("sA", [128, 3, 32])
    sB = din("sB", [128, 3, CT * 64])
    bT = din("bT", [128, 2, CT * 64])
    cTp = din("cTp", [128, 2, 32 * 128])
    dsk = din("dsk", [128, CT])
    maskB = din("maskB", [128, 128])
    rowm = din("rowm", [128, 1])
    ident = din("ident", [128, 128])
    onesc = din("onesc", [128, 128])
    iota = din("iota", [128, 128])

    out = nc.dram_tensor("out", [NT, D], F32, kind="ExternalOutput").ap()

    gluT_r, gluT_w = dscr("gluT", [CW, NT + 512])
    ssmT_r, ssmT_w = dscr("ssmT", [CW, NALL])
    gateT_r, gateT_w = dscr("gateT", [2 * D, NT])
    mconvT_r, mconvT_w = dscr("mconvT", [D, NT])
    zT_r, zT_w = dscr("zT", [CW, NT], BF16)
    mergedT_r, mergedT_w = dscr("mergedT", [D, NT], BF16)
    x1s_r, x1s_w = dscr("x1s", [NT, D])
    hn2T_r, hn2T_w = dscr("hn2T", [D, NT], BF16)
    Mscr_r, Mscr_w = dscr("Mscr", [NGO, 16, 128, 512 * 8], BF16)
    qTs_r, qTs_w = dscr("qTs", [D, NT])

    PS = [nc.psum_tensor(f"ps{i}", [128, 512], F32).__enter__() for i in range(8)]
    PSB = [cx.buf(f"ps{i}") for i in range(8)]

    def psbufs():
        return [cx.buf(f"ps{i}") for i in range(8)]

    def rmsnorm_T(sb, xt_ap, xb, gb_t, gb_b, hn, hn_b, junk, junk_b, st, st_b, identb, identb_b, psb, ps_i, dst_fn, dst_b):
        ss, rs = st[:, 0:1], st[:, 1:2]
        cx.add("act", lambda e: e.activation(out=junk[:], in_=xt_ap, func=AF.Square, accum_out=ss), [xb], [junk_b, st_b])
        cx.add("act", lambda e: e.activation(out=rs, in_=ss, func=AF.Sqrt, scale=1.0 / D, bias=eps_rms[:, 0:1]), [st_b], [st_b])
        cx.add("dve", lambda e: e.reciprocal(out=rs, in_=rs), [st_b], [st_b])
        cx.add("dve", lambda e: e.scalar_tensor_tensor(out=hn[:], in0=xt_ap, scalar=rs, in1=gb_t[:], op0=ALU.mult, op1=ALU.mult),
               [xb, st_b, gb_b], [hn_b])
        for half in range(2):
            pst = PS[ps_i + half].bitcast(BF16)
            for kk in range(8):
                k = half * 8 + kk
                cx.add("pe", lambda e, k=k, kk=kk, pst=pst: e.transpose(out=pst[:, kk * 128:(kk + 1) * 128], in_=hn[:, k * 128:(k + 1) * 128], identity=identb[:]),
                       [hn_b, identb_b], [psb[ps_i + half]])
            dst = dst_fn(half * 8, half * 8 + 8)
            cx.add("act", lambda e, pst=pst, dst=dst: e.copy(out=dst, in_=pst[:].rearrange("p (k t) -> p k t", k=8)),
                   [psb[ps_i + half]], [dst_b])

    def gelu_tanh(src_ap, src_b, shape, tmp1, tmp1_b, tmp2, tmp2_b, out_ap, out_b, extra_mul=None, extra_b=None):
        cx.add("act", lambda e: e.activation(out=tmp1, in_=src_ap, func=AF.Square), [src_b], [tmp1_b])
        cx.add("dve", lambda e: e.tensor_scalar(out=tmp1, in0=tmp1, scalar1=0.044715, scalar2=1.0, op0=ALU.mult, op1=ALU.add), [tmp1_b], [tmp1_b])
        cx.add("dve", lambda e: e.tensor_tensor(out=tmp1, in0=tmp1, in1=src_ap, op=ALU.mult), [tmp1_b, src_b], [tmp1_b])
        cx.add("act", lambda e: e.activation(out=tmp2, in_=tmp1, func=AF.Sigmoid, scale=1.5957691216057308), [tmp1_b], [tmp2_b])
        if extra_mul is None:
            cx.add("dve", lambda e: e.tensor_tensor(out=out_ap, in0=tmp2, in1=src_ap, op=ALU.mult), [tmp2_b, src_b], [out_b])
        else:
            cx.add("dve", lambda e: e.tensor_tensor(out=tmp2, in0=tmp2, in1=src_ap, op=ALU.mult), [tmp2_b, src_b], [tmp2_b])
            cx.add("dve", lambda e: e.tensor_tensor(out=out_ap, in0=tmp2, in1=extra_mul, op=ALU.mult), [tmp2_b, extra_b], [out_b])

    def load_cast(sb_stage, stage_bufs, counter, dram_ap, dst_ap, dst_b, n, key, cast_eng="pool"):
        s = counter[0] % 2
        counter[0] += 1
        cx.dma(dst_ap, dram_ap, [], [dst_b], key=f"{key}{s}", q="pool")

    eps_rms = nc.sbuf_tensor("eps_rms", [128, 1], F32).__enter__()
    eps_ln = nc.sbuf_tensor("eps_ln", [128, 1], F32).__enter__()
    ident_f = nc.sbuf_tensor("ident_f", [128, 128], F32).__enter__()
    ident_b = nc.sbuf_tensor("ident_b", [128, 128], BF16).__enter__()
    cb0 = cx.buf("const")
    cx.add("dve", lambda e: e.memset(eps_rms[:], RMS_EPS), [], [cb0])
    cx.add("dve", lambda e: e.memset(eps_ln[:], LN_EPS), [], [cb0])
    cx.dma(ident_f[:], ident, [], [cb0], key="c0")
    cx.add("dve", lambda e: e.tensor_copy(out=ident_b[:], in_=ident_f[:]), [cb0], [cb0])
    cx.flush()

    def stage_A():
        sb = SB(nc)
        psb = psbufs()
        constb = cx.buf("constA")
        hnT = sb.sb("A_hnT", [128, DK, NALL], BF16)
        hnTb = [cx.buf(f"hnT{g}") for g in range(NG)]
        gb_t = sb.sb("A_gb", [128, D], F32)
        gbb = cx.buf("gb")
        cx.dma(gb_t[:], g1b, [], [gbb], key="c0")
        bg_t = sb.sb("A_bg", [128, 32], F32)
        cx.dma(bg_t[:], bgate, [], [constb], key="c0")
        xts = [sb.sb(f"A_xt{i}", [128, D], F32) for i in range(2)]
        xtb = [cx.buf(f"xt{i}") for i in range(2)]
        hn = sb.sb("A_hn", [128, D], BF16)
        hn_b = cx.buf("hn")
        junk = sb.sb("A_junk", [128, D], BF16)
        junk_b = cx.buf("junk")
        sts = sb.sb("A_st", [128, 2], F32)
        st_b = cx.buf("st")
        idb = cx.buf("identb")
        for tt in range(NALL // 128):
            s = tt % 2
            cx.dma(xts[s][:], xin[tt * 128:(tt + 1) * 128, :], [], [xtb[s]], key=f"xt{s}")
            g = tt // 4
            rmsnorm_T(sb, xts[s][:], xtb[s], gb_t, gbb, hn, hn_b, junk, junk_b, sts, st_b, ident_b, idb, psb, 0,
                      lambda k0, k1, tt=tt: hnT[:, k0:k1, tt * 128:(tt + 1) * 128], hnTb[g])
        NW = 3
        wbf = [sb.sb(f"A_wbf{i}", [128, DK * 128], BF16) for i in range(NW)]
        wbfb = [cx.buf(f"wbf{i}") for i in range(NW)]
        ngc = NGO + 1
        abuf = sb.sb("A_abuf", [128, ngc, 512], F32)
        abufb = [cx.buf(f"abuf{i}") for i in range(ngc)]
        evs = [sb.sb(f"A_ev{i}", [128, 512], F32) for i in range(3)]
        evb = [cx.buf(f"ev{i}") for i in range(3)]
        sgs = [sb.sb(f"A_sg{i}", [128, 512], F32) for i in range(2)]
        sgb = [cx.buf(f"sg{i}") for i in range(2)]
        cnt = {"w": 0, "ps": 0, "ev": 0, "sg": 0}
        conv_groups = list(range(NG // 2 - 1, NG))
        own_groups = list(range(NG // 2, NG))
        order = []
        for c in range(8):
            order.append((c, "a", conv_groups))
            order.append((c + 8, "g", conv_groups))
        for c in range(16, 24):
            order.append((c, "s", list(range(NG))))
        for c in range(24, 56):
            order.append((c, "t", own_groups))
        for (ct, kind, groups) in order:
            ws = cnt["w"] % NW
            cnt["w"] += 1
            cx.dma(wbf[ws][:], w_in_r[ct], [], [wbfb[ws]], key=f"wbf{ws}", q="pool")
            for gi, g in enumerate(groups):
                pi = 2 + cnt["ps"] % 4
                cnt["ps"] += 1
                for k in range(DK):
                    cx.add("pe", lambda e, ws=ws, k=k, g=g, pi=pi: e.matmul(PS[pi][:], lhsT=wbf[ws][:, k * 128:(k + 1) * 128], rhs=hnT[:, k, g * 512:(g + 1) * 512],
                                                                            start=(k == 0), stop=(k == DK - 1)),
                           [wbfb[ws], hnTb[g]], [psb[pi]])
                if kind == "a":
                    cx.add("act", lambda e, gi=gi, pi=pi: e.copy(out=abuf[:, gi, :], in_=PS[pi][:]), [psb[pi]], [abufb[gi]])
                elif kind == "g":
                    c = ct - 8
                    si = cnt["sg"] % 2
                    cnt["sg"] += 1
                    ei = cnt["ev"] % 3
                    cnt["ev"] += 1
                    cx.add("act", lambda e, si=si, pi=pi: e.activation(out=sgs[si][:], in_=PS[pi][:], func=AF.Sigmoid), [psb[pi]], [sgb[si]])
                    cx.add("dve", lambda e, si=si, ei=ei, gi=gi: e.tensor_tensor(out=evs[ei][:], in0=abuf[:, gi, :], in1=sgs[si][:], op=ALU.mult),
                           [abufb[gi], sgb[si]], [evb[ei]])
                    cx.dma(gluT_w[c * 128:(c + 1) * 128, gi * 512:(gi + 1) * 512], evs[ei][:], [evb[ei]], [], key=f"ev{ei}", q="pool")
                elif kind == "s":
                    c = ct - 16
                    ei = cnt["ev"] % 3
                    cnt["ev"] += 1
                    cx.add("act", lambda e, ei=ei, pi=pi: e.copy(out=evs[ei][:], in_=PS[pi][:]), [psb[pi]], [evb[ei]])
                    cx.dma(ssmT_w[c * 128:(c + 1) * 128, g * 512:(g + 1) * 512], evs[ei][:], [evb[ei]], [], key=f"ev{ei}", q="pool")
                else:
                    c = ct - 24
                    ei = cnt["ev"] % 3
                    cnt["ev"] += 1
                    cx.add("act", lambda e, ei=ei, pi=pi, c=c: e.activation(out=evs[ei][:], in_=PS[pi][:], func=AF.Sigmoid, bias=bg_t[:, c:c + 1]),
                           [psb[pi], constb], [evb[ei]])
                    go = g - NG // 2
                    cx.dma(gateT_w[c * 128:(c + 1) * 128, go * 512:(go + 1) * 512], evs[ei][:], [evb[ei]], [], key=f"ev{ei}", q="pool")
        cx.flush()
        sb.release()

    def stage_C():
        sb = SB(nc)
        psb = psbufs()
        cb_ = cx.buf("constC")
        cw_t = sb.sb("C_cw", [128, CT, KCONV], F32)
        cb_t = sb.sb("C_cb", [128, CT], F32)
        lng_t = sb.sb("C_lng", [128, CT], F32)
        lnb_t = sb.sb("C_lnb", [128, CT], F32)
        ones_t = sb.sb("C_ones", [128, 128], F32)
        for t, d in ((cw_t, cw), (cb_t, cb), (lng_t, lng), (lnb_t, lnb), (ones_t, onesc)):
            cx.dma(t[:], d, [], [cb_], key="c0")
        cwo_bf = sb.sb("C_cwo", [128, CT, D], BF16)
        cwob = cx.buf("cwo")
        stg = [sb.sb(f"C_stg{i}", [128, D], F32) for i in range(2)]
        stgb = [cx.buf(f"stg{i}") for i in range(2)]
        ctr = [0]
        for c in range(CT):
            load_cast(stg, stgb, ctr, cwo[:, c, :], cwo_bf[:, c, :], cwob, D, "stg")
        gts = [sb.sb(f"C_gt{i}", [128, 512 + 32], F32) for i in range(2)]
        gtb = [cx.buf(f"gt{i}") for i in range(2)]
        gth = [sb.sb(f"C_gth{i}", [128, 512 + 32], BF16) for i in range(2)]
        gthb = [cx.buf(f"gth{i}") for i in range(2)]
        dg = sb.sb("C_dg", [128, CT, KCONV, 128], BF16)
        dgb = cx.buf("dg")
        idf = ident_f[:].unsqueeze(1).to_broadcast([128, KCONV, 128])
        for c in range(CT):
            cx.add("dve", lambda e, c=c: e.tensor_tensor(out=dg[:, c, :, :], in0=idf, in1=cw_t[:, c, :].unsqueeze(2).to_broadcast([128, KCONV, 128]), op=ALU.mult),
                   [cb_], [dgb], chain=True)
        y = sb.sb("C_y", [128, CT, 512], F32)
        yb = [cx.buf(f"y{c}") for c in range(CT)]
        ysq = sb.sb("C_ysq", [128, CT, 512], F32)
        ysqb = [cx.buf(f"ysq{c}") for c in range(CT)]
        mean_t = sb.sb("C_mean", [128, 512], F32)
        rstd_t = sb.sb("C_rstd", [128, 512], F32)
        stb = cx.buf("stats")
        zt = [sb.sb(f"C_z{i}", [128, 512], F32) for i in range(2)]
        ztb = [cx.buf(f"z{i}") for i in range(2)]
        actT = sb.sb("C_act", [128, CT, 512], BF16)
        actb = [cx.buf(f"act{c}") for c in range(CT)]
        g0t = [sb.sb(f"C_g0{i}", [128, 512], F32) for i in range(2)]
        g0b = [cx.buf(f"g0{i}") for i in range(2)]
        mct = [sb.sb(f"C_mc{i}", [128, 512], F32) for i in range(2)]
        mcb = [cx.buf(f"mc{i}") for i in range(2)]
        n = 0
        for gi in range(NGO):
            for c in range(CT):
                s = n % 2
                n += 1
                base = 512 + gi * 512 - 32
                cx.dma(gts[s][:], gluT_r[c * 128:(c + 1) * 128, base:base + 544], [], [gtb[s]], key=f"gt{s}")
                cx.add("act", lambda e, s=s: e.copy(out=gth[s][:], in_=gts[s][:]), [gtb[s]], [gthb[s]])
                pc = 4 + (n % 4)
                for k in range(KCONV):
                    cx.add("pe", lambda e, s=s, c=c, k=k, pc=pc: e.matmul(PS[pc][:], lhsT=dg[:, c, k, :], rhs=gth[s][:, 2 + k:514 + k], start=(k == 0), stop=(k == KCONV - 1)),
                           [dgb, gthb[s]], [psb[pc]])
                cx.add("act", lambda e, c=c, pc=pc: e.activation(out=y[:, c, :], in_=PS[pc][:], func=AF.Identity, bias=cb_t[:, c:c + 1]), [psb[pc], cb_], [yb[c]])
                cx.add("act", lambda e, c=c: e.activation(out=ysq[:, c, :], in_=y[:, c, :], func=AF.Square), [yb[c]], [ysqb[c]])
            for c in range(CT):
                cx.add("pe", lambda e, c=c: e.matmul(PS[0][:], lhsT=ones_t[:], rhs=y[:, c, :], start=(c == 0), stop=(c == CT - 1)), [cb_, yb[c]], [psb[0]])
            for c in range(CT):
                cx.add("pe", lambda e, c=c: e.matmul(PS[1][:], lhsT=ones_t[:], rhs=ysq[:, c, :], start=(c == 0), stop=(c == CT - 1)), [cb_, ysqb[c]], [psb[1]])
            cx.add("act", lambda e: e.copy(out=mean_t[:], in_=PS[0][:]), [psb[0]], [stb])
            cx.add("dve", lambda e: e.tensor_tensor(out=rstd_t[:], in0=mean_t[:], in1=mean_t[:], op=ALU.mult), [stb], [stb])
            cx.add("dve", lambda e: e.tensor_tensor(out=rstd_t[:], in0=PS[1][:], in1=rstd_t[:], op=ALU.subtract), [stb, psb[1]], [stb])
            cx.add("act", lambda e: e.activation(out=rstd_t[:], in_=rstd_t[:], func=AF.Sqrt, bias=eps_ln[:, 0:1]), [stb], [stb])
            cx.add("dve", lambda e: e.reciprocal(out=rstd_t[:], in_=rstd_t[:]), [stb], [stb])
            for c in range(CT):
                s = c % 2
                cx.add("dve", lambda e, s=s, c=c: e.tensor_tensor(out=zt[s][:], in0=y[:, c, :], in1=mean_t[:], op=ALU.subtract), [yb[c], stb], [ztb[s]])
                cx.add("dve", lambda e, s=s: e.tensor_tensor(out=zt[s][:], in0=zt[s][:], in1=rstd_t[:], op=ALU.mult), [ztb[s], stb], [ztb[s]])
                cx.add("act", lambda e, s=s, c=c: e.activation(out=actT[:, c, :], in_=zt[s][:], func=AF.Silu, scale=lng_t[:, c:c + 1], bias=lnb_t[:, c:c + 1]),
                       [ztb[s], cb_], [actb[c]])
            for dt in range(DK):
                pi = 2 + dt % 2
                s = dt % 2
                for c in range(CT):
                    cx.add("pe", lambda e, c=c, dt=dt, pi=pi: e.matmul(PS[pi][:], lhsT=cwo_bf[:, c, dt * 128:(dt + 1) * 128], rhs=actT[:, c, :],
                                                                        start=(c == 0), stop=(c == CT - 1)), [cwob, actb[c]], [psb[pi]])
                cx.dma(g0t[s][:], gateT_r[dt * 128:(dt + 1) * 128, gi * 512:(gi + 1) * 512], [], [g0b[s]], key=f"g0{s}")
                cx.add("dve", lambda e, s=s, pi=pi: e.tensor_tensor(out=mct[s][:], in0=PS[pi][:], in1=g0t[s][:], op=ALU.mult), [psb[pi], g0b[s]], [mcb[s]])
                cx.dma(mconvT_w[dt * 128:(dt + 1) * 128, gi * 512:(gi + 1) * 512], mct[s][:], [mcb[s]], [], key=f"mc{s}", q="pool")
        cx.flush()
        sb.release()

    TS = 256
    NCH = NALL // TS

    def stage_S():
        sb = SB(nc)
        sbt = SB(nc)
        psb = psbufs()
        PI = math.pi
        cosT = sb.sb("S_cosT", [128, 32, TS], F32)
        sinT = sb.sb("S_sinT", [128, 32, TS], F32)
        ur = sb.sb("S_ur", [128, 32], F32)
        ui = sb.sb("S_ui", [128, 32], F32)
        rA = sb.sb("S_rA", [128, 32], F32)
        BTr = sb.sb("S_BTr", [128, CT, 128], BF16)
        BTi = sb.sb("S_BTi", [128, CT, 128], BF16)
        BTr3 = sb.sb("S_BTr3", [128, CT, 128], BF16)
        BTi3 = sb.sb("S_BTi3", [128, CT, 128], BF16)
        CTr = sb.sb("S_CTr", [128, 32 * 128], BF16)
        CTi = sb.sb("S_CTi", [128, 32 * 128], BF16)
        dsk_t = sb.sb("S_dsk", [128, CT], F32)

        def lam_bar(pref, src, n):
            t = {k: sbt.sb(f"S_{pref}_{k}", [128, n], F32) for k in ("dt", "ar", "th", "r", "sn", "cs", "lr", "li", "tmp")}
            b = cx.buf(pref)
            raw = sbt.sb(f"S_{pref}_raw", [128, 3, n], F32)
            cx.dma(raw[:], src, [], [b], key="c0")
            cx.add("act", lambda e: e.activation(out=t["dt"][:], in_=raw[:, 2, :], func=AF.Exp), [b], [b])
            cx.add("dve", lambda e: e.tensor_tensor(out=t["ar"][:], in0=raw[:, 0, :], in1=t["dt"][:], op=ALU.mult), [b], [b])
            cx.add("dve", lambda e: e.tensor_tensor(out=t["th"][:], in0=raw[:, 1, :], in1=t["dt"][:], op=ALU.mult), [b], [b])
            cx.add("act", lambda e: e.activation(out=t["r"][:], in_=t["ar"][:], func=AF.Exp), [b], [b])
            for _ in range(5):
                cx.add("dve", lambda e: e.tensor_scalar(out=t["tmp"][:], in0=t["th"][:], scalar1=PI, scalar2=2 * PI, op0=ALU.is_gt, op1=ALU.mult), [b], [b])
                cx.add("dve", lambda e: e.tensor_tensor(out=t["th"][:], in0=t["th"][:], in1=t["tmp"][:], op=ALU.subtract), [b], [b])
            cx.add("act", lambda e: e.activation(out=t["sn"][:], in_=t["th"][:], func=AF.Sin), [b], [b])
            cx.add("dve", lambda e: e.tensor_scalar(out=t["cs"][:], in0=t["th"][:], scalar1=PI / 2, scalar2=None, op0=ALU.add), [b], [b])
            cx.add("dve", lambda e: e.tensor_scalar(out=t["tmp"][:], in0=t["cs"][:], scalar1=PI, scalar2=2 * PI, op0=ALU.is_gt, op1=ALU.mult), [b], [b])
            cx.add("dve", lambda e: e.tensor_tensor(out=t["tmp"][:], in0=t["cs"][:], in1=t["tmp"][:], op=ALU.subtract), [b], [b])
            cx.add("act", lambda e: e.activation(out=t["cs"][:], in_=t["tmp"][:], func=AF.Sin), [b], [b])
            cx.add("dve", lambda e: e.tensor_tensor(out=t["lr"][:], in0=t["r"][:], in1=t["cs"][:], op=ALU.mult), [b], [b])
            cx.add("dve", lambda e: e.tensor_tensor(out=t["li"][:], in0=t["r"][:], in1=t["sn"][:], op=ALU.mult), [b], [b])
            t["raw"] = raw
            return t, b

        def cmul(o_r, o_i, a_r, a_i, b_r, b_i, t1, t2, bufs_r, bufs_w, eng="dve"):
            cx.add(eng, lambda e: e.tensor_tensor(out=t1, in0=a_r, in1=b_r, op=ALU.mult), bufs_r, bufs_w)
            cx.add(eng, lambda e: e.tensor_tensor(out=t2, in0=a_i, in1=b_i, op=ALU.mult), bufs_r, bufs_w)
            cx.add(eng, lambda e: e.tensor_tensor(out=o_r, in0=t1, in1=t2, op=ALU.subtract), bufs_r, bufs_w)
            cx.add(eng, lambda e: e.tensor_tensor(out=t1, in0=a_r, in1=b_i, op=ALU.mult), bufs_r, bufs_w)
            cx.add(eng, lambda e: e.tensor_tensor(out=t2, in0=a_i, in1=b_r, op=ALU.mult), bufs_r, bufs_w)
            cx.add(eng, lambda e: e.tensor_tensor(out=o_i, in0=t1, in1=t2, op=ALU.add), bufs_r, bufs_w)

        A, Ab = lam_bar("A", sA, 32)
        tb = cx.buf("tables")
        ur2 = sbt.sb("S_ur2", [128, 32], F32)
        ui2 = sbt.sb("S_ui2", [128, 32], F32)
        tt1 = sbt.sb("S_tt1", [128, 32, TS // 2], F32)
        tt2 = sbt.sb("S_tt2", [128, 32, TS // 2], F32)
        cx.add("dve", lambda e: e.tensor_copy(out=rA[:], in_=A["r"][:]), [Ab], [tb])
        cx.add("dve", lambda e: e.memset(cosT[:, :, 0:1], 1.0), [], [tb])
        cx.add("dve", lambda e: e.memset(sinT[:, :, 0:1], 0.0), [], [tb])
        cx.add("dve", lambda e: e.tensor_copy(out=ur[:], in_=A["cs"][:]), [Ab], [tb])
        cx.add("dve", lambda e: e.tensor_copy(out=ui[:], in_=A["sn"][:]), [Ab], [tb])
        m = 1
        while m < TS:
            urb = ur[:].unsqueeze(2).to_broadcast([128, 32, m])
            uib = ui[:].unsqueeze(2).to_broadcast([128, 32, m])
            cmul(cosT[:, :, m:2 * m], sinT[:, :, m:2 * m], cosT[:, :, 0:m], sinT[:, :, 0:m], urb, uib, tt1[:, :, 0:m], tt2[:, :, 0:m], [tb], [tb])
            cmul(ur2[:], ui2[:], ur[:], ui[:], ur[:], ui[:], tt1[:, :, 0], tt2[:, :, 0], [tb], [tb])
            cx.add("dve", lambda e: e.tensor_copy(out=ur[:], in_=ur2[:]), [tb], [tb])
            cx.add("dve", lambda e: e.tensor_copy(out=ui[:], in_=ui2[:]), [tb], [tb])
            m *= 2
        Bp, Bb = lam_bar("B", sB, CT * 64)
        nB = CT * 64
        braw = sbt.sb("S_braw", [128, 2, nB], F32)
        cx.dma(braw[:], bT, [], [Bb], key="c0")
        tB = {k: sbt.sb(f"S_tB_{k}", [128, nB], F32) for k in ("nr", "den", "cr", "ci", "t1", "t2", "br", "bi")}
        cx.add("dve", lambda e: e.tensor_scalar(out=tB["nr"][:], in0=Bp["lr"][:], scalar1=-1.0, scalar2=None, op0=ALU.add), [Bb], [Bb])
        are, aim = Bp["raw"][:, 0, :], Bp["raw"][:, 1, :]
        cx.add("dve", lambda e: e.tensor_tensor(out=tB["den"][:], in0=are, in1=are, op=ALU.mult), [Bb], [Bb])
        cx.add("dve", lambda e: e.tensor_tensor(out=tB["t1"][:], in0=aim, in1=aim, op=ALU.mult), [Bb], [Bb])
        cx.add("dve", lambda e: e.tensor_tensor(out=tB["den"][:], in0=tB["den"][:], in1=tB["t1"][:], op=ALU.add), [Bb], [Bb])
        cx.add("dve", lambda e: e.reciprocal(out=tB["den"][:], in_=tB["den"][:]), [Bb], [Bb])
        cx.add("dve", lambda e: e.tensor_tensor(out=tB["t1"][:], in0=tB["nr"][:], in1=are, op=ALU.mult), [Bb], [Bb])
        cx.add("dve", lambda e: e.tensor_tensor(out=tB["t2"][:], in0=Bp["li"][:], in1=aim, op=ALU.mult), [Bb], [Bb])
        cx.add("dve", lambda e: e.tensor_tensor(out=tB["cr"][:], in0=tB["t1"][:], in1=tB["t2"][:], op=ALU.add), [Bb], [Bb])
        cx.add("dve", lambda e: e.tensor_tensor(out=tB["t1"][:], in0=Bp["li"][:], in1=are, op=ALU.mult), [Bb], [Bb])
        cx.add("dve", lambda e: e.tensor_tensor(out=tB["t2"][:], in0=tB["nr"][:], in1=aim, op=ALU.mult), [Bb], [Bb])
        cx.add("dve", lambda e: e.tensor_tensor(out=tB["ci"][:], in0=tB["t1"][:], in1=tB["t2"][:], op=ALU.subtract), [Bb], [Bb])
        cx.add("dve", lambda e: e.tensor_tensor(out=tB["cr"][:], in0=tB["cr"][:], in1=tB["den"][:], op=ALU.mult), [Bb], [Bb])
        cx.add("dve", lambda e: e.tensor_tensor(out=tB["ci"][:], in0=tB["ci"][:], in1=tB["den"][:], op=ALU.mult), [Bb], [Bb])
        cmul(tB["br"][:], tB["bi"][:], tB["cr"][:], tB["ci"][:], braw[:, 0, :], braw[:, 1, :], tB["t1"][:], tB["t2"][:], [Bb], [Bb])
        mB = sbt.sb("S_maskB", [128, 128], F32)
        cx.dma(mB[:], maskB, [], [Bb], key="c0")
        mBb = mB[:].rearrange("p (g q) -> p g q", g=2).unsqueeze(1).to_broadcast([128, CT, 2, 64])
        for src, dst in ((tB["br"], BTr), (tB["bi"], BTi)):
            sv = src[:].rearrange("p (k q) -> p k q", k=CT).unsqueeze(2).to_broadcast([128, CT, 2, 64])
            cx.add("dve", lambda e, sv=sv, dst=dst: e.tensor_tensor(out=dst[:].rearrange("p k (g q) -> p k g q", g=2), in0=sv, in1=mBb, op=ALU.mult), [Bb], [Bb])
        rm_t = sbt.sb("S_rowm", [128, 1], F32)
        cx.dma(rm_t[:], rowm, [], [Bb], key="c0")
        for src, dst in ((BTr, BTr3), (BTi, BTi3)):
            cx.add("dve", lambda e, src=src, dst=dst: e.tensor_scalar(out=dst[:], in0=src[:], scalar1=rm_t[:, 0:1], scalar2=None, op0=ALU.mult), [Bb], [Bb])
        Cb = cx.buf("C")
        cx.dma(CTr[:], cTp[:, 0, :], [], [Cb], key="cc", q="pool")
        cx.dma(CTi[:], cTp[:, 1, :], [], [Cb], key="cc", q="pool")
        cx.add("pool", lambda e: e.tensor_scalar(out=CTi[:], in0=CTi[:], scalar1=-1.0, scalar2=None, op0=ALU.mult), [Cb], [Cb])
        cx.dma(dsk_t[:], dsk, [], [Cb], key="c0")
        cx.flush()
        sbt.release()
        psb = psbufs()
        zin_r = sb.sb("S_zinr", [128, 32], F32)
        zin_i = sb.sb("S_zini", [128, 32], F32)
        zl_r = sb.sb("S_zlr", [128, 32], F32)
        zl_i = sb.sb("S_zli", [128, 32], F32)
        zc1 = sb.sb("S_zc1", [128, 32], F32)
        zc2 = sb.sb("S_zc2", [128, 32], F32)
        zb = cx.buf("zstate")
        cx.add("dve", lambda e: e.memset(zin_r[:], 0.0), [], [zb])
        cx.add("dve", lambda e: e.memset(zin_i[:], 0.0), [], [zb])
        NTMP = 4
        uts = [sb.sb(f"S_ut{i}", [128, CT, TS], F32) for i in range(2)]
        utb = [cx.buf(f"ut{i}") for i in range(2)]
        utsb = [sb.sb(f"S_utb{i}", [128, CT, TS], BF16) for i in range(2)]
        utbb = [cx.buf(f"utbb{i}") for i in range(2)]
        xrb = [sb.sb(f"S_xrb{i}", [128, TS], BF16) for i in range(NTMP)]
        xib = [sb.sb(f"S_xib{i}", [128, TS], BF16) for i in range(NTMP)]
        xrbb = [cx.buf(f"xrb{i}") for i in range(NTMP)]
        xibb = [cx.buf(f"xib{i}") for i in range(NTMP)]
        tmp = {k: [sb.sb(f"S_{k}{i}", [128, TS], F32) for i in range(NTMP)] for k in ("t1", "t2", "wr", "wi", "zr", "zi", "xr", "xi")}
        tmpb = {k: [cx.buf(f"{k}{i}") for i in range(NTMP)] for k in tmp}
        yv = [sb.sb(f"S_yv{i}", [128, TS], F32) for i in range(2)]
        yvb = [cx.buf(f"yv{i}") for i in range(2)]
        g1 = [sb.sb(f"S_g1{i}", [128, TS], F32) for i in range(2)]
        g1b_ = [cx.buf(f"g1{i}") for i in range(2)]
        g2 = [sb.sb(f"S_g2{i}", [128, TS], F32) for i in range(2)]
        g2b_ = [cx.buf(f"g2{i}") for i in range(2)]
        zo = [sb.sb(f"S_zo{i}", [128, TS], BF16) for i in range(2)]
        zob = [cx.buf(f"zo{i}") for i in range(2)]
        n = 0
        for ci in range(NCH):
            own = ci >= NCH // 2
            us = ci % 2
            cx.dma_k(uts[us], ssmT_r[:, ci * TS:(ci + 1) * TS], CT, [], [utb[us]], f"ut{us}")
            cx.add("act", lambda e, us=us: e.copy(out=utsb[us][:], in_=uts[us][:]), [utb[us]], [utbb[us]])
            for j in range(32):
                k, po = j // 4, 32 * (j % 4)
                s = n % NTMP
                n += 1
                pb = 0 + 2 * (j % 2)
                if po == 96:
                    lr_, li_, p0, p1 = BTr3, BTi3, 64, 128
                else:
                    lr_, li_, p0, p1 = BTr, BTi, po, po + 32
                cx.add("pe", lambda e, k=k, pb=pb, us=us, lr_=lr_, p0=p0, p1=p1: e.matmul(PS[pb][:, 0:TS], lhsT=lr_[p0:p1, k, :], rhs=utsb[us][p0:p1, k, :], start=True, stop=True),
                       [Bb, utbb[us]], [psb[pb]])
                cx.add("pe", lambda e, k=k, pb=pb, us=us, li_=li_, p0=p0, p1=p1: e.matmul(PS[pb + 1][:, 0:TS], lhsT=li_[p0:p1, k, :], rhs=utsb[us][p0:p1, k, :], start=True, stop=True),
                       [Bb, utbb[us]], [psb[pb + 1]])
                bre, bim = PS[pb][:, 0:TS], PS[pb + 1][:, 0:TS]
                cs, sn = cosT[:, j, :], sinT[:, j, :]
                T = {kk: tmp[kk][s][:] for kk in tmp}
                TB = {kk: tmpb[kk][s] for kk in tmp}
                cx.add("dve", lambda e, T=T, bre=bre, cs=cs: e.tensor_tensor(out=T["t1"], in0=bre, in1=cs, op=ALU.mult), [psb[pb], tb], [TB["t1"]])
                cx.add("dve", lambda e, T=T, bim=bim, sn=sn: e.tensor_tensor(out=T["t2"], in0=bim, in1=sn, op=ALU.mult), [psb[pb + 1], tb], [TB["t2"]])
                cx.add("pool", lambda e, T=T: e.tensor_tensor(out=T["wr"], in0=T["t1"], in1=T["t2"], op=ALU.add), [TB["t1"], TB["t2"]], [TB["wr"]])
                cx.add("dve", lambda e, T=T, bim=bim, cs=cs: e.tensor_tensor(out=T["xr"], in0=bim, in1=cs, op=ALU.mult), [psb[pb + 1], tb], [TB["xr"]])
                cx.add("dve", lambda e, T=T, bre=bre, sn=sn: e.tensor_tensor(out=T["xi"], in0=bre, in1=sn, op=ALU.mult), [psb[pb], tb], [TB["xi"]])
                cx.add("pool", lambda e, T=T: e.tensor_tensor(out=T["wi"], in0=T["xr"], in1=T["xi"], op=ALU.subtract), [TB["xr"], TB["xi"]], [TB["wi"]])
                rb = rA[:, j:j + 1].to_broadcast([128, TS])
                cx.add("dve", lambda e, T=T, rb=rb, j=j: e.tensor_tensor_scan(out=T["zr"], data0=rb, data1=T["wr"], initial=zin_r[:, j:j + 1], op0=ALU.mult, op1=ALU.add),
                       [TB["wr"], tb, zb], [TB["zr"]])
                cx.add("dve", lambda e, T=T, rb=rb, j=j: e.tensor_tensor_scan(out=T["zi"], data0=rb, data1=T["wi"], initial=zin_i[:, j:j + 1], op0=ALU.mult, op1=ALU.add),
                       [TB["wi"], tb, zb], [TB["zi"]])
                cx.add("pool", lambda e, T=T, j=j: e.tensor_copy(out=zl_r[:, j:j + 1], in_=T["zr"][:, TS - 1:TS]), [TB["zr"]], [zb])
                cx.add("pool", lambda e, T=T, j=j: e.tensor_copy(out=zl_i[:, j:j + 1], in_=T["zi"][:, TS - 1:TS]), [TB["zi"]], [zb])
                if own:
                    cx.add("dve", lambda e, T=T, cs=cs: e.tensor_tensor(out=T["t1"], in0=T["zr"], in1=cs, op=ALU.mult), [TB["zr"], tb], [TB["t1"]])
                    cx.add("dve", lambda e, T=T, sn=sn: e.tensor_tensor(out=T["t2"], in0=T["zi"], in1=sn, op=ALU.mult), [TB["zi"], tb], [TB["t2"]])
                    cx.add("pool", lambda e, T=T, s=s: e.tensor_tensor(out=xrb[s][:], in0=T["t1"], in1=T["t2"], op=ALU.subtract), [TB["t1"], TB["t2"]], [xrbb[s]])
                    cx.add("dve", lambda e, T=T, sn=sn: e.tensor_tensor(out=T["wr"], in0=T["zr"], in1=sn, op=ALU.mult), [TB["zr"], tb], [TB["wr"]])
                    cx.add("dve", lambda e, T=T, cs=cs: e.tensor_tensor(out=T["wi"], in0=T["zi"], in1=cs, op=ALU.mult), [TB["zi"], tb], [TB["wi"]])
                    cx.add("pool", lambda e, T=T, s=s: e.tensor_tensor(out=xib[s][:], in0=T["wr"], in1=T["wi"], op=ALU.add), [TB["wr"], TB["wi"]], [xibb[s]])
                    py = 4 + (k % 2)
                    cx.add("pe", lambda e, s=s, j=j, py=py: e.matmul(PS[py][:, 0:TS], lhsT=CTr[:, j * 128:(j + 1) * 128], rhs=xrb[s][:], start=(j % 4 == 0), stop=False),
                           [Cb, xrbb[s]], [psb[py]])
                    cx.add("pe", lambda e, s=s, j=j, py=py: e.matmul(PS[py][:, 0:TS], lhsT=CTi[:, j * 128:(j + 1) * 128], rhs=xib[s][:], start=False, stop=(j % 4 == 3)),
                           [Cb, xibb[s]], [psb[py]])
                    if j % 4 == 3:
                        ys = k % 2
                        cx.add("dve", lambda e, ys=ys, k=k, py=py, us=us: e.scalar_tensor_tensor(out=yv[ys][:], in0=uts[us][:, k, :], scalar=dsk_t[:, k:k + 1], in1=PS[py][:, 0:TS],
                                                                                               op0=ALU.mult, op1=ALU.add), [utb[us], Cb, psb[py]], [yvb[ys]])
                        gelu_tanh(yv[ys][:], yvb[ys], None, g1[ys][:], g1b_[ys], g2[ys][:], g2b_[ys], zo[ys][:], zob[ys])
                        t0 = (ci - NCH // 2) * TS
                        cx.dma(zT_w[k * 128:(k + 1) * 128, t0:t0 + TS], zo[ys][:], [zob[ys]], [], key=f"zo{ys}")
            cmul(zin_r[:], zin_i[:], zl_r[:], zl_i[:], ur[:], ui[:], zc1[:], zc2[:], [zb, tb], [zb])
        cx.flush()
        sb.release()

    def stage_M1():
        sb = SB(nc)
        psb = psbufs()
        wv = sb.sb("M_wv", [128, CT, D], BF16)
        wg = sb.sb("M_wg", [128, CT, D], BF16)
        wb = cx.buf("w")
        stg = [sb.sb(f"M_stg{i}", [128, D], F32) for i in range(2)]
        stgb = [cx.buf(f"stg{i}") for i in range(2)]
        ctr = [0]
        for c in range(CT):
            load_cast(stg, stgb, ctr, wval[:, c, :], wv[:, c, :], wb, D, "stg")
            load_cast(stg, stgb, ctr, wgate[:, c, :], wg[:, c, :], wb, D, "stg", cast_eng="act")
        zts = [sb.sb(f"M_zt{i}", [128, CT, 512], BF16) for i in range(2)]
        ztb = [cx.buf(f"zt{i}") for i in range(2)]
        sg = [sb.sb(f"M_sg{i}", [128, 512], F32) for i in range(2)]
        sgb = [cx.buf(f"sg{i}") for i in range(2)]
        g1t = [sb.sb(f"M_g1{i}", [128, 512], F32) for i in range(2)]
        g1tb = [cx.buf(f"g1{i}") for i in range(2)]
        mct = [sb.sb(f"M_mc{i}", [128, 512], F32) for i in range(2)]
        mctb = [cx.buf(f"mc{i}") for i in range(2)]
        mo = [sb.sb(f"M_mo{i}", [128, 512], BF16) for i in range(2)]
        mob = [cx.buf(f"mo{i}") for i in range(2)]
        for gi in range(NGO):
            zs = gi % 2
            cx.dma_k(zts[zs], zT_r[:, gi * 512:(gi + 1) * 512], CT, [], [ztb[zs]], f"zt{zs}")
            for dt in range(DK):
                s = dt % 2
                pv, pg = 0 + s, 2 + s
                for c in range(CT):
                    cx.add("pe", lambda e, c=c, dt=dt, pv=pv, zs=zs: e.matmul(PS[pv][:], lhsT=wv[:, c, dt * 128:(dt + 1) * 128], rhs=zts[zs][:, c, :], start=(c == 0), stop=(c == CT - 1)),
                           [wb, ztb[zs]], [psb[pv]])
                for c in range(CT):
                    cx.add("pe", lambda e, c=c, dt=dt, pg=pg, zs=zs: e.matmul(PS[pg][:], lhsT=wg[:, c, dt * 128:(dt + 1) * 128], rhs=zts[zs][:, c, :], start=(c == 0), stop=(c == CT - 1)),
                           [wb, ztb[zs]], [psb[pg]])
                cx.dma(g1t[s][:], gateT_r[D + dt * 128:D + (dt + 1) * 128, gi * 512:(gi + 1) * 512], [], [g1tb[s]], key=f"g1{s}")
                cx.dma(mct[s][:], mconvT_r[dt * 128:(dt + 1) * 128, gi * 512:(gi + 1) * 512], [], [mctb[s]], key=f"mc{s}")
                cx.add("act", lambda e, s=s, pg=pg: e.activation(out=sg[s][:], in_=PS[pg][:], func=AF.Sigmoid), [psb[pg]], [sgb[s]])
                cx.add("dve", lambda e, s=s, pv=pv: e.tensor_tensor(out=sg[s][:], in0=PS[pv][:], in1=sg[s][:], op=ALU.mult), [psb[pv], sgb[s]], [sgb[s]])
                cx.add("dve", lambda e, s=s: e.tensor_tensor(out=sg[s][:], in0=sg[s][:], in1=g1t[s][:], op=ALU.mult), [sgb[s], g1tb[s]], [sgb[s]])
                cx.add("dve", lambda e, s=s: e.tensor_tensor(out=mo[s][:], in0=sg[s][:], in1=mct[s][:], op=ALU.add), [sgb[s], mctb[s]], [mob[s]])
                cx.dma(mergedT_w[dt * 128:(dt + 1) * 128, gi * 512:(gi + 1) * 512], mo[s][:], [mob[s]], [], key=f"mo{s}", q="pool")
        cx.flush()
        sb.release()

    def stage_M2():
        sb = SB(nc)
        psb = psbufs()
        wo = sb.sb("O_wo", [128, DK, D], BF16)
        wb = cx.buf("w")
        stg = [sb.sb(f"O_stg{i}", [128, D], F32) for i in range(2)]
        stgb = [cx.buf(f"stg{i}") for i in range(2)]
        ctr = [0]
        for k in range(DK):
            load_cast(stg, stgb, ctr, wout[:, k, :], wo[:, k, :], wb, D, "stg", cast_eng=("pool" if k % 2 else "act"))
        gb_t = sb.sb("O_gb", [128, D], F32)
        gbb = cx.buf("gb")
        cx.dma(gb_t[:], g2b, [], [gbb], key="c0")
        mts = [sb.sb(f"O_mt{i}", [128, DK, 512], BF16) for i in range(2)]
        mtb = [cx.buf(f"mt{i}") for i in range(2)]
        xts = [sb.sb(f"O_xt{i}", [128, D], F32) for i in range(2)]
        xtb = [cx.buf(f"xt{i}") for i in range(2)]
        hn = sb.sb("O_hn", [128, D], BF16)
        hn_b = cx.buf("hn")
        junk = sb.sb("O_junk", [128, D], BF16)
        junk_b = cx.buf("junk")
        sts = sb.sb("O_st", [128, 2], F32)
        st_b = cx.buf("st")
        idb = cx.buf("identb")
        hts = [sb.sb(f"O_ht{i}", [128, DK, 128], BF16) for i in range(2)]
        htb = [cx.buf(f"ht{i}") for i in range(2)]
        n = 0
        for gi in range(NGO):
            ms = gi % 2
            cx.dma_k(mts[ms], mergedT_r[:, gi * 512:(gi + 1) * 512], DK, [], [mtb[ms]], f"mt{ms}")
            for ts_ in range(4):
                s = n % 2
                n += 1
                tok0 = gi * 512 + ts_ * 128
                cx.dma(xts[s][:], xin[NT + tok0:NT + tok0 + 128, :], [], [xtb[s]], key=f"xt{s}")
                for dc in range(4):
                    pi = 2 + dc
                    for k in range(DK):
                        cx.add("pe", lambda e, k=k, dc=dc, pi=pi, ms=ms, ts_=ts_: e.matmul(PS[pi][:], lhsT=mts[ms][:, k, ts_ * 128:(ts_ + 1) * 128], rhs=wo[:, k, dc * 512:(dc + 1) * 512],
                                                                                          start=(k == 0), stop=(k == DK - 1)), [mtb[ms], wb], [psb[pi]])
                    cx.add("dve", lambda e, s=s, dc=dc, pi=pi: e.tensor_tensor(out=xts[s][:, dc * 512:(dc + 1) * 512], in0=PS[pi][:], in1=xts[s][:, dc * 512:(dc + 1) * 512], op=ALU.add),
                           [psb[pi], xtb[s]], [xtb[s]])
                cx.dma(x1s_w[tok0:tok0 + 128, :], xts[s][:], [xtb[s]], [], key=f"x1{s}", q="pool")
                rmsnorm_T(sb, xts[s][:], xtb[s], gb_t, gbb, hn, hn_b, junk, junk_b, sts, st_b, ident_b, idb, psb, 0,
                          lambda k0, k1, s=s: hts[s][:, k0:k1, :], htb[s])
                cx.dma_k(hts[s], hn2T_w[:, tok0:tok0 + 128], DK, [htb[s]], [], f"ht{s}", store=True, q="pool")
        cx.flush()
        sb.release()

    def stage_Q0():
        sb = SB(nc)
        psb = psbufs()
        wq_bf = sb.sb("Q0_wq", [128, DK, D], BF16)
        wb = cx.buf("w")
        stg = [sb.sb(f"Q0_stg{i}", [128, D], F32) for i in range(2)]
        stgb = [cx.buf(f"stg{i}") for i in range(2)]
        ctr = [0]
        for k in range(DK):
            load_cast(stg, stgb, ctr, wq[:, k, :], wq_bf[:, k, :], wb, D, "stg", cast_eng=("pool" if k % 2 else "act"))
        hts = [sb.sb(f"Q0_ht{i}", [128, DK, 512], BF16) for i in range(2)]
        htb = [cx.buf(f"ht{i}") for i in range(2)]
        evs = [sb.sb(f"Q0_ev{i}", [128, 512], F32) for i in range(3)]
        evb = [cx.buf(f"ev{i}") for i in range(3)]
        n = 0
        for gi in range(NGO):
            hs = gi % 2
            cx.dma_k(hts[hs], hn2T_r[:, gi * 512:(gi + 1) * 512], DK, [], [htb[hs]], f"ht{hs}")
            for hc in range(16):
                pi = hc % 4
                ei = n % 3
                n += 1
                for k in range(DK):
                    cx.add("pe", lambda e, k=k, hc=hc, pi=pi, hs=hs: e.matmul(PS[pi][:], lhsT=wq_bf[:, k, hc * 128:(hc + 1) * 128], rhs=hts[hs][:, k, :], start=(k == 0), stop=(k == DK - 1)),
                           [wb, htb[hs]], [psb[pi]])
                cx.add("act", lambda e, ei=ei, pi=pi: e.copy(out=evs[ei][:], in_=PS[pi][:]), [psb[pi]], [evb[ei]])
                cx.dma(qTs_w[hc * 128:(hc + 1) * 128, gi * 512:(gi + 1) * 512], evs[ei][:], [evb[ei]], [], key=f"ev{ei}", q="pool")
        cx.flush()
        sb.release()

    def stage_Q():
        sb = SB(nc)
        psb = psbufs()
        kT_t = sb.sb("Q_kT", [128, 16, 128], F32)
        io_t = sb.sb("Q_iota", [128, 128], F32)
        cb_ = cx.buf("const")
        cx.dma(kT_t[:], kT, [], [cb_], key="c0")
        cx.dma(io_t[:], iota, [], [cb_], key="c0")
        qT = sb.sb("Q_qT", [128, 16, 512], F32)
        qTb1 = cx.buf("qT")
        qTb = [qTb1] * 16
        S = sb.sb("Q_S", [128, 16, 128], F32)
        Sb = cx.buf("S")
        Sw = sb.sb("Q_Sw", [128, 128], F32)
        Swb = cx.buf("Sw")
        v16 = sb.sb("Q_v16", [128, 16, 16], F32)
        i16u = sb.sb("Q_i16u", [128, 16, 16], U32)
        i16 = sb.sb("Q_i16", [128, 16, 16], F32)
        vb = cx.buf("v16")
        cand = sb.sb("Q_cand", [128, 8, 256], F32)
        candw = sb.sb("Q_candw", [128, 256], F32)
        candb = cx.buf("cand")
        candwb = cx.buf("candw")
        best = sb.sb("Q_best", [128, 8, 16], F32)
        posu = sb.sb("Q_posu", [128, 8, 16], U32)
        r1u = sb.sb("Q_r1u", [128, 8, 16], U32)
        r2u = sb.sb("Q_r2u", [128, 8, 16], U32)
        r1f = sb.sb("Q_r1f", [128, 8, 16], F32)
        r2f = sb.sb("Q_r2f", [128, 8, 16], F32)
        bb = cx.buf("best")
        eq = sb.sb("Q_eq", [128, 8, 16, 16], F32)
        eqb = cx.buf("eq")
        IJG = sb.sb("Q_IJG", [128, 3, 128], F32)
        ijgb = cx.buf("ijg")
        zs_ = sb.sb("Q_zs", [128, 8], F32)
        IJGT = sb.sb("Q_IJGT", [128, 3, 128], F32)
        ijgtb = cx.buf("ijgt")
        Aoh = [sb.sb(f"Q_A{i}", [128, 64, 128], BF16) for i in range(2)]
        Boh = [sb.sb(f"Q_B{i}", [128, 64, 128], BF16) for i in range(2)]
        Abs = [cx.buf(f"A{i}") for i in range(2)]
        Bbs = [cx.buf(f"B{i}") for i in range(2)]
        nh = 0
        nta = 0
        nJG = sb.sb("Q_nJG", [128, 2, 128], F32)
        njgb = cx.buf("njg")
        tmpA = [sb.sb(f"Q_tmpA{i}", [128, 128], F32) for i in range(4)]
        tmpAb = [cx.buf(f"tmpA{i}") for i in range(4)]
        Mst = [sb.sb(f"Q_Mst{i}", [128, 16, 128, 8], BF16) for i in range(1)]
        Mstb = [cx.buf(f"Mst{i}") for i in range(1)]
        n = 0
        for gi in range(NGO):
            cx.dma_k(qT, qTs_r[:, gi * 512:(gi + 1) * 512], 16, [], [qTb1], "qT")
            for ts_ in range(4):
                ms = 0
                n += 1
                for hc in range(16):
                    pi = hc // 4
                    cx.add("pe", lambda e, hc=hc, pi=pi, ts_=ts_: e.matmul(PS[pi][:, (hc % 4) * 128:(hc % 4 + 1) * 128], lhsT=qT[:, hc, ts_ * 128:(ts_ + 1) * 128], rhs=kT_t[:, hc, :],
                                                                            start=True, stop=True), [qTb[hc], cb_], [psb[pi]])
                for pi in range(4):
                    cx.add("act", lambda e, pi=pi: e.copy(out=S[:, pi * 4:(pi + 1) * 4, :], in_=PS[pi][:].rearrange("p (a n) -> p a n", a=4)), [psb[pi]], [Sb])
                for hc in range(16):
                    cx.add("dve", lambda e, hc=hc: e.max(out=v16[:, hc, 0:8], in_=S[:, hc, :]), [Sb], [vb])
                    cx.add("dve", lambda e, hc=hc: e.max_index(out=i16u[:, hc, 0:8], in_max=v16[:, hc, 0:8], in_values=S[:, hc, :]), [Sb, vb], [vb])
                    cx.add("dve", lambda e, hc=hc: e.match_replace(out=Sw[:], in_to_replace=v16[:, hc, 0:8], in_values=S[:, hc, :], imm_value=NEG), [Sb, vb], [Swb])
                    cx.add("dve", lambda e, hc=hc: e.max(out=v16[:, hc, 8:16], in_=Sw[:]), [Swb], [vb])
                    cx.add("dve", lambda e, hc=hc: e.max_index(out=i16u[:, hc, 8:16], in_max=v16[:, hc, 8:16], in_values=Sw[:]), [Swb, vb], [vb])
                cx.add("dve", lambda e: e.tensor_copy(out=i16[:], in_=i16u[:]), [vb], [vb])
                v4 = v16[:].rearrange("p (h c) k -> p h c k", c=2)
                i4 = i16[:].rearrange("p (h c) k -> p h c k", c=2)
                for h in range(8):
                    cx.add("dve", lambda e, h=h: e.tensor_tensor(out=cand[:, h, :].rearrange("p (a b) -> p a b", a=16),
                                                                  in0=v4[:, h, 0, :].unsqueeze(2).to_broadcast([128, 16, 16]),
                                                                  in1=v4[:, h, 1, :].unsqueeze(1).to_broadcast([128, 16, 16]), op=ALU.add), [vb], [candb])
                    cx.add("dve", lambda e, h=h: e.max(out=best[:, h, 0:8], in_=cand[:, h, :]), [candb], [bb])
                    cx.add("dve", lambda e, h=h: e.max_index(out=posu[:, h, 0:8], in_max=best[:, h, 0:8], in_values=cand[:, h, :]), [candb, bb], [bb])
                    cx.add("dve", lambda e, h=h: e.match_replace(out=candw[:], in_to_replace=best[:, h, 0:8], in_values=cand[:, h, :], imm_value=NEG), [candb, bb], [candwb])
                    cx.add("dve", lambda e, h=h: e.max(out=best[:, h, 8:16], in_=candw[:]), [candwb], [bb])
                    cx.add("dve", lambda e, h=h: e.max_index(out=posu[:, h, 8:16], in_max=best[:, h, 8:16], in_values=candw[:]), [candwb, bb], [bb])
                cx.add("dve", lambda e: e.tensor_single_scalar(out=r1u[:], in_=posu[:], scalar=4, op=ALU.logical_shift_right), [bb], [bb])
                cx.add("dve", lambda e: e.tensor_single_scalar(out=r2u[:], in_=posu[:], scalar=15, op=ALU.bitwise_and), [bb], [bb])
                cx.add("dve", lambda e: e.tensor_copy(out=r1f[:], in_=r1u[:]), [bb], [bb])
                cx.add("dve", lambda e: e.tensor_copy(out=r2f[:], in_=r2u[:]), [bb], [bb])
                io16 = io_t[:, 0:16].unsqueeze(1).unsqueeze(1).to_broadcast([128, 8, 16, 16])
                for (rf, cidx, slot) in ((r1f, 0, 0), (r2f, 1, 1)):
                    cx.add("dve", lambda e, rf=rf: e.tensor_tensor(out=eq[:], in0=rf[:].unsqueeze(3).to_broadcast([128, 8, 16, 16]), in1=io16, op=ALU.is_equal), [bb, cb_], [eqb])
                    cx.add("dve", lambda e, cidx=cidx: e.tensor_tensor(out=eq[:], in0=eq[:], in1=i4[:, :, cidx, :].unsqueeze(2).to_broadcast([128, 8, 16, 16]), op=ALU.mult), [eqb, vb], [eqb])
                    cx.add("dve", lambda e, slot=slot: e.tensor_reduce(out=IJG[:, slot, :].rearrange("p (h k) -> p h k", h=8), in_=eq[:], axis=AX.X, op=ALU.add), [eqb], [ijgb])
                cx.add("dve", lambda e: e.tensor_tensor(out=best[:], in0=best[:], in1=best[:, :, 0:1].to_broadcast([128, 8, 16]), op=ALU.subtract), [bb], [bb])
                cx.add("act", lambda e: e.activation(out=best[:], in_=best[:], func=AF.Exp), [bb], [bb])
                cx.add("dve", lambda e: e.tensor_reduce(out=zs_[:], in_=best[:], axis=AX.X, op=ALU.add), [bb], [bb])
                cx.add("dve", lambda e: e.reciprocal(out=zs_[:], in_=zs_[:]), [bb], [bb])
                cx.add("dve", lambda e: e.tensor_tensor(out=IJG[:, 2, :].rearrange("p (h k) -> p h k", h=8), in0=best[:], in1=zs_[:].unsqueeze(2).to_broadcast([128, 8, 16]), op=ALU.mult),
                       [bb], [ijgb])
                for a in range(3):
                    cx.add("pe", lambda e, a=a: e.transpose(out=PS[4][:, a * 128:(a + 1) * 128], in_=IJG[:, a, :], identity=ident_f[:]), [ijgb], [psb[4]])
                cx.add("act", lambda e: e.copy(out=IJGT[:], in_=PS[4][:, 0:384].rearrange("p (a t) -> p a t", a=3)), [psb[4]], [ijgtb])
                for hf in range(2):
                    hb_ = nh % 2
                    nh += 1
                    A_, B_ = Aoh[hb_], Boh[hb_]
                    iob = io_t[:].unsqueeze(1).to_broadcast([128, 64, 128])
                    cx.add("dve", lambda e, A_=A_, hf=hf: e.tensor_tensor(out=A_[:], in0=iob, in1=IJGT[:, 0, hf * 64:(hf + 1) * 64].unsqueeze(2).to_broadcast([128, 64, 128]), op=ALU.is_equal),
                           [cb_, ijgtb], [Abs[hb_]])
                    for tl in range(64):
                        t = hf * 64 + tl
                        cx.add("dve", lambda e, B_=B_, tl=tl, t=t: e.tensor_scalar(out=B_[:, tl, :], in0=io_t[:], scalar1=IJGT[:, 1, t:t + 1], scalar2=IJGT[:, 2, t:t + 1],
                                                                                 op0=ALU.is_equal, op1=ALU.mult), [cb_, ijgtb], [Bbs[hb_]], chain=True)
                    for t16 in range(4):
                        pbase = 4 * (t16 % 2)
                        for tl in range(16):
                            tloc = t16 * 16 + tl
                            pi = pbase + tl // 4
                            cx.add("pe", lambda e, tloc=tloc, pi=pi, tl=tl, A_=A_, B_=B_: e.matmul(PS[pi][:, (tl % 4) * 128:(tl % 4 + 1) * 128], lhsT=A_[:, tloc, :], rhs=B_[:, tloc, :], start=True, stop=True),
                                   [Abs[hb_], Bbs[hb_]], [psb[pi]])
                        for q4 in range(4):
                            pi = pbase + q4
                            tt0 = hf * 64 + t16 * 16 + q4 * 4
                            cx.add("act", lambda e, pi=pi, tt0=tt0, ms=ms: e.copy(out=Mst[ms][:, :, tt0:tt0 + 4, :].rearrange("p c t j -> p t c j"),
                                                                                  in_=PS[pi][:].rearrange("p (t c j) -> p t c j", t=4, c=16)), [psb[pi]], [Mstb[ms]], chain=True)
                t0 = ts_ * 128
                for jc in range(16):
                    cx.dma(Mscr_w[gi, jc, :, t0 * 8:(t0 + 128) * 8], Mst[ms][:, jc, :, :].rearrange("p t j -> p (t j)"), [Mstb[ms]], [], key=f"Mst{ms}")
        cx.flush()
        sb.release()

    def stage_P():
        sb = SB(nc)
        psb = psbufs()
        gb_t = sb.sb("P_gb", [128, D], F32)
        gbb = cx.buf("gb")
        cx.dma(gb_t[:], gfb, [], [gbb], key="c0")
        hts = sb.sb("P_ht", [128, DK, 512], BF16)
        htb = cx.buf("ht")
        acc = sb.sb("P_acc", [128, 4, D], F32)
        accb = [cx.buf(f"acc{i}") for i in range(4)]
        Mc = [sb.sb(f"P_Mc{i}", [128, 512, 8], BF16) for i in range(2)]
        Mcb = [cx.buf(f"Mc{i}") for i in range(2)]
        AT = sb.sb("P_AT", [128, 8, 512], BF16)
        ATb = [cx.buf(f"AT{i}") for i in range(8)]
        vtb = sb.sb("P_vtb", [128, 8, D], BF16)
        vtbb = [cx.buf(f"vtb{i}") for i in range(8)]
        NU = 4
        utb = [sb.sb(f"P_utb{i}", [128, DK * 128], BF16) for i in range(NU)]
        utbb = [cx.buf(f"utb{i}") for i in range(NU)]
        t1 = [sb.sb(f"P_t1{i}", [128, 512], F32) for i in range(2)]
        t1b = [cx.buf(f"t1{i}") for i in range(2)]
        t2 = [sb.sb(f"P_t2{i}", [128, 512], F32) for i in range(2)]
        t2b = [cx.buf(f"t2{i}") for i in range(2)]
        x1t = sb.sb("P_x1t", [128, D], F32)
        x1b = cx.buf("x1t")
        junk = sb.sb("P_junk", [128, D], BF16)
        junk_b = cx.buf("junk")
        sts = sb.sb("P_st", [128, 2], F32)
        st_b = cx.buf("st")
        n = 0
        for tp in range(NGO):
            cx.dma_k(hts, hn2T_r[:, tp * 512:(tp + 1) * 512], DK, [], [htb], "ht")
            for jc in range(16):
                mcs = jc % 2
                cx.dma(Mc[mcs][:].rearrange("p t j -> p (t j)"), Mscr_r[tp, jc, :, :], [], [Mcb[mcs]], key=f"Mc{mcs}")
                for jj in range(8):
                    j = jc * 8 + jj
                    s = n % 2
                    su = n % NU
                    n += 1
                    cx.dma(utb[su][:], UTr[j], [], [utbb[su]], key=f"utb{su}", q="pool")
                    cx.dma(vtb[:, jj, :], Vr[j], [], [vtbb[jj]], key=f"vtb{jj}", q="pool")
                    pi = 4 + (n % 4)
                    for k in range(DK):
                        cx.add("pe", lambda e, k=k, su=su, pi=pi: e.matmul(PS[pi][:], lhsT=utb[su][:, k * 128:(k + 1) * 128], rhs=hts[:, k, :], start=(k == 0), stop=(k == DK - 1)),
                               [utbb[su], htb], [psb[pi]])
                    gelu_tanh(PS[pi][:], psb[pi], None, t1[s][:], t1b[s], t2[s][:], t2b[s], AT[:, jj, :], ATb[jj], extra_mul=Mc[mcs][:, :, jj], extra_b=Mcb[mcs])
                for ts_ in range(4):
                    for dc in range(4):
                        pi = dc
                        for jj in range(8):
                            cx.add("pe", lambda e, jj=jj, ts_=ts_, dc=dc, pi=pi: e.matmul(PS[pi][:], lhsT=AT[:, jj, ts_ * 128:(ts_ + 1) * 128], rhs=vtb[:, jj, dc * 512:(dc + 1) * 512],
                                                                                          start=(jj == 0), stop=(jj == 7)), [ATb[jj], vtbb[jj]], [psb[pi]])
                        if jc == 0:
                            cx.add("act", lambda e, ts_=ts_, dc=dc, pi=pi: e.copy(out=acc[:, ts_, dc * 512:(dc + 1) * 512], in_=PS[pi][:]), [psb[pi]], [accb[ts_]])
                        else:
                            cx.add("dve", lambda e, ts_=ts_, dc=dc, pi=pi: e.tensor_tensor(out=acc[:, ts_, dc * 512:(dc + 1) * 512], in0=PS[pi][:], in1=acc[:, ts_, dc * 512:(dc + 1) * 512], op=ALU.add),
                                   [psb[pi], accb[ts_]], [accb[ts_]])
            for ts_ in range(4):
                tok0 = tp * 512 + ts_ * 128
                cx.dma(x1t[:], x1s_r[tok0:tok0 + 128, :], [], [x1b], key="x1t")
                cx.add("dve", lambda e, ts_=ts_: e.tensor_tensor(out=x1t[:], in0=x1t[:], in1=acc[:, ts_, :], op=ALU.add), [x1b, accb[ts_]], [x1b])
                ss, rs = sts[:, 0:1], sts[:, 1:2]
                cx.add("act", lambda e: e.activation(out=junk[:], in_=x1t[:], func=AF.Square, accum_out=ss), [x1b], [junk_b, st_b])
                cx.add("act", lambda e: e.activation(out=rs, in_=ss, func=AF.Sqrt, scale=1.0 / D, bias=eps_rms[:, 0:1]), [st_b], [st_b])
                cx.add("dve", lambda e: e.reciprocal(out=rs, in_=rs), [st_b], [st_b])
                cx.add("dve", lambda e, ts_=ts_: e.scalar_tensor_tensor(out=acc[:, ts_, :], in0=x1t[:], scalar=rs, in1=gb_t[:], op0=ALU.mult, op1=ALU.mult),
                       [x1b, st_b, gbb], [accb[ts_]])
                cx.dma(out[tok0:tok0 + 128, :], acc[:, ts_, :], [accb[ts_]], [], key=f"out{ts_ % 2}")
        cx.flush()
        sb.release()

    stages = {"A": stage_A, "C": stage_C, "S": stage_S, "M1": stage_M1, "M2": stage_M2, "Q0": stage_Q0, "Q": stage_Q, "P": stage_P}
    return nc, cx, stages


def _c(a):
    return np.ascontiguousarray(a, dtype=np.float32)


def prep_shared(p):
    s = {}
    tile128 = lambda v: _c(np.broadcast_to(v[None, :], (128, v.shape[0])))
    s["g1b"] = tile128(p["norm_mix"])
    s["g2b"] = tile128(p["norm_ffn"])
    s["gfb"] = tile128(p["norm_final"])
    s["w_in_r"] = _c(p["w_in"].reshape(16, 128, 56, 128).transpose(2, 1, 0, 3).reshape(56, 128, 2048))
    s["bgate"] = _c(p["b_gate"].reshape(32, 128).T)
    s["cw"] = _c(p["conv_w_dw"].reshape(KCONV, CT, 128).transpose(2, 1, 0))
    s["cb"] = _c(p["conv_b_dw"].reshape(CT, 128).T)
    s["lng"] = _c(p["conv_ln_g"].reshape(CT, 128).T)
    s["lnb"] = _c(p["conv_ln_b"].reshape(CT, 128).T)
    s["cwo"] = _c(p["conv_w_out"].reshape(CT, 128, D).transpose(1, 0, 2))
    s["wval"] = _c(p["ssm_w_val"].reshape(CT, 128, D).transpose(1, 0, 2))
    s["wgate"] = _c(p["ssm_w_gate"].reshape(CT, 128, D).transpose(1, 0, 2))
    s["wout"] = _c(p["w_out"].reshape(DK, 128, D).transpose(1, 0, 2))
    s["wq"] = _c(p["peer_w_q"].reshape(DK, 128, D).transpose(1, 0, 2))
    s["kT"] = _c(p["peer_sub_keys"].reshape(16, 128, 128).transpose(2, 0, 1))
    s["UTr"] = _c(p["peer_u"].reshape(128, 128, 16, 128).transpose(1, 3, 2, 0).reshape(128, 128, 2048))
    s["Vr"] = _c(p["peer_v"].reshape(128, 128, D).transpose(1, 0, 2))
    def st_layout(a):
        return a.reshape(32, 2, 64).transpose(1, 2, 0).reshape(128, 32)
    ldt = np.broadcast_to(p["ssm_log_dt"][:, None], (64, 64))
    s["sA"] = _c(np.stack([st_layout(p["ssm_a_re"]), st_layout(p["ssm_a_im"]), st_layout(ldt)], axis=1))
    def b_layout_rep(a):
        x = a.reshape(8, 8, 1, 64)
        x = np.broadcast_to(x, (8, 8, 16, 64))
        return x.transpose(1, 2, 0, 3).reshape(128, 8 * 64)
    s["sB"] = _c(np.stack([b_layout_rep(p["ssm_a_re"]), b_layout_rep(p["ssm_a_im"]), b_layout_rep(ldt)], axis=1))
    def bT_layout(b):
        return b.reshape(8, 8, 64, 16).transpose(1, 3, 0, 2).reshape(128, 8 * 64)
    s["bT"] = _c(np.stack([bT_layout(p["ssm_b_re"]), bT_layout(p["ssm_b_im"])], axis=1))
    def cT_layout(c):
        o = np.zeros((2, 64, 32, 128), np.float32)
        cc = c.reshape(32, 2, 16, 64)
        for j in range(32):
            for g2 in range(2):
                col0 = 32 * (j % 4) + 16 * g2
                o[g2, :, j, col0:col0 + 16] = cc[j, g2].T
        return o.reshape(128, 32 * 128)
    s["cTp"] = _c(np.stack([cT_layout(p["ssm_c_re"]), cT_layout(p["ssm_c_im"])], axis=1))
    s["dsk"] = _c(p["ssm_d"].reshape(CT, 128).T)
    mB = np.zeros((128, 128), np.float32)
    for q in range(128):
        gl = q // 16
        mB[q, (gl % 2) * 64:(gl % 2) * 64 + 64] = 1.0
    s["maskB"] = mB
    rm = np.zeros((128, 1), np.float32)
    rm[96:] = 1.0
    s["rowm"] = rm
    s["ident"] = np.eye(128, dtype=np.float32)
    s["onesc"] = np.full((128, 128), 1.0 / CW, np.float32)
    s["iota"] = _c(np.broadcast_to(np.arange(128, dtype=np.float32)[None, :], (128, 128)))
    return s


_CACHE = {}


def kernel(**inputs):
    x = np.asarray(inputs["x"], dtype=np.float32)
    B, S, _ = x.shape
    NT = S // 2
    p = {}
    for k, v in inputs.items():
        if k == "x":
            continue
        v = np.asarray(v, dtype=np.float32)
        p[k] = v if k == "norm_final" else v[0]
    shared = prep_shared(p)
    if NT not in _CACHE:
        nc, cx, stages = build(NT)
        for name in ("A", "C", "S", "M1", "M2", "Q0", "Q", "P"):
            stages[name]()
        _CACHE[NT] = nc
    nc = _CACHE[NT]
    in_maps = []
    for c in range(NCORES):
        b, half = c // 2, c % 2
        own = x[b, half * NT:(half + 1) * NT]
        prev = x[b, 0:NT] if half == 1 else np.zeros_like(own)
        m = dict(shared)
        m["xin"] = _c(np.concatenate([prev, own], axis=0))
        in_maps.append(m)
    res = run_bass_kernel_spmd(nc, in_maps, core_ids=list(range(NCORES)))
    outp = np.empty((B, S, D), np.float32)
    for c in range(NCORES):
        b, half = c // 2, c % 2
        outp[b, half * NT:(half + 1) * NT] = res.results[c]["out"]
    return outp
```

```python
import math
import numpy as np
import concourse.bass as bass
import concourse.mybir as mybir
from concourse.bass_utils import run_bass_kernel_spmd

F32 = mybir.dt.float32
BF16 = mybir.dt.bfloat16
U32 = mybir.dt.uint32
AF = mybir.ActivationFunctionType
ALU = mybir.AluOpType
AX = mybir.AxisListType

D = 2048
DK = 16
CW = 1024
CT = 8
KCONV = 31
NSEQ = 4096
NCORES = 8
RMS_EPS = 1e-6
LN_EPS = 1e-5
NEG = -1.0e30


class Buf:
    __slots__ = ("name", "w", "r")

    def __init__(self, name=""):
        self.name = name
        self.w = None
        self.r = []


class Op:
    __slots__ = ("eng", "fn", "deps", "signal", "tok", "dma_key")

    def __init__(self, eng, fn, dma_key):
        self.eng = eng
        self.fn = fn
        self.deps = []
        self.signal = False
        self.tok = None
        self.dma_key = dma_key


class Ctx:
    ENGS = ("pe", "dve", "act", "pool", "sp")

    def __init__(self, nc):
        self.nc = nc
        self.e = {"pe": nc.tensor, "dve": nc.vector, "act": nc.scalar, "pool": nc.gpsimd, "sp": nc.sync}
        self.ops = []
        self.gen = 0
        self.sem = {k: nc.semaphore("sem_" + k).__enter__() for k in self.ENGS if k != "sp"}
        self.cnt = {k: 0 for k in self.ENGS}
        self.seen = {k: {} for k in self.ENGS}
        self.dsem = {}
        self.dcnt = {}
        self.bufs = []
        self.nops = 0

    def buf(self, name=""):
        b = Buf(name)
        self.bufs.append(b)
        return b

    def add(self, eng, fn, reads=(), writes=(), dma_key=None, chain=False):
        op = Op(eng, fn, dma_key)
        deps = set()
        for b in reads:
            if b.w is not None:
                deps.add(b.w)
        for b in writes:
            if b.w is not None:
                if not (chain and b.w.eng == eng and b.w.dma_key is None and dma_key is None):
                    deps.add(b.w)
            for o in b.r:
                deps.add(o)
        deps.discard(op)
        op.deps = list(deps)
        for d in op.deps:
            d.signal = True
        for b in writes:
            b.w = op
            b.r = []
        for b in reads:
            b.r.append(op)
        self.ops.append(op)
        return op

    def dma(self, out, in_, reads=(), writes=(), key="d", q="sp"):
        return self.add(q, lambda e: e.dma_start(out=out, in_=in_), reads, writes, dma_key=key)

    def dma_k(self, sb_t, dram2d, nk, reads, writes, key, store=False, q="sp"):
        for k in range(nk):
            if store:
                self.dma(dram2d[k * 128:(k + 1) * 128, :], sb_t[:, k, :], reads, writes, key=key, q=q)
            else:
                self.dma(sb_t[:, k, :], dram2d[k * 128:(k + 1) * 128, :], reads, writes, key=key)

    def _wait(self, eng, semkey, sem, val):
        s = self.seen[eng]
        if s.get(semkey, 0) >= val:
            return
        s[semkey] = val
        self.e[eng].wait_ge(sem, val)

    def flush(self):
        last = {}
        for op in self.ops:
            if op.dma_key is None:
                last[op.eng] = op
        for op in last.values():
            op.signal = True
        for op in self.ops:
            eng = op.eng
            for d in op.deps:
                if d.dma_key is not None:
                    k = d.dma_key
                    self._wait(eng, "D" + k, self.dsem[k], self.dcnt[k])
                else:
                    if d.eng == "pe" and eng == "pe" and op.dma_key is None:
                        continue
                    self._wait(eng, d.eng, self.sem[d.eng], d.tok)
            ins = op.fn(self.e[eng])
            self.nops += 1
            if op.dma_key is not None:
                k = op.dma_key
                if k not in self.dsem:
                    self.dsem[k] = self.nc.semaphore("dsem_" + k).__enter__()
                    self.dcnt[k] = 0
                self.dcnt[k] += 16
                ins.then_inc(self.dsem[k], 16)
                op.tok = self.dcnt[k]
            elif op.signal:
                self.cnt[eng] += 1
                ins.then_inc(self.sem[eng], 1)
                op.tok = self.cnt[eng]
        for eng in self.ENGS:
            for k in self.dsem:
                if self.dcnt[k] > 0:
                    self._wait(eng, "D" + k, self.dsem[k], self.dcnt[k])
            for o in self.ENGS:
                if o != eng and self.cnt[o] > 0:
                    self._wait(eng, o, self.sem[o], self.cnt[o])
        self.ops = []
        for b in self.bufs:
            b.w = None
            b.r = []
        self.gen += 1
        self.sem = {k: self.nc.semaphore(f"sem_{k}_{self.gen}").__enter__() for k in self.ENGS if k != "sp"}
        self.cnt = {k: 0 for k in self.ENGS}
        for e in self.ENGS:
            for k in self.ENGS:
                self.seen[e].pop(k, None)


class SB:
    def __init__(self, nc):
        self.nc = nc
        self.guards = []

    def sb(self, name, shape, dt):
        g = self.nc.sbuf_tensor(name, list(shape), dt)
        t = g.__enter__()
        self.guards.append(g)
        return t

    def ps(self, name, shape, dt):
        g = self.nc.psum_tensor(name, list(shape), dt)
        t = g.__enter__()
        self.guards.append(g)
        return t

    def release(self):
        for g in reversed(self.guards):
            g.__exit__(None, None, None)
        self.guards = []


def build(NT, debug=False, iso=False, big=True):
    NALL = 2 * NT
    NG = NALL // 512
    NGO = NT // 512
    nc = bass.Bass("TRN2", target_bir_lowering=False)
    cx = Ctx(nc)

    def din(name, shape, dt=F32):
        return nc.dram_tensor(name, list(shape), dt, kind="ExternalInput").ap()

    def dscr(name, shape, dt=F32):
        if iso:
            w = nc.dram_tensor(name, list(shape), dt, kind="ExternalOutput").ap()
            r = nc.dram_tensor(name + "_in", list(shape), dt, kind="ExternalInput").ap()
            return r, w
        kind = "ExternalOutput" if debug else "Internal"
        a = nc.dram_tensor(name, list(shape), dt, kind=kind).ap()
        return a, a

    xin = din("xin", [NALL, D])
    g1b = din("g1b", [128, D])
    g2b = din("g2b", [128, D])
    gfb = din("gfb", [128, D])
    w_in_r = din("w_in_r", [56, 128, DK * 128])
    bgate = din("bgate", [128, 32])
    cw = din("cw", [128, CT, KCONV])
    cb = din("cb", [128, CT])
    lng = din("lng", [128, CT])
    lnb = din("lnb", [128, CT])
    cwo = din("cwo", [128, CT, D])
    wval = din("wval", [128, CT, D])
    wgate = din("wgate", [128, CT, D])
    wout = din("wout", [128, DK, D])
    wq = din("wq", [128, DK, D])
    kT = din("kT", [128, 16, 128])
    UTr = din("UTr", [128, 128, DK * 128] if big else [1, 1, 1])
    Vr = din("Vr", [128, 128, D] if big else [1, 1, 1])
    sA = din("sA", [128, 3, 32])
    sB = din("sB", [128, 3, CT * 64])
    bT = din("bT", [128, 2, CT * 64])
    cTp = din("cTp", [128, 2, 32 * 128])
    dsk = din("dsk", [128, CT])
    maskB = din("maskB", [128, 128])
    rowm = din("rowm", [128, 1])
    ident = din("ident", [128, 128])
    onesc = din("onesc", [128, 128])
    iota = din("iota", [128, 128])

    out = nc.dram_tensor("out", [NT, D], F32, kind="ExternalOutput").ap()

    gluT_r, gluT_w = dscr("gluT", [CW, NT + 512])
    ssmT_r, ssmT_w = dscr("ssmT", [CW, NALL])
    gateT_r, gateT_w = dscr("gateT", [2 * D, NT])
    mconvT_r, mconvT_w = dscr("mconvT", [D, NT])
    zT_r, zT_w = dscr("zT", [CW, NT], BF16)
    mergedT_r, mergedT_w = dscr("mergedT", [D, NT], BF16)
    x1s_r, x1s_w = dscr("x1s", [NT, D])
    hn2T_r, hn2T_w = dscr("hn2T", [D, NT], BF16)
    Mscr_r, Mscr_w = dscr("Mscr", [NGO, 16, 128, 512 * 8], BF16)
    qTs_r, qTs_w = dscr("qTs", [D, NT])

    PS = [nc.psum_tensor(f"ps{i}", [128, 512], F32).__enter__() for i in range(8)]
    PSB = [cx.buf(f"ps{i}") for i in range(8)]

    def psbufs():
        return [cx.buf(f"ps{i}") for i in range(8)]

    def rmsnorm_T(sb, xt_ap, xb, gb_t, gb_b, hn, hn_b, junk, junk_b, st, st_b, identb, identb_b, psb, ps_i, dst_fn, dst_b):
        ss, rs = st[:, 0:1], st[:, 1:2]
        cx.add("act", lambda e: e.activation(out=junk[:], in_=xt_ap, func=AF.Square, accum_out=ss), [xb], [junk_b, st_b])
        cx.add("act", lambda e: e.activation(out=rs, in_=ss, func=AF.Sqrt, scale=1.0 / D, bias=eps_rms[:, 0:1]), [st_b], [st_b])
        cx.add("dve", lambda e: e.reciprocal(out=rs, in_=rs), [st_b], [st_b])
        cx.add("dve", lambda e: e.scalar_tensor_tensor(out=hn[:], in0=xt_ap, scalar=rs, in1=gb_t[:], op0=ALU.mult, op1=ALU.mult),
               [xb, st_b, gb_b], [hn_b])
        for half in range(2):
            pst = PS[ps_i + half].bitcast(BF16)
            for kk in range(8):
                k = half * 8 + kk
                cx.add("pe", lambda e, k=k, kk=kk, pst=pst: e.transpose(out=pst[:, kk * 128:(kk + 1) * 128], in_=hn[:, k * 128:(k + 1) * 128], identity=identb[:]),
                       [hn_b, identb_b], [psb[ps_i + half]])
            dst = dst_fn(half * 8, half * 8 + 8)
            cx.add("act", lambda e, pst=pst, dst=dst: e.copy(out=dst, in_=pst[:].rearrange("p (k t) -> p k t", k=8)),
                   [psb[ps_i + half]], [dst_b])

    def gelu_tanh(src_ap, src_b, shape, tmp1, tmp1_b, tmp2, tmp2_b, out_ap, out_b, extra_mul=None, extra_b=None):
        cx.add("act", lambda e: e.activation(out=tmp1, in_=src_ap, func=AF.Square), [src_b], [tmp1_b])
        cx.add("dve", lambda e: e.tensor_scalar(out=tmp1, in0=tmp1, scalar1=0.044715, scalar2=1.0, op0=ALU.mult, op1=ALU.add), [tmp1_b], [tmp1_b])
        cx.add("dve", lambda e: e.tensor_tensor(out=tmp1, in0=tmp1, in1=src_ap, op=ALU.mult), [tmp1_b, src_b], [tmp1_b])
        cx.add("act", lambda e: e.activation(out=tmp2, in_=tmp1, func=AF.Sigmoid, scale=1.5957691216057308), [tmp1_b], [tmp2_b])
        if extra_mul is None:
            cx.add("dve", lambda e: e.tensor_tensor(out=out_ap, in0=tmp2, in1=src_ap, op=ALU.mult), [tmp2_b, src_b], [out_b])
        else:
            cx.add("dve", lambda e: e.tensor_tensor(out=tmp2, in0=tmp2, in1=src_ap, op=ALU.mult), [tmp2_b, src_b], [tmp2_b])
            cx.add("dve", lambda e: e.tensor_tensor(out=out_ap, in0=tmp2, in1=extra_mul, op=ALU.mult), [tmp2_b, extra_b], [out_b])

    def load_cast(sb_stage, stage_bufs, counter, dram_ap, dst_ap, dst_b, n, key, cast_eng="pool"):
        s = counter[0] % 2
        counter[0] += 1
        cx.dma(dst_ap, dram_ap, [], [dst_b], key=f"{key}{s}", q="pool")

    eps_rms = nc.sbuf_tensor("eps_rms", [128, 1], F32).__enter__()
    eps_ln = nc.sbuf_tensor("eps_ln", [128, 1], F32).__enter__()
    ident_f = nc.sbuf_tensor("ident_f", [128, 128], F32).__enter__()
    ident_b = nc.sbuf_tensor("ident_b", [128, 128], BF16).__enter__()
    cb0 = cx.buf("const")
    cx.add("dve", lambda e: e.memset(eps_rms[:], RMS_EPS), [], [cb0])
    cx.add("dve", lambda e: e.memset(eps_ln[:], LN_EPS), [], [cb0])
    cx.dma(ident_f[:], ident, [], [cb0], key="c0")
    cx.add("dve", lambda e: e.tensor_copy(out=ident_b[:], in_=ident_f[:]), [cb0], [cb0])
    cx.flush()

    def stage_A():
        sb = SB(nc)
        psb = psbufs()
        constb = cx.buf("constA")
        hnT = sb.sb("A_hnT", [128, DK, NALL], BF16)
        hnTb = [cx.buf(f"hnT{g}") for g in range(NG)]
        gb_t = sb.sb("A_gb", [128, D], F32)
        gbb = cx.buf("gb")
        cx.dma(gb_t[:], g1b, [], [gbb], key="c0")
        bg_t = sb.sb("A_bg", [128, 32], F32)
        cx.dma(bg_t[:], bgate, [], [constb], key="c0")
        xts = [sb.sb(f"A_xt{i}", [128, D], F32) for i in range(2)]
        xtb = [cx.buf(f"xt{i}") for i in range(2)]
        hn = sb.sb("A_hn", [128, D], BF16)
        hn_b = cx.buf("hn")
        junk = sb.sb("A_junk", [128, D], BF16)
        junk_b = cx.buf("junk")
        sts = sb.sb("A_st", [128, 2], F32)
        st_b = cx.buf("st")
        idb = cx.buf("identb")
        for tt in range(NALL // 128):
            s = tt % 2
            cx.dma(xts[s][:], xin[tt * 128:(tt + 1) * 128, :], [], [xtb[s]], key=f"xt{s}")
            g = tt // 4
            rmsnorm_T(sb, xts[s][:], xtb[s], gb_t, gbb, hn, hn_b, junk, junk_b, sts, st_b, ident_b, idb, psb, 0,
                      lambda k0, k1, tt=tt: hnT[:, k0:k1, tt * 128:(tt + 1) * 128], hnTb[g])
        NW = 3
        wbf = [sb.sb(f"A_wbf{i}", [128, DK * 128], BF16) for i in range(NW)]
        wbfb = [cx.buf(f"wbf{i}") for i in range(NW)]
        ngc = NGO + 1
        abuf = sb.sb("A_abuf", [128, ngc, 512], F32)
        abufb = [cx.buf(f"abuf{i}") for i in range(ngc)]
        evs = [sb.sb(f"A_ev{i}", [128, 512], F32) for i in range(3)]
        evb = [cx.buf(f"ev{i}") for i in range(3)]
        sgs = [sb.sb(f"A_sg{i}", [128, 512], F32) for i in range(2)]
        sgb = [cx.buf(f"sg{i}") for i in range(2)]
        cnt = {"w": 0, "ps": 0, "ev": 0, "sg": 0}
        conv_groups = list(range(NG // 2 - 1, NG))
        own_groups = list(range(NG // 2, NG))
        order = []
        for c in range(8):
            order.append((c, "a", conv_groups))
            order.append((c + 8, "g", conv_groups))
        for c in range(16, 24):
            order.append((c, "s", list(range(NG))))
        for c in range(24, 56):
            order.append((c, "t", own_groups))
        for (ct, kind, groups) in order:
            ws = cnt["w"] % NW
            cnt["w"] += 1
            cx.dma(wbf[ws][:], w_in_r[ct], [], [wbfb[ws]], key=f"wbf{ws}", q="pool")
            for gi, g in enumerate(groups):
                pi = 2 + cnt["ps"] % 4
                cnt["ps"] += 1
                for k in range(DK):
                    cx.add("pe", lambda e, ws=ws, k=k, g=g, pi=pi: e.matmul(PS[pi][:], lhsT=wbf[ws][:, k * 128:(k + 1) * 128], rhs=hnT[:, k, g * 512:(g + 1) * 512],
                                                                            start=(k == 0), stop=(k == DK - 1)),
                           [wbfb[ws], hnTb[g]], [psb[pi]])
                if kind == "a":
                    cx.add("act", lambda e, gi=gi, pi=pi: e.copy(out=abuf[:, gi, :], in_=PS[pi][:]), [psb[pi]], [abufb[gi]])
                elif kind == "g":
                    c = ct - 8
                    si = cnt["sg"] % 2
                    cnt["sg"] += 1
                    ei = cnt["ev"] % 3
                    cnt["ev"] += 1
                    cx.add("act", lambda e, si=si, pi=pi: e.activation(out=sgs[si][:], in_=PS[pi][:], func=AF.Sigmoid), [psb[pi]], [sgb[si]])
                    cx.add("dve", lambda e, si=si, ei=ei, gi=gi: e.tensor_tensor(out=evs[ei][:], in0=abuf[:, gi, :], in1=sgs[si][:], op=ALU.mult),
                           [abufb[gi], sgb[si]], [evb[ei]])
                    cx.dma(gluT_w[c * 128:(c + 1) * 128, gi * 512:(gi + 1) * 512], evs[ei][:], [evb[ei]], [], key=f"ev{ei}", q="pool")
                elif kind == "s":
                    c = ct - 16
                    ei = cnt["ev"] % 3
                    cnt["ev"] += 1
                    cx.add("act", lambda e, ei=ei, pi=pi: e.copy(out=evs[ei][:], in_=PS[pi][:]), [psb[pi]], [evb[ei]])
                    cx.dma(ssmT_w[c * 128:(c + 1) * 128, g * 512:(g + 1) * 512], evs[ei][:], [evb[ei]], [], key=f"ev{ei}", q="pool")
                else:
                    c = ct - 24
                    ei = cnt["ev"] % 3
                    cnt["ev"] += 1
                    cx.add("act", lambda e, ei=ei, pi=pi, c=c: e.activation(out=evs[ei][:], in_=PS[pi][:], func=AF.Sigmoid, bias=bg_t[:, c:c + 1]),
                           [psb[pi], constb], [evb[ei]])
                    go = g - NG // 2
                    cx.dma(gateT_w[c * 128:(c + 1) * 128, go * 512:(go + 1) * 512], evs[ei][:], [evb[ei]], [], key=f"ev{ei}", q="pool")
        cx.flush()
        sb.release()

    def stage_C():
        sb = SB(nc)
        psb = psbufs()
        cb_ = cx.buf("constC")
        cw_t = sb.sb("C_cw", [128, CT, KCONV], F32)
        cb_t = sb.sb("C_cb", [128, CT], F32)
        lng_t = sb.sb("C_lng", [128, CT], F32)
        lnb_t = sb.sb("C_lnb", [128, CT], F32)
        ones_t = sb.sb("C_ones", [128, 128], F32)
        for t, d in ((cw_t, cw), (cb_t, cb), (lng_t, lng), (lnb_t, lnb), (ones_t, onesc)):
            cx.dma(t[:], d, [], [cb_], key="c0")
        cwo_bf = sb.sb("C_cwo", [128, CT, D], BF16)
        cwob = cx.buf("cwo")
        stg = [sb.sb(f"C_stg{i}", [128, D], F32) for i in range(2)]
        stgb = [cx.buf(f"stg{i}") for i in range(2)]
        ctr = [0]
        for c in range(CT):
            load_cast(stg, stgb, ctr, cwo[:, c, :], cwo_bf[:, c, :], cwob, D, "stg")
        gts = [sb.sb(f"C_gt{i}", [128, 512 + 32], F32) for i in range(2)]
        gtb = [cx.buf(f"gt{i}") for i in range(2)]
        gth = [sb.sb(f"C_gth{i}", [128, 512 + 32], BF16) for i in range(2)]
        gthb = [cx.buf(f"gth{i}") for i in range(2)]
        dg = sb.sb("C_dg", [128, CT, KCONV, 128], BF16)
        dgb = cx.buf("dg")
        idf = ident_f[:].unsqueeze(1).to_broadcast([128, KCONV, 128])
        for c in range(CT):
            cx.add("dve", lambda e, c=c: e.tensor_tensor(out=dg[:, c, :, :], in0=idf, in1=cw_t[:, c, :].unsqueeze(2).to_broadcast([128, KCONV, 128]), op=ALU.mult),
                   [cb_], [dgb], chain=True)
        y = sb.sb("C_y", [128, CT, 512], F32)
        yb = [cx.buf(f"y{c}") for c in range(CT)]
        ysq = sb.sb("C_ysq", [128, CT, 512], F32)
        ysqb = [cx.buf(f"ysq{c}") for c in range(CT)]
        mean_t = sb.sb("C_mean", [128, 512], F32)
        rstd_t = sb.sb("C_rstd", [128, 512], F32)
        stb = cx.buf("stats")
        zt = [sb.sb(f"C_z{i}", [128, 512], F32) for i in range(2)]
        ztb = [cx.buf(f"z{i}") for i in range(2)]
        actT = sb.sb("C_act", [128, CT, 512], BF16)
        actb = [cx.buf(f"act{c}") for c in range(CT)]
        g0t = [sb.sb(f"C_g0{i}", [128, 512], F32) for i in range(2)]
        g0b = [cx.buf(f"g0{i}") for i in range(2)]
        mct = [sb.sb(f"C_mc{i}", [128, 512], F32) for i in range(2)]
        mcb = [cx.buf(f"mc{i}") for i in range(2)]
        n = 0
        for gi in range(NGO):
            for c in range(CT):
                s = n % 2
                n += 1
                base = 512 + gi * 512 - 32
                cx.dma(gts[s][:], gluT_r[c * 128:(c + 1) * 128, base:base + 544], [], [gtb[s]], key=f"gt{s}")
                cx.add("act", lambda e, s=s: e.copy(out=gth[s][:], in_=gts[s][:]), [gtb[s]], [gthb[s]])
                pc = 4 + (n % 4)
                for k in range(KCONV):
                    cx.add("pe", lambda e, s=s, c=c, k=k, pc=pc: e.matmul(PS[pc][:], lhsT=dg[:, c, k, :], rhs=gth[s][:, 2 + k:514 + k], start=(k == 0), stop=(k == KCONV - 1)),
                           [dgb, gthb[s]], [psb[pc]])
                cx.add("act", lambda e, c=c, pc=pc: e.activation(out=y[:, c, :], in_=PS[pc][:], func=AF.Identity, bias=cb_t[:, c:c + 1]), [psb[pc], cb_], [yb[c]])
                cx.add("act", lambda e, c=c: e.activation(out=ysq[:, c, :], in_=y[:, c, :], func=AF.Square), [yb[c]], [ysqb[c]])
            for c in range(CT):
                cx.add("pe", lambda e, c=c: e.matmul(PS[0][:], lhsT=ones_t[:], rhs=y[:, c, :], start=(c == 0), stop=(c == CT - 1)), [cb_, yb[c]], [psb[0]])
            for c in range(CT):
                cx.add("pe", lambda e, c=c: e.matmul(PS[1][:], lhsT=ones_t[:], rhs=ysq[:, c, :], start=(c == 0), stop=(c == CT - 1)), [cb_, ysqb[c]], [psb[1]])
            cx.add("act", lambda e: e.copy(out=mean_t[:], in_=PS[0][:]), [psb[0]], [stb])
            cx.add("dve", lambda e: e.tensor_tensor(out=rstd_t[:], in0=mean_t[:], in1=mean_t[:], op=ALU.mult), [stb], [stb])
            cx.add("dve", lambda e: e.tensor_tensor(out=rstd_t[:], in0=PS[1][:], in1=rstd_t[:], op=ALU.subtract), [stb, psb[1]], [stb])
            cx.add("act", lambda e: e.activation(out=rstd_t[:], in_=rstd_t[:], func=AF.Sqrt, bias=eps_ln[:, 0:1]), [stb], [stb])
            cx.add("dve", lambda e: e.reciprocal(out=rstd_t[:], in_=rstd_t[:]), [stb], [stb])
            for c in range(CT):
                s = c % 2
                cx.add("dve", lambda e, s=s, c=c: e.tensor_tensor(out=zt[s][:], in0=y[:, c, :], in1=mean_t[:], op=ALU.subtract), [yb[c], stb], [ztb[s]])
                cx.add("dve", lambda e, s=s: e.tensor_tensor(out=zt[s][:], in0=zt[s][:], in1=rstd_t[:], op=ALU.mult), [ztb[s], stb], [ztb[s]])
                cx.add("act", lambda e, s=s, c=c: e.activation(out=actT[:, c, :], in_=zt[s][:], func=AF.Silu, scale=lng_t[:, c:c + 1], bias=lnb_t[:, c:c + 1]),
                       [ztb[s], cb_], [actb[c]])
            for dt in range(DK):
                pi = 2 + dt % 2
                s = dt % 2
                for c in range(CT):
                    cx.add("pe", lambda e, c=c, dt=dt, pi=pi: e.matmul(PS[pi][:], lhsT=cwo_bf[:, c, dt * 128:(dt + 1) * 128], rhs=actT[:, c, :],
                                                                        start=(c == 0), stop=(c == CT - 1)), [cwob, actb[c]], [psb[pi]])
                cx.dma(g0t[s][:], gateT_r[dt * 128:(dt + 1) * 128, gi * 512:(gi + 1) * 512], [], [g0b[s]], key=f"g0{s}")
                cx.add("dve", lambda e, s=s, pi=pi: e.tensor_tensor(out=mct[s][:], in0=PS[pi][:], in1=g0t[s][:], op=ALU.mult), [psb[pi], g0b[s]], [mcb[s]])
                cx.dma(mconvT_w[dt * 128:(dt + 1) * 128, gi * 512:(gi + 1) * 512], mct[s][:], [mcb[s]], [], key=f"mc{s}", q="pool")
        cx.flush()
        sb.release()

    TS = 256
    NCH = NALL // TS

    def stage_S():
        sb = SB(nc)
        sbt = SB(nc)
        psb = psbufs()
        PI = math.pi
        cosT = sb.sb("S_cosT", [128, 32, TS], F32)
        sinT = sb.sb("S_sinT", [128, 32, TS], F32)
        ur = sb.sb("S_ur", [128, 32], F32)
        ui = sb.sb("S_ui", [128, 32], F32)
        rA = sb.sb("S_rA", [128, 32], F32)
        BTr = sb.sb("S_BTr", [128, CT, 128], BF16)
        BTi = sb.sb("S_BTi", [128, CT, 128], BF16)
        BTr3 = sb.sb("S_BTr3", [128, CT, 128], BF16)
        BTi3 = sb.sb("S_BTi3", [128, CT, 128], BF16)
        CTr = sb.sb("S_CTr", [128, 32 * 128], BF16)
        CTi = sb.sb("S_CTi", [128, 32 * 128], BF16)
        dsk_t = sb.sb("S_dsk", [128, CT], F32)

        def lam_bar(pref, src, n):
            t = {k: sbt.sb(f"S_{pref}_{k}", [128, n], F32) for k in ("dt", "ar", "th", "r", "sn", "cs", "lr", "li", "tmp")}
            b = cx.buf(pref)
            raw = sbt.sb(f"S_{pref}_raw", [128, 3, n], F32)
            cx.dma(raw[:], src, [], [b], key="c0")
            cx.add("act", lambda e: e.activation(out=t["dt"][:], in_=raw[:, 2, :], func=AF.Exp), [b], [b])
            cx.add("dve", lambda e: e.tensor_tensor(out=t["ar"][:], in0=raw[:, 0, :], in1=t["dt"][:], op=ALU.mult), [b], [b])
            cx.add("dve", lambda e: e.tensor_tensor(out=t["th"][:], in0=raw[:, 1, :], in1=t["dt"][:], op=ALU.mult), [b], [b])
            cx.add("act", lambda e: e.activation(out=t["r"][:], in_=t["ar"][:], func=AF.Exp), [b], [b])
            for _ in range(5):
                cx.add("dve", lambda e: e.tensor_scalar(out=t["tmp"][:], in0=t["th"][:], scalar1=PI, scalar2=2 * PI, op0=ALU.is_gt, op1=ALU.mult), [b], [b])
                cx.add("dve", lambda e: e.tensor_tensor(out=t["th"][:], in0=t["th"][:], in1=t["tmp"][:], op=ALU.subtract), [b], [b])
            cx.add("act", lambda e: e.activation(out=t["sn"][:], in_=t["th"][:], func=AF.Sin), [b], [b])
            cx.add("dve", lambda e: e.tensor_scalar(out=t["cs"][:], in0=t["th"][:], scalar1=PI / 2, scalar2=None, op0=ALU.add), [b], [b])
            cx.add("dve", lambda e: e.tensor_scalar(out=t["tmp"][:], in0=t["cs"][:], scalar1=PI, scalar2=2 * PI, op0=ALU.is_gt, op1=ALU.mult), [b], [b])
            cx.add("dve", lambda e: e.tensor_tensor(out=t["tmp"][:], in0=t["cs"][:], in1=t["tmp"][:], op=ALU.subtract), [b], [b])
            cx.add("act", lambda e: e.activation(out=t["cs"][:], in_=t["tmp"][:], func=AF.Sin), [b], [b])
            cx.add("dve", lambda e: e.tensor_tensor(out=t["lr"][:], in0=t["r"][:], in1=t["cs"][:], op=ALU.mult), [b], [b])
            cx.add("dve", lambda e: e.tensor_tensor(out=t["li"][:], in0=t["r"][:], in1=t["sn"][:], op=ALU.mult), [b], [b])
            t["raw"] = raw
            return t, b

        def cmul(o_r, o_i, a_r, a_i, b_r, b_i, t1, t2, bufs_r, bufs_w, eng="dve"):
            cx.add(eng, lambda e: e.tensor_tensor(out=t1, in0=a_r, in1=b_r, op=ALU.mult), bufs_r, bufs_w)
            cx.add(eng, lambda e: e.tensor_tensor(out=t2, in0=a_i, in1=b_i, op=ALU.mult), bufs_r, bufs_w)
            cx.add(eng, lambda e: e.tensor_tensor(out=o_r, in0=t1, in1=t2, op=ALU.subtract), bufs_r, bufs_w)
            cx.add(eng, lambda e: e.tensor_tensor(out=t1, in0=a_r, in1=b_i, op=ALU.mult), bufs_r, bufs_w)
            cx.add(eng, lambda e: e.tensor_tensor(out=t2, in0=a_i, in1=b_r, op=ALU.mult), bufs_r, bufs_w)
            cx.add(eng, lambda e: e.tensor_tensor(out=o_i, in0=t1, in1=t2, op=ALU.add), bufs_r, bufs_w)

        A, Ab = lam_bar("A", sA, 32)
        tb = cx.buf("tables")
        ur2 = sbt.sb("S_ur2", [128, 32], F32)
        ui2 = sbt.sb("S_ui2", [128, 32], F32)
        tt1 = sbt.sb("S_tt1", [128, 32, TS // 2], F32)
        tt2 = sbt.sb("S_tt2", [128, 32, TS // 2], F32)
        cx.add("dve", lambda e: e.tensor_copy(out=rA[:], in_=A["r"][:]), [Ab], [tb])
        cx.add("dve", lambda e: e.memset(cosT[:, :, 0:1], 1.0), [], [tb])
        cx.add("dve", lambda e: e.memset(sinT[:, :, 0:1], 0.0), [], [tb])
        cx.add("dve", lambda e: e.tensor_copy(out=ur[:], in_=A["cs"][:]), [Ab], [tb])
        cx.add("dve", lambda e: e.tensor_copy(out=ui[:], in_=A["sn"][:]), [Ab], [tb])
        m = 1
        while m < TS:
            urb = ur[:].unsqueeze(2).to_broadcast([128, 32, m])
            uib = ui[:].unsqueeze(2).to_broadcast([128, 32, m])
            cmul(cosT[:, :, m:2 * m], sinT[:, :, m:2 * m], cosT[:, :, 0:m], sinT[:, :, 0:m], urb, uib, tt1[:, :, 0:m], tt2[:, :, 0:m], [tb], [tb])
            cmul(ur2[:], ui2[:], ur[:], ui[:], ur[:], ui[:], tt1[:, :, 0], tt2[:, :, 0], [tb], [tb])
            cx.add("dve", lambda e: e.tensor_copy(out=ur[:], in_=ur2[:]), [tb], [tb])
            cx.add("dve", lambda e: e.tensor_copy(out=ui[:], in_=ui2[:]), [tb], [tb])
            m *= 2
        Bp, Bb = lam_bar("B", sB, CT * 64)
        nB = CT * 64
        braw = sbt.sb("S_braw", [128, 2, nB], F32)
        cx.dma(braw[:], bT, [], [Bb], key="c0")
        tB = {k: sbt.sb(f"S_tB_{k}", [128, nB], F32) for k in ("nr", "den", "cr", "ci", "t1", "t2", "br", "bi")}
        cx.add("dve", lambda e: e.tensor_scalar(out=tB["nr"][:], in0=Bp["lr"][:], scalar1=-1.0, scalar2=None, op0=ALU.add), [Bb], [Bb])
        are, aim = Bp["raw"][:, 0, :], Bp["raw"][:, 1, :]
        cx.add("dve", lambda e: e.tensor_tensor(out=tB["den"][:], in0=are, in1=are, op=ALU.mult), [Bb], [Bb])
        cx.add("dve", lambda e: e.tensor_tensor(out=tB["t1"][:], in0=aim, in1=aim, op=ALU.mult), [Bb], [Bb])
        cx.add("dve", lambda e: e.tensor_tensor(out=tB["den"][:], in0=tB["den"][:], in1=tB["t1"][:], op=ALU.add), [Bb], [Bb])
        cx.add("dve", lambda e: e.reciprocal(out=tB["den"][:], in_=tB["den"][:]), [Bb], [Bb])
        cx.add("dve", lambda e: e.tensor_tensor(out=tB["t1"][:], in0=tB["nr"][:], in1=are, op=ALU.mult), [Bb], [Bb])
        cx.add("dve", lambda e: e.tensor_tensor(out=tB["t2"][:], in0=Bp["li"][:], in1=aim, op=ALU.mult), [Bb], [Bb])
        cx.add("dve", lambda e: e.tensor_tensor(out=tB["cr"][:], in0=tB["t1"][:], in1=tB["t2"][:], op=ALU.add), [Bb], [Bb])
        cx.add("dve", lambda e: e.tensor_tensor(out=tB["t1"][:], in0=Bp["li"][:], in1=are, op=ALU.mult), [Bb], [Bb])
        cx.add("dve", lambda e: e.tensor_tensor(out=tB["t2"][:], in0=tB["nr"][:], in1=aim, op=ALU.mult), [Bb], [Bb])
        cx.add("dve", lambda e: e.tensor_tensor(out=tB["ci"][:], in0=tB["t1"][:], in1=tB["t2"][:], op=ALU.subtract), [Bb], [Bb])
        cx.add("dve", lambda e: e.tensor_tensor(out=tB["cr"][:], in0=tB["cr"][:], in1=tB["den"][:], op=ALU.mult), [Bb], [Bb])
        cx.add("dve", lambda e: e.tensor_tensor(out=tB["ci"][:], in0=tB["ci"][:], in1=tB["den"][:], op=ALU.mult), [Bb], [Bb])
        cmul(tB["br"][:], tB["bi"][:], tB["cr"][:], tB["ci"][:], braw[:, 0, :], braw[:, 1, :], tB["t1"][:], tB["t2"][:], [Bb], [Bb])
        mB = sbt.sb("S_maskB", [128, 128], F32)
        cx.dma(mB[:], maskB, [], [Bb], key="c0")
        mBb = mB[:].rearrange("p (g q) -> p g q", g=2).unsqueeze(1).to_broadcast([128, CT, 2, 64])
        for src, dst in ((tB["br"], BTr), (tB["bi"], BTi)):
            sv = src[:].rearrange("p (k q) -> p k q", k=CT).unsqueeze(2).to_broadcast([128, CT, 2, 64])
            cx.add("dve", lambda e, sv=sv, dst=dst: e.tensor_tensor(out=dst[:].rearrange("p k (g q) -> p k g q", g=2), in0=sv, in1=mBb, op=ALU.mult), [Bb], [Bb])
        rm_t = sbt.sb("S_rowm", [128, 1], F32)
        cx.dma(rm_t[:], rowm, [], [Bb], key="c0")
        for src, dst in ((BTr, BTr3), (BTi, BTi3)):
            cx.add("dve", lambda e, src=src, dst=dst: e.tensor_scalar(out=dst[:], in0=src[:], scalar1=rm_t[:, 0:1], scalar2=None, op0=ALU.mult), [Bb], [Bb])
        Cb = cx.buf("C")
        cx.dma(CTr[:], cTp[:, 0, :], [], [Cb], key="cc", q="pool")
        cx.dma(CTi[:], cTp[:, 1, :], [], [Cb], key="cc", q="pool")
        cx.add("pool", lambda e: e.tensor_scalar(out=CTi[:], in0=CTi[:], scalar1=-1.0, scalar2=None, op0=ALU.mult), [Cb], [Cb])
        cx.dma(dsk_t[:], dsk, [], [Cb], key="c0")
        cx.flush()
        sbt.release()
        psb = psbufs()
        zin_r = sb.sb("S_zinr", [128, 32], F32)
        zin_i = sb.sb("S_zini", [128, 32], F32)
        zl_r = sb.sb("S_zlr", [128, 32], F32)
        zl_i = sb.sb("S_zli", [128, 32], F32)
        zc1 = sb.sb("S_zc1", [128, 32], F32)
        zc2 = sb.sb("S_zc2", [128, 32], F32)
        zb = cx.buf("zstate")
        cx.add("dve", lambda e: e.memset(zin_r[:], 0.0), [], [zb])
        cx.add("dve", lambda e: e.memset(zin_i[:], 0.0), [], [zb])
        NTMP = 6
        uts = [sb.sb(f"S_ut{i}", [128, CT, TS], F32) for i in range(2)]
        utb = [cx.buf(f"ut{i}") for i in range(2)]
        utsb = [sb.sb(f"S_utb{i}", [128, CT, TS], BF16) for i in range(2)]
        utbb = [cx.buf(f"utbb{i}") for i in range(2)]
        xrb = [sb.sb(f"S_xrb{i}", [128, TS], BF16) for i in range(NTMP)]
        xib = [sb.sb(f"S_xib{i}", [128, TS], BF16) for i in range(NTMP)]
        xrbb = [cx.buf(f"xrb{i}") for i in range(NTMP)]
        xibb = [cx.buf(f"xib{i}") for i in range(NTMP)]
        tmp = {k: [sb.sb(f"S_{k}{i}", [128, TS], F32) for i in range(NTMP)] for k in ("t1", "t2", "wr", "wi", "zr", "zi", "xr", "xi")}
        tmpb = {k: [cx.buf(f"{k}{i}") for i in range(NTMP)] for k in tmp}
        yv = [sb.sb(f"S_yv{i}", [128, TS], F32) for i in range(2)]
        yvb = [cx.buf(f"yv{i}") for i in range(2)]
        g1 = [sb.sb(f"S_g1{i}", [128, TS], F32) for i in range(2)]
        g1b_ = [cx.buf(f"g1{i}") for i in range(2)]
        g2 = [sb.sb(f"S_g2{i}", [128, TS], F32) for i in range(2)]
        g2b_ = [cx.buf(f"g2{i}") for i in range(2)]
        zo = [sb.sb(f"S_zo{i}", [128, TS], BF16) for i in range(2)]
        zob = [cx.buf(f"zo{i}") for i in range(2)]
        n = 0
        for ci in range(NCH):
            own = ci >= NCH // 2
            us = ci % 2
            cx.dma_k(uts[us], ssmT_r[:, ci * TS:(ci + 1) * TS], CT, [], [utb[us]], f"ut{us}")
            cx.add("act", lambda e, us=us: e.copy(out=utsb[us][:], in_=uts[us][:]), [utb[us]], [utbb[us]])
            for j in range(32):
                k, po = j // 4, 32 * (j % 4)
                s = n % NTMP
                n += 1
                pb = 0 + 2 * (j % 2)
                if po == 96:
                    lr_, li_, p0, p1 = BTr3, BTi3, 64, 128
                else:
                    lr_, li_, p0, p1 = BTr, BTi, po, po + 32
                cx.add("pe", lambda e, k=k, pb=pb, us=us, lr_=lr_, p0=p0, p1=p1: e.matmul(PS[pb][:, 0:TS], lhsT=lr_[p0:p1, k, :], rhs=utsb[us][p0:p1, k, :], start=True, stop=True),
                       [Bb, utbb[us]], [psb[pb]])
                cx.add("pe", lambda e, k=k, pb=pb, us=us, li_=li_, p0=p0, p1=p1: e.matmul(PS[pb + 1][:, 0:TS], lhsT=li_[p0:p1, k, :], rhs=utsb[us][p0:p1, k, :], start=True, stop=True),
                       [Bb, utbb[us]], [psb[pb + 1]])
                bre, bim = PS[pb][:, 0:TS], PS[pb + 1][:, 0:TS]
                cs, sn = cosT[:, j, :], sinT[:, j, :]
                T = {kk: tmp[kk][s][:] for kk in tmp}
                TB = {kk: tmpb[kk][s] for kk in tmp}
                cx.add("dve", lambda e, T=T, bre=bre, cs=cs: e.tensor_tensor(out=T["t1"], in0=bre, in1=cs, op=ALU.mult), [psb[pb], tb], [TB["t1"]])
                cx.add("dve", lambda e, T=T, bim=bim, sn=sn: e.tensor_tensor(out=T["t2"], in0=bim, in1=sn, op=ALU.mult), [psb[pb + 1], tb], [TB["t2"]])
                cx.add("pool", lambda e, T=T: e.tensor_tensor(out=T["wr"], in0=T["t1"], in1=T["t2"], op=ALU.add), [TB["t1"], TB["t2"]], [TB["wr"]])
                cx.add("dve", lambda e, T=T, bim=bim, cs=cs: e.tensor_tensor(out=T["xr"], in0=bim, in1=cs, op=ALU.mult), [psb[pb + 1], tb], [TB["xr"]])
                cx.add("dve", lambda e, T=T, bre=bre, sn=sn: e.tensor_tensor(out=T["xi"], in0=bre, in1=sn, op=ALU.mult), [psb[pb], tb], [TB["xi"]])
                cx.add("pool", lambda e, T=T: e.tensor_tensor(out=T["wi"], in0=T["xr"], in1=T["xi"], op=ALU.subtract), [TB["xr"], TB["xi"]], [TB["wi"]])
                rb = rA[:, j:j + 1].to_broadcast([128, TS])
                cx.add("dve", lambda e, T=T, rb=rb, j=j: e.tensor_tensor_scan(out=T["zr"], data0=rb, data1=T["wr"], initial=zin_r[:, j:j + 1], op0=ALU.mult, op1=ALU.add),
                       [TB["wr"], tb, zb], [TB["zr"]])
                cx.add("dve", lambda e, T=T, rb=rb, j=j: e.tensor_tensor_scan(out=T["zi"], data0=rb, data1=T["wi"], initial=zin_i[:, j:j + 1], op0=ALU.mult, op1=ALU.add),
                       [TB["wi"], tb, zb], [TB["zi"]])
                cx.add("pool", lambda e, T=T, j=j: e.tensor_copy(out=zl_r[:, j:j + 1], in_=T["zr"][:, TS - 1:TS]), [TB["zr"]], [zb])
                cx.add("pool", lambda e, T=T, j=j: e.tensor_copy(out=zl_i[:, j:j + 1], in_=T["zi"][:, TS - 1:TS]), [TB["zi"]], [zb])
                if own:
                    cx.add("dve", lambda e, T=T, cs=cs: e.tensor_tensor(out=T["t1"], in0=T["zr"], in1=cs, op=ALU.mult), [TB["zr"], tb], [TB["t1"]])
                    cx.add("dve", lambda e, T=T, sn=sn: e.tensor_tensor(out=T["t2"], in0=T["zi"], in1=sn, op=ALU.mult), [TB["zi"], tb], [TB["t2"]])
                    cx.add("pool", lambda e, T=T, s=s: e.tensor_tensor(out=xrb[s][:], in0=T["t1"], in1=T["t2"], op=ALU.subtract), [TB["t1"], TB["t2"]], [xrbb[s]])
                    cx.add("dve", lambda e, T=T, sn=sn: e.tensor_tensor(out=T["wr"], in0=T["zr"], in1=sn, op=ALU.mult), [TB["zr"], tb], [TB["wr"]])
                    cx.add("dve", lambda e, T=T, cs=cs: e.tensor_tensor(out=T["wi"], in0=T["zi"], in1=cs, op=ALU.mult), [TB["zi"], tb], [TB["wi"]])
                    cx.add("pool", lambda e, T=T, s=s: e.tensor_tensor(out=xib[s][:], in0=T["wr"], in1=T["wi"], op=ALU.add), [TB["wr"], TB["wi"]], [xibb[s]])
                    py = 4 + (k % 2)
                    cx.add("pe", lambda e, s=s, j=j, py=py: e.matmul(PS[py][:, 0:TS], lhsT=CTr[:, j * 128:(j + 1) * 128], rhs=xrb[s][:], start=(j % 4 == 0), stop=False),
                           [Cb, xrbb[s]], [psb[py]])
                    cx.add("pe", lambda e, s=s, j=j, py=py: e.matmul(PS[py][:, 0:TS], lhsT=CTi[:, j * 128:(j + 1) * 128], rhs=xib[s][:], start=False, stop=(j % 4 == 3)),
                           [Cb, xibb[s]], [psb[py]])
                    if j % 4 == 3:
                        ys = k % 2
                        cx.add("dve", lambda e, ys=ys, k=k, py=py, us=us: e.scalar_tensor_tensor(out=yv[ys][:], in0=uts[us][:, k, :], scalar=dsk_t[:, k:k + 1], in1=PS[py][:, 0:TS],
                                                                                               op0=ALU.mult, op1=ALU.add), [utb[us], Cb, psb[py]], [yvb[ys]])
                        gelu_tanh(yv[ys][:], yvb[ys], None, g1[ys][:], g1b_[ys], g2[ys][:], g2b_[ys], zo[ys][:], zob[ys])
                        t0 = (ci - NCH // 2) * TS
                        cx.dma(zT_w[k * 128:(k + 1) * 128, t0:t0 + TS], zo[ys][:], [zob[ys]], [], key=f"zo{ys}")
            cmul(zin_r[:], zin_i[:], zl_r[:], zl_i[:], ur[:], ui[:], zc1[:], zc2[:], [zb, tb], [zb])
        cx.flush()
        sb.release()

    def stage_M1():
        sb = SB(nc)
        psb = psbufs()
        wv = sb.sb("M_wv", [128, CT, D], BF16)
        wg = sb.sb("M_wg", [128, CT, D], BF16)
        wb = cx.buf("w")
        stg = [sb.sb(f"M_stg{i}", [128, D], F32) for i in range(2)]
        stgb = [cx.buf(f"stg{i}") for i in range(2)]
        ctr = [0]
        for c in range(CT):
            load_cast(stg, stgb, ctr, wval[:, c, :], wv[:, c, :], wb, D, "stg")
            load_cast(stg, stgb, ctr, wgate[:, c, :], wg[:, c, :], wb, D, "stg", cast_eng="act")
        zts = [sb.sb(f"M_zt{i}", [128, CT, 512], BF16) for i in range(2)]
        ztb = [cx.buf(f"zt{i}") for i in range(2)]
        sg = [sb.sb(f"M_sg{i}", [128, 512], F32) for i in range(2)]
        sgb = [cx.buf(f"sg{i}") for i in range(2)]
        g1t = [sb.sb(f"M_g1{i}", [128, 512], F32) for i in range(2)]
        g1tb = [cx.buf(f"g1{i}") for i in range(2)]
        mct = [sb.sb(f"M_mc{i}", [128, 512], F32) for i in range(2)]
        mctb = [cx.buf(f"mc{i}") for i in range(2)]
        mo = [sb.sb(f"M_mo{i}", [128, 512], BF16) for i in range(2)]
        mob = [cx.buf(f"mo{i}") for i in range(2)]
        for gi in range(NGO):
            zs = gi % 2
            cx.dma_k(zts[zs], zT_r[:, gi * 512:(gi + 1) * 512], CT, [], [ztb[zs]], f"zt{zs}")
            for dt in range(DK):
                s = dt % 2
                pv, pg = 0 + s, 2 + s
                for c in range(CT):
                    cx.add("pe", lambda e, c=c, dt=dt, pv=pv, zs=zs: e.matmul(PS[pv][:], lhsT=wv[:, c, dt * 128:(dt + 1) * 128], rhs=zts[zs][:, c, :], start=(c == 0), stop=(c == CT - 1)),
                           [wb, ztb[zs]], [psb[pv]])
                for c in range(CT):
                    cx.add("pe", lambda e, c=c, dt=dt, pg=pg, zs=zs: e.matmul(PS[pg][:], lhsT=wg[:, c, dt * 128:(dt + 1) * 128], rhs=zts[zs][:, c, :], start=(c == 0), stop=(c == CT - 1)),
                           [wb, ztb[zs]], [psb[pg]])
                cx.dma(g1t[s][:], gateT_r[D + dt * 128:D + (dt + 1) * 128, gi * 512:(gi + 1) * 512], [], [g1tb[s]], key=f"g1{s}")
                cx.dma(mct[s][:], mconvT_r[dt * 128:(dt + 1) * 128, gi * 512:(gi + 1) * 512], [], [mctb[s]], key=f"mc{s}")
                cx.add("act", lambda e, s=s, pg=pg: e.activation(out=sg[s][:], in_=PS[pg][:], func=AF.Sigmoid), [psb[pg]], [sgb[s]])
                cx.add("dve", lambda e, s=s, pv=pv: e.tensor_tensor(out=sg[s][:], in0=PS[pv][:], in1=sg[s][:], op=ALU.mult), [psb[pv], sgb[s]], [sgb[s]])
                cx.add("dve", lambda e, s=s: e.tensor_tensor(out=sg[s][:], in0=sg[s][:], in1=g1t[s][:], op=ALU.mult), [sgb[s], g1tb[s]], [sgb[s]])
                cx.add("dve", lambda e, s=s: e.tensor_tensor(out=mo[s][:], in0=sg[s][:], in1=mct[s][:], op=ALU.add), [sgb[s], mctb[s]], [mob[s]])
                cx.dma(mergedT_w[dt * 128:(dt + 1) * 128, gi * 512:(gi + 1) * 512], mo[s][:], [mob[s]], [], key=f"mo{s}", q="pool")
        cx.flush()
        sb.release()

    def stage_M2():
        sb = SB(nc)
        psb = psbufs()
        wo = sb.sb("O_wo", [128, DK, D], BF16)
        wb = cx.buf("w")
        stg = [sb.sb(f"O_stg{i}", [128, D], F32) for i in range(2)]
        stgb = [cx.buf(f"stg{i}") for i in range(2)]
        ctr = [0]
        for k in range(DK):
            load_cast(stg, stgb, ctr, wout[:, k, :], wo[:, k, :], wb, D, "stg", cast_eng=("pool" if k % 2 else "act"))
        gb_t = sb.sb("O_gb", [128, D], F32)
        gbb = cx.buf("gb")
        cx.dma(gb_t[:], g2b, [], [gbb], key="c0")
        mts = [sb.sb(f"O_mt{i}", [128, DK, 512], BF16) for i in range(2)]
        mtb = [cx.buf(f"mt{i}") for i in range(2)]
        xts = [sb.sb(f"O_xt{i}", [128, D], F32) for i in range(2)]
        xtb = [cx.buf(f"xt{i}") for i in range(2)]
        hn = sb.sb("O_hn", [128, D], BF16)
        hn_b = cx.buf("hn")
        junk = sb.sb("O_junk", [128, D], BF16)
        junk_b = cx.buf("junk")
        sts = sb.sb("O_st", [128, 2], F32)
        st_b = cx.buf("st")
        idb = cx.buf("identb")
        hts = [sb.sb(f"O_ht{i}", [128, DK, 128], BF16) for i in range(2)]
        htb = [cx.buf(f"ht{i}") for i in range(2)]
        n = 0
        for gi in range(NGO):
            ms = gi % 2
            cx.dma_k(mts[ms], mergedT_r[:, gi * 512:(gi + 1) * 512], DK, [], [mtb[ms]], f"mt{ms}")
            for ts_ in range(4):
                s = n % 2
                n += 1
                tok0 = gi * 512 + ts_ * 128
                cx.dma(xts[s][:], xin[NT + tok0:NT + tok0 + 128, :], [], [xtb[s]], key=f"xt{s}")
                for dc in range(4):
                    pi = 2 + dc
                    for k in range(DK):
                        cx.add("pe", lambda e, k=k, dc=dc, pi=pi, ms=ms, ts_=ts_: e.matmul(PS[pi][:], lhsT=mts[ms][:, k, ts_ * 128:(ts_ + 1) * 128], rhs=wo[:, k, dc * 512:(dc + 1) * 512],
                                                                                          start=(k == 0), stop=(k == DK - 1)), [mtb[ms], wb], [psb[pi]])
                    cx.add("dve", lambda e, s=s, dc=dc, pi=pi: e.tensor_tensor(out=xts[s][:, dc * 512:(dc + 1) * 512], in0=PS[pi][:], in1=xts[s][:, dc * 512:(dc + 1) * 512], op=ALU.add),
                           [psb[pi], xtb[s]], [xtb[s]])
                cx.dma(x1s_w[tok0:tok0 + 128, :], xts[s][:], [xtb[s]], [], key=f"x1{s}", q="pool")
                rmsnorm_T(sb, xts[s][:], xtb[s], gb_t, gbb, hn, hn_b, junk, junk_b, sts, st_b, ident_b, idb, psb, 0,
                          lambda k0, k1, s=s: hts[s][:, k0:k1, :], htb[s])
                cx.dma_k(hts[s], hn2T_w[:, tok0:tok0 + 128], DK, [htb[s]], [], f"ht{s}", store=True, q="pool")
        cx.flush()
        sb.release()

    def stage_Q0():
        sb = SB(nc)
        psb = psbufs()
        wq_bf = sb.sb("Q0_wq", [128, DK, D], BF16)
        wb = cx.buf("w")
        stg = [sb.sb(f"Q0_stg{i}", [128, D], F32) for i in range(2)]
        stgb = [cx.buf(f"stg{i}") for i in range(2)]
        ctr = [0]
        for k in range(DK):
            load_cast(stg, stgb, ctr, wq[:, k, :], wq_bf[:, k, :], wb, D, "stg", cast_eng=("pool" if k % 2 else "act"))
        hts = [sb.sb(f"Q0_ht{i}", [128, DK, 512], BF16) for i in range(2)]
        htb = [cx.buf(f"ht{i}") for i in range(2)]
        evs = [sb.sb(f"Q0_ev{i}", [128, 512], F32) for i in range(3)]
        evb = [cx.buf(f"ev{i}") for i in range(3)]
        n = 0
        for gi in range(NGO):
            hs = gi % 2
            cx.dma_k(hts[hs], hn2T_r[:, gi * 512:(gi + 1) * 512], DK, [], [htb[hs]], f"ht{hs}")
            for hc in range(16):
                pi = hc % 4
                ei = n % 3
                n += 1
                for k in range(DK):
                    cx.add("pe", lambda e, k=k, hc=hc, pi=pi, hs=hs: e.matmul(PS[pi][:], lhsT=wq_bf[:, k, hc * 128:(hc + 1) * 128], rhs=hts[hs][:, k, :], start=(k == 0), stop=(k == DK - 1)),
                           [wb, htb[hs]], [psb[pi]])
                cx.add("act", lambda e, ei=ei, pi=pi: e.copy(out=evs[ei][:], in_=PS[pi][:]), [psb[pi]], [evb[ei]])
                cx.dma(qTs_w[hc * 128:(hc + 1) * 128, gi * 512:(gi + 1) * 512], evs[ei][:], [evb[ei]], [], key=f"ev{ei}", q="pool")
        cx.flush()
        sb.release()

    def stage_Q():
        sb = SB(nc)
        psb = psbufs()
        kT_t = sb.sb("Q_kT", [128, 16, 128], F32)
        io_t = sb.sb("Q_iota", [128, 128], F32)
        cb_ = cx.buf("const")
        cx.dma(kT_t[:], kT, [], [cb_], key="c0")
        cx.dma(io_t[:], iota, [], [cb_], key="c0")
        qT = sb.sb("Q_qT", [128, 16, 512], F32)
        qTb1 = cx.buf("qT")
        qTb = [qTb1] * 16
        S = sb.sb("Q_S", [128, 16, 128], F32)
        Sb = cx.buf("S")
        Sw = sb.sb("Q_Sw", [128, 128], F32)
        Swb = cx.buf("Sw")
        v16 = sb.sb("Q_v16", [128, 16, 16], F32)
        i16u = sb.sb("Q_i16u", [128, 16, 16], U32)
        i16 = sb.sb("Q_i16", [128, 16, 16], F32)
        vb = cx.buf("v16")
        cand = sb.sb("Q_cand", [128, 8, 256], F32)
        candw = sb.sb("Q_candw", [128, 256], F32)
        candb = cx.buf("cand")
        candwb = cx.buf("candw")
        best = sb.sb("Q_best", [128, 8, 16], F32)
        posu = sb.sb("Q_posu", [128, 8, 16], U32)
        r1u = sb.sb("Q_r1u", [128, 8, 16], U32)
        r2u = sb.sb("Q_r2u", [128, 8, 16], U32)
        r1f = sb.sb("Q_r1f", [128, 8, 16], F32)
        r2f = sb.sb("Q_r2f", [128, 8, 16], F32)
        bb = cx.buf("best")
        eq = sb.sb("Q_eq", [128, 8, 16, 16], F32)
        eqb = cx.buf("eq")
        IJG = sb.sb("Q_IJG", [128, 3, 128], F32)
        ijgb = cx.buf("ijg")
        zs_ = sb.sb("Q_zs", [128, 8], F32)
        IJGT = sb.sb("Q_IJGT", [128, 3, 128], F32)
        ijgtb = cx.buf("ijgt")
        Aoh = [sb.sb(f"Q_A{i}", [128, 64, 128], BF16) for i in range(2)]
        Boh = [sb.sb(f"Q_B{i}", [128, 64, 128], BF16) for i in range(2)]
        Abs = [cx.buf(f"A{i}") for i in range(2)]
        Bbs = [cx.buf(f"B{i}") for i in range(2)]
        nh = 0
        nta = 0
        nJG = sb.sb("Q_nJG", [128, 2, 128], F32)
        njgb = cx.buf("njg")
        tmpA = [sb.sb(f"Q_tmpA{i}", [128, 128], F32) for i in range(4)]
        tmpAb = [cx.buf(f"tmpA{i}") for i in range(4)]
        Mst = [sb.sb(f"Q_Mst{i}", [128, 16, 128, 8], BF16) for i in range(2)]
        Mstb = [cx.buf(f"Mst{i}") for i in range(2)]
        n = 0
        for gi in range(NGO):
            cx.dma_k(qT, qTs_r[:, gi * 512:(gi + 1) * 512], 16, [], [qTb1], "qT")
            for ts_ in range(4):
                ms = n % 2
                n += 1
                for hc in range(16):
                    pi = hc // 4
                    cx.add("pe", lambda e, hc=hc, pi=pi, ts_=ts_: e.matmul(PS[pi][:, (hc % 4) * 128:(hc % 4 + 1) * 128], lhsT=qT[:, hc, ts_ * 128:(ts_ + 1) * 128], rhs=kT_t[:, hc, :],
                                                                            start=True, stop=True), [qTb[hc], cb_], [psb[pi]])
                for pi in range(4):
                    cx.add("act", lambda e, pi=pi: e.copy(out=S[:, pi * 4:(pi + 1) * 4, :], in_=PS[pi][:].rearrange("p (a n) -> p a n", a=4)), [psb[pi]], [Sb])
                for hc in range(16):
                    cx.add("dve", lambda e, hc=hc: e.max(out=v16[:, hc, 0:8], in_=S[:, hc, :]), [Sb], [vb])
                    cx.add("dve", lambda e, hc=hc: e.max_index(out=i16u[:, hc, 0:8], in_max=v16[:, hc, 0:8], in_values=S[:, hc, :]), [Sb, vb], [vb])
                    cx.add("dve", lambda e, hc=hc: e.match_replace(out=Sw[:], in_to_replace=v16[:, hc, 0:8], in_values=S[:, hc, :], imm_value=NEG), [Sb, vb], [Swb])
                    cx.add("dve", lambda e, hc=hc: e.max(out=v16[:, hc, 8:16], in_=Sw[:]), [Swb], [vb])
                    cx.add("dve", lambda e, hc=hc: e.max_index(out=i16u[:, hc, 8:16], in_max=v16[:, hc, 8:16], in_values=Sw[:]), [Swb, vb], [vb])
                cx.add("dve", lambda e: e.tensor_copy(out=i16[:], in_=i16u[:]), [vb], [vb])
                v4 = v16[:].rearrange("p (h c) k -> p h c k", c=2)
                i4 = i16[:].rearrange("p (h c) k -> p h c k", c=2)
                for h in range(8):
                    cx.add("dve", lambda e, h=h: e.tensor_tensor(out=cand[:, h, :].rearrange("p (a b) -> p a b", a=16),
                                                                  in0=v4[:, h, 0, :].unsqueeze(2).to_broadcast([128, 16, 16]),
                                                                  in1=v4[:, h, 1, :].unsqueeze(1).to_broadcast([128, 16, 16]), op=ALU.add), [vb], [candb])
                    cx.add("dve", lambda e, h=h: e.max(out=best[:, h, 0:8], in_=cand[:, h, :]), [candb], [bb])
                    cx.add("dve", lambda e, h=h: e.max_index(out=posu[:, h, 0:8], in_max=best[:, h, 0:8], in_values=cand[:, h, :]), [candb, bb], [bb])
                    cx.add("dve", lambda e, h=h: e.match_replace(out=candw[:], in_to_replace=best[:, h, 0:8], in_values=cand[:, h, :], imm_value=NEG), [candb, bb], [candwb])
                    cx.add("dve", lambda e, h=h: e.max(out=best[:, h, 8:16], in_=candw[:]), [candwb], [bb])
                    cx.add("dve", lambda e, h=h: e.max_index(out=posu[:, h, 8:16], in_max=best[:, h, 8:16], in_values=candw[:]), [candwb, bb], [bb])
                cx.add("dve", lambda e: e.tensor_single_scalar(out=r1u[:], in_=posu[:], scalar=4, op=ALU.logical_shift_right), [bb], [bb])
                cx.add("dve", lambda e: e.tensor_single_scalar(out=r2u[:], in_=posu[:], scalar=15, op=ALU.bitwise_and), [bb], [bb])
                cx.add("dve", lambda e: e.tensor_copy(out=r1f[:], in_=r1u[:]), [bb], [bb])
                cx.add("dve", lambda e: e.tensor_copy(out=r2f[:], in_=r2u[:]), [bb], [bb])
                io16 = io_t[:, 0:16].unsqueeze(1).unsqueeze(1).to_broadcast([128, 8, 16, 16])
                for (rf, cidx, slot) in ((r1f, 0, 0), (r2f, 1, 1)):
                    cx.add("dve", lambda e, rf=rf: e.tensor_tensor(out=eq[:], in0=rf[:].unsqueeze(3).to_broadcast([128, 8, 16, 16]), in1=io16, op=ALU.is_equal), [bb, cb_], [eqb])
                    cx.add("dve", lambda e, cidx=cidx: e.tensor_tensor(out=eq[:], in0=eq[:], in1=i4[:, :, cidx, :].unsqueeze(2).to_broadcast([128, 8, 16, 16]), op=ALU.mult), [eqb, vb], [eqb])
                    cx.add("dve", lambda e, slot=slot: e.tensor_reduce(out=IJG[:, slot, :].rearrange("p (h k) -> p h k", h=8), in_=eq[:], axis=AX.X, op=ALU.add), [eqb], [ijgb])
                cx.add("dve", lambda e: e.tensor_tensor(out=best[:], in0=best[:], in1=best[:, :, 0:1].to_broadcast([128, 8, 16]), op=ALU.subtract), [bb], [bb])
                cx.add("act", lambda e: e.activation(out=best[:], in_=best[:], func=AF.Exp), [bb], [bb])
                cx.add("dve", lambda e: e.tensor_reduce(out=zs_[:], in_=best[:], axis=AX.X, op=ALU.add), [bb], [bb])
                cx.add("dve", lambda e: e.reciprocal(out=zs_[:], in_=zs_[:]), [bb], [bb])
                cx.add("dve", lambda e: e.tensor_tensor(out=IJG[:, 2, :].rearrange("p (h k) -> p h k", h=8), in0=best[:], in1=zs_[:].unsqueeze(2).to_broadcast([128, 8, 16]), op=ALU.mult),
                       [bb], [ijgb])
                for a in range(3):
                    cx.add("pe", lambda e, a=a: e.transpose(out=PS[4][:, a * 128:(a + 1) * 128], in_=IJG[:, a, :], identity=ident_f[:]), [ijgb], [psb[4]])
                cx.add("act", lambda e: e.copy(out=IJGT[:], in_=PS[4][:, 0:384].rearrange("p (a t) -> p a t", a=3)), [psb[4]], [ijgtb])
                for hf in range(2):
                    hb_ = nh % 2
                    nh += 1
                    A_, B_ = Aoh[hb_], Boh[hb_]
                    iob = io_t[:].unsqueeze(1).to_broadcast([128, 64, 128])
                    cx.add("dve", lambda e, A_=A_, hf=hf: e.tensor_tensor(out=A_[:], in0=iob, in1=IJGT[:, 0, hf * 64:(hf + 1) * 64].unsqueeze(2).to_broadcast([128, 64, 128]), op=ALU.is_equal),
                           [cb_, ijgtb], [Abs[hb_]])
                    for tl in range(64):
                        t = hf * 64 + tl
                        cx.add("dve", lambda e, B_=B_, tl=tl, t=t: e.tensor_scalar(out=B_[:, tl, :], in0=io_t[:], scalar1=IJGT[:, 1, t:t + 1], scalar2=IJGT[:, 2, t:t + 1],
                                                                                 op0=ALU.is_equal, op1=ALU.mult), [cb_, ijgtb], [Bbs[hb_]], chain=True)
                    for t16 in range(4):
                        pbase = 4 * (t16 % 2)
                        for tl in range(16):
                            tloc = t16 * 16 + tl
                            pi = pbase + tl // 4
                            cx.add("pe", lambda e, tloc=tloc, pi=pi, tl=tl, A_=A_, B_=B_: e.matmul(PS[pi][:, (tl % 4) * 128:(tl % 4 + 1) * 128], lhsT=A_[:, tloc, :], rhs=B_[:, tloc, :], start=True, stop=True),
                                   [Abs[hb_], Bbs[hb_]], [psb[pi]])
                        for q4 in range(4):
                            pi = pbase + q4
                            tt0 = hf * 64 + t16 * 16 + q4 * 4
                            cx.add("act", lambda e, pi=pi, tt0=tt0, ms=ms: e.copy(out=Mst[ms][:, :, tt0:tt0 + 4, :].rearrange("p c t j -> p t c j"),
                                                                                  in_=PS[pi][:].rearrange("p (t c j) -> p t c j", t=4, c=16)), [psb[pi]], [Mstb[ms]], chain=True)
                t0 = ts_ * 128
                for jc in range(16):
                    cx.dma(Mscr_w[gi, jc, :, t0 * 8:(t0 + 128) * 8], Mst[ms][:, jc, :, :].rearrange("p t j -> p (t j)"), [Mstb[ms]], [], key=f"Mst{ms}")
        cx.flush()
        sb.release()

    def stage_P():
        sb = SB(nc)
        psb = psbufs()
        gb_t = sb.sb("P_gb", [128, D], F32)
        gbb = cx.buf("gb")
        cx.dma(gb_t[:], gfb, [], [gbb], key="c0")
        hts = sb.sb("P_ht", [128, DK, 512], BF16)
        htb = cx.buf("ht")
        acc = sb.sb("P_acc", [128, 4, D], F32)
        accb = [cx.buf(f"acc{i}") for i in range(4)]
        Mc = [sb.sb(f"P_Mc{i}", [128, 512, 8], BF16) for i in range(2)]
        Mcb = [cx.buf(f"Mc{i}") for i in range(2)]
        AT = sb.sb("P_AT", [128, 8, 512], BF16)
        ATb = [cx.buf(f"AT{i}") for i in range(8)]
        vtb = sb.sb("P_vtb", [128, 8, D], BF16)
        vtbb = [cx.buf(f"vtb{i}") for i in range(8)]
        NU = 4
        utb = [sb.sb(f"P_utb{i}", [128, DK * 128], BF16) for i in range(NU)]
        utbb = [cx.buf(f"utb{i}") for i in range(NU)]
        t1 = [sb.sb(f"P_t1{i}", [128, 512], F32) for i in range(2)]
        t1b = [cx.buf(f"t1{i}") for i in range(2)]
        t2 = [sb.sb(f"P_t2{i}", [128, 512], F32) for i in range(2)]
        t2b = [cx.buf(f"t2{i}") for i in range(2)]
        x1t = sb.sb("P_x1t", [128, D], F32)
        x1b = cx.buf("x1t")
        junk = sb.sb("P_junk", [128, D], BF16)
        junk_b = cx.buf("junk")
        sts = sb.sb("P_st", [128, 2], F32)
        st_b = cx.buf("st")
        n = 0
        for tp in range(NGO):
            cx.dma_k(hts, hn2T_r[:, tp * 512:(tp + 1) * 512], DK, [], [htb], "ht")
            for jc in range(16):
                mcs = jc % 2
                cx.dma(Mc[mcs][:].rearrange("p t j -> p (t j)"), Mscr_r[tp, jc, :, :], [], [Mcb[mcs]], key=f"Mc{mcs}")
                for jj in range(8):
                    j = jc * 8 + jj
                    s = n % 2
                    su = n % NU
                    n += 1
                    cx.dma(utb[su][:], UTr[j], [], [utbb[su]], key=f"utb{su}", q="pool")
                    cx.dma(vtb[:, jj, :], Vr[j], [], [vtbb[jj]], key=f"vtb{jj}", q="pool")
                    pi = 4 + (n % 4)
                    for k in range(DK):
                        cx.add("pe", lambda e, k=k, su=su, pi=pi: e.matmul(PS[pi][:], lhsT=utb[su][:, k * 128:(k + 1) * 128], rhs=hts[:, k, :], start=(k == 0), stop=(k == DK - 1)),
                               [utbb[su], htb], [psb[pi]])
                    gelu_tanh(PS[pi][:], psb[pi], None, t1[s][:], t1b[s], t2[s][:], t2b[s], AT[:, jj, :], ATb[jj], extra_mul=Mc[mcs][:, :, jj], extra_b=Mcb[mcs])
                for ts_ in range(4):
                    for dc in range(4):
                        pi = dc
                        for jj in range(8):
                            cx.add("pe", lambda e, jj=jj, ts_=ts_, dc=dc, pi=pi: e.matmul(PS[pi][:], lhsT=AT[:, jj, ts_ * 128:(ts_ + 1) * 128], rhs=vtb[:, jj, dc * 512:(dc + 1) * 512],
                                                                                          start=(jj == 0), stop=(jj == 7)), [ATb[jj], vtbb[jj]], [psb[pi]])
                        if jc == 0:
                            cx.add("act", lambda e, ts_=ts_, dc=dc, pi=pi: e.copy(out=acc[:, ts_, dc * 512:(dc + 1) * 512], in_=PS[pi][:]), [psb[pi]], [accb[ts_]])
                        else:
                            cx.add("dve", lambda e, ts_=ts_, dc=dc, pi=pi: e.tensor_tensor(out=acc[:, ts_, dc * 512:(dc + 1) * 512], in0=PS[pi][:], in1=acc[:, ts_, dc * 512:(dc + 1) * 512], op=ALU.add),
                                   [psb[pi], accb[ts_]], [accb[ts_]])
            for ts_ in range(4):
                tok0 = tp * 512 + ts_ * 128
                cx.dma(x1t[:], x1s_r[tok0:tok0 + 128, :], [], [x1b], key="x1t")
                cx.add("dve", lambda e, ts_=ts_: e.tensor_tensor(out=x1t[:], in0=x1t[:], in1=acc[:, ts_, :], op=ALU.add), [x1b, accb[ts_]], [x1b])
                ss, rs = sts[:, 0:1], sts[:, 1:2]
                cx.add("act", lambda e: e.activation(out=junk[:], in_=x1t[:], func=AF.Square, accum_out=ss), [x1b], [junk_b, st_b])
                cx.add("act", lambda e: e.activation(out=rs, in_=ss, func=AF.Sqrt, scale=1.0 / D, bias=eps_rms[:, 0:1]), [st_b], [st_b])
                cx.add("dve", lambda e: e.reciprocal(out=rs, in_=rs), [st_b], [st_b])
                cx.add("dve", lambda e, ts_=ts_: e.scalar_tensor_tensor(out=acc[:, ts_, :], in0=x1t[:], scalar=rs, in1=gb_t[:], op0=ALU.mult, op1=ALU.mult),
                       [x1b, st_b, gbb], [accb[ts_]])
                cx.dma(out[tok0:tok0 + 128, :], acc[:, ts_, :], [accb[ts_]], [], key=f"out{ts_ % 2}")
        cx.flush()
        sb.release()

    stages = {"A": stage_A, "C": stage_C, "S": stage_S, "M1": stage_M1, "M2": stage_M2, "Q0": stage_Q0, "Q": stage_Q, "P": stage_P}
    return nc, cx, stages


def _c(a):
    return np.ascontiguousarray(a, dtype=np.float32)


def prep_shared(p):
    s = {}
    tile128 = lambda v: _c(np.broadcast_to(v[None, :], (128, v.shape[0])))
    s["g1b"] = tile128(p["norm_mix"])
    s["g2b"] = tile128(p["norm_ffn"])
    s["gfb"] = tile128(p["norm_final"])
    s["w_in_r"] = _c(p["w_in"].reshape(16, 128, 56, 128).transpose(2, 1, 0, 3).reshape(56, 128, 2048))
    s["bgate"] = _c(p["b_gate"].reshape(32, 128).T)
    s["cw"] = _c(p["conv_w_dw"].reshape(KCONV, CT, 128).transpose(2, 1, 0))
    s["cb"] = _c(p["conv_b_dw"].reshape(CT, 128).T)
    s["lng"] = _c(p["conv_ln_g"].reshape(CT, 128).T)
    s["lnb"] = _c(p["conv_ln_b"].reshape(CT, 128).T)
    s["cwo"] = _c(p["conv_w_out"].reshape(CT, 128, D).transpose(1, 0, 2))
    s["wval"] = _c(p["ssm_w_val"].reshape(CT, 128, D).transpose(1, 0, 2))
    s["wgate"] = _c(p["ssm_w_gate"].reshape(CT, 128, D).transpose(1, 0, 2))
    s["wout"] = _c(p["w_out"].reshape(DK, 128, D).transpose(1, 0, 2))
    s["wq"] = _c(p["peer_w_q"].reshape(DK, 128, D).transpose(1, 0, 2))
    s["kT"] = _c(p["peer_sub_keys"].reshape(16, 128, 128).transpose(2, 0, 1))
    s["UTr"] = _c(p["peer_u"].reshape(128, 128, 16, 128).transpose(1, 3, 2, 0).reshape(128, 128, 2048))
    s["Vr"] = _c(p["peer_v"].reshape(128, 128, D).transpose(1, 0, 2))
    def st_layout(a):
        return a.reshape(32, 2, 64).transpose(1, 2, 0).reshape(128, 32)
    ldt = np.broadcast_to(p["ssm_log_dt"][:, None], (64, 64))
    s["sA"] = _c(np.stack([st_layout(p["ssm_a_re"]), st_layout(p["ssm_a_im"]), st_layout(ldt)], axis=1))
    def b_layout_rep(a):
        x = a.reshape(8, 8, 1, 64)
        x = np.broadcast_to(x, (8, 8, 16, 64))
        return x.transpose(1, 2, 0, 3).reshape(128, 8 * 64)
    s["sB"] = _c(np.stack([b_layout_rep(p["ssm_a_re"]), b_layout_rep(p["ssm_a_im"]), b_layout_rep(ldt)], axis=1))
    def bT_layout(b):
        return b.reshape(8, 8, 64, 16).transpose(1, 3, 0, 2).reshape(128, 8 * 64)
    s["bT"] = _c(np.stack([bT_layout(p["ssm_b_re"]), bT_layout(p["ssm_b_im"])], axis=1))
    def cT_layout(c):
        o = np.zeros((2, 64, 32, 128), np.float32)
        cc = c.reshape(32, 2, 16, 64)
        for j in range(32):
            for g2 in range(2):
                col0 = 32 * (j % 4) + 16 * g2
                o[g2, :, j, col0:col0 + 16] = cc[j, g2].T
        return o.reshape(128, 32 * 128)
    s["cTp"] = _c(np.stack([cT_layout(p["ssm_c_re"]), cT_layout(p["ssm_c_im"])], axis=1))
    s["dsk"] = _c(p["ssm_d"].reshape(CT, 128).T)
    mB = np.zeros((128, 128), np.float32)
    for q in range(128):
        gl = q // 16
        mB[q, (gl % 2) * 64:(gl % 2) * 64 + 64] = 1.0
    s["maskB"] = mB
    rm = np.zeros((128, 1), np.float32)
    rm[96:] = 1.0
    s["rowm"] = rm
    s["ident"] = np.eye(128, dtype=np.float32)
    s["onesc"] = np.full((128, 128), 1.0 / CW, np.float32)
    s["iota"] = _c(np.broadcast_to(np.arange(128, dtype=np.float32)[None, :], (128, 128)))
    return s


_CACHE = {}


def kernel(**inputs):
    x = np.asarray(inputs["x"], dtype=np.float32)
    B, S, _ = x.shape
    NT = S // 2
    p = {}
    for k, v in inputs.items():
        if k == "x":
            continue
        v = np.asarray(v, dtype=np.float32)
        p[k] = v if k == "norm_final" else v[0]
    shared = prep_shared(p)
    if NT not in _CACHE:
        nc, cx, stages = build(NT)
        for name in ("A", "C", "S", "M1", "M2", "Q0", "Q", "P"):
            stages[name]()
        _CACHE[NT] = nc
    nc = _CACHE[NT]
    in_maps = []
    for c in range(NCORES):
        b, half = c // 2, c % 2
        own = x[b, half * NT:(half + 1) * NT]
        prev = x[b, 0:NT] if half == 1 else np.zeros_like(own)
        m = dict(shared)
        m["xin"] = _c(np.concatenate([prev, own], axis=0))
        in_maps.append(m)
    res = run_bass_kernel_spmd(nc, in_maps, core_ids=list(range(NCORES)))
    outp = np.empty((B, S, D), np.float32)
    for c in range(NCORES):
        b, half = c // 2, c % 2
        outp[b, half * NT:(half + 1) * NT] = res.results[c]["out"]
    return outp
```
